# Optimizing a Trainium2 kernel written in Bass

```python
import math
import jax, jax.numpy as jnp
from jax import lax
import numpy as np

D_MODEL = 1024
BATCH = 2
SEQ = 8192
DEPTH = 1

N_META = 16
BLOCK = 128
HEAD_DIM = 64
ROT_DIM = HEAD_DIM // 4
ROPE_THETA = 500000.0
DSA_HEADS = 8
KV_RANK = 256
IDX_HEADS = 8
IDX_DIM = 64
TOPK_MAX = 256
FOX_HEADS = 8
DSA_W = DSA_HEADS * HEAD_DIM
FOX_W = FOX_HEADS * HEAD_DIM
N_GROUPS = 4
EXPERTS_PER_GROUP = 8
N_EXPERTS = N_GROUPS * EXPERTS_PER_GROUP
TOPK_IN_GROUP = 2
D_EXPERT = 256
MOE_BLOCK = 128
DN_ALPHA = (2.0 * DEPTH) ** 0.25
DN_BETA = (8.0 * DEPTH) ** -0.25
LN_EPS = 1e-5
RMS_EPS = 1e-6
NEG = -1e30
IN_SPLITS = (DSA_W, KV_RANK, IDX_HEADS * IDX_DIM, IDX_DIM, IDX_HEADS, FOX_W, FOX_W, FOX_W, FOX_HEADS, D_MODEL, D_MODEL)
IN_WIDTH = sum(IN_SPLITS)

kernel_name = "hybrid_dsa_fox_gated_hmoe_deepnorm"


def layer_norm(x, g, b):
    xf = x.astype(jnp.float32)
    mu = xf.mean(-1, keepdims=True)
    var = jnp.square(xf - mu).mean(-1, keepdims=True)
    return ((xf - mu) * lax.rsqrt(var + LN_EPS)).astype(x.dtype) * g + b


def rms_norm(x, g):
    xf = x.astype(jnp.float32)
    return (xf * lax.rsqrt(jnp.square(xf).mean(-1, keepdims=True) + RMS_EPS)).astype(x.dtype) * g


def partial_rope(x, pos):
    half = ROT_DIM // 2
    inv = ROPE_THETA ** (-jnp.arange(half, dtype=jnp.float32) / half)
    ang = pos.astype(jnp.float32)[:, None] * inv[None, :]
    cos = jnp.cos(ang)[None, :, None, :]
    sin = jnp.sin(ang)[None, :, None, :]
    xr = x[..., :ROT_DIM].astype(jnp.float32)
    x1, x2 = xr[..., :half], xr[..., half:]
    rot = jnp.concatenate([x1 * cos - x2 * sin, x2 * cos + x1 * sin], axis=-1).astype(x.dtype)
    return jnp.concatenate([rot, x[..., ROT_DIM:]], axis=-1)


def to_blocks(a):
    B, Lp = a.shape[0], a.shape[1]
    return jnp.moveaxis(a.reshape((B, Lp // BLOCK, BLOCK) + a.shape[2:]), 1, 0)


def from_blocks(a):
    nb, B = a.shape[0], a.shape[1]
    return jnp.moveaxis(a, 0, 1).reshape((B, nb * BLOCK) + a.shape[3:])


def dsa_attention(q, k, v, qi, ki, wi, n_keys):
    Lp = q.shape[1]
    dh = q.shape[-1]
    pad = Lp - n_keys
    topk = min(TOPK_MAX, n_keys // 4)
    kpos = jnp.arange(Lp)
    ki32 = ki.astype(jnp.float32)

    def blk(args):
        qb, qib, wib, b0 = args
        qpos = b0 * BLOCK + jnp.arange(BLOCK)
        valid = (kpos[None, :] <= qpos[:, None]) & (kpos[None, :] >= pad)
        s_idx = jnp.einsum('bqhd,bkd->bqhk', qib.astype(jnp.float32), ki32) * (IDX_DIM ** -0.5)
        s_idx = jnp.einsum('bqhk,bqh->bqk', jax.nn.relu(s_idx), wib.astype(jnp.float32))
        s_idx = jnp.where(valid[None], s_idx, NEG)
        _, sel = lax.top_k(s_idx, topk)
        sel_valid = (sel <= qpos[None, :, None]) & (sel >= pad)
        ks = jax.vmap(lambda kk, ii: kk[ii])(k, sel)
        vs = jax.vmap(lambda vv, ii: vv[ii])(v, sel)
        s = jnp.einsum('bqhd,bqkhd->bhqk', qb.astype(jnp.float32), ks.astype(jnp.float32)) * (dh ** -0.5)
        s = jnp.where(sel_valid[:, None], s, NEG)
        p = jax.nn.softmax(s, axis=-1)
        o = jnp.einsum('bhqk,bqkhd->bqhd', p, vs.astype(jnp.float32))
        return o.astype(q.dtype)

    nb = Lp // BLOCK
    out = lax.map(blk, (to_blocks(q), to_blocks(qi), to_blocks(wi), jnp.arange(nb)))
    return from_blocks(out)


def fox_attention(q, k, v, log_f, n_keys):
    Lp = q.shape[1]
    dh = q.shape[-1]
    pad = Lp - n_keys
    kpos = jnp.arange(Lp)
    c = jnp.cumsum(log_f, axis=1)
    c_k = jnp.transpose(c, (0, 2, 1))
    k32 = k.astype(jnp.float32)
    v32 = v.astype(jnp.float32)

    def blk(args):
        qb, cqb, b0 = args
        qpos = b0 * BLOCK + jnp.arange(BLOCK)
        valid = (kpos[None, :] <= qpos[:, None]) & (kpos[None, :] >= pad)
        s = jnp.einsum('bqhd,bkhd->bhqk', qb.astype(jnp.float32), k32) * (dh ** -0.5)
        s = s + jnp.transpose(cqb, (0, 2, 1))[..., None] - c_k[:, :, None, :]
        s = jnp.where(valid[None, None], s, NEG)
        p = jax.nn.softmax(s, axis=-1)
        o = jnp.einsum('bhqk,bkhd->bqhd', p, v32)
        return o.astype(q.dtype)

    nb = Lp // BLOCK
    out = lax.map(blk, (to_blocks(q), to_blocks(c), jnp.arange(nb)))
    return from_blocks(out)


def token_mixer(h, pos, w_in, b_forget, kv_norm_g, w_kv_up, w_branch_dsa, w_branch_fox, w_out):
    B, T, _ = h.shape
    pad = (-N_META) % BLOCK
    proj = h @ w_in
    offs = np.cumsum(IN_SPLITS)[:-1].tolist()
    (q_a, c_kv, q_i, k_i, w_i, q_f, k_f, v_f, f_lg, g_a, g_f) = jnp.split(proj, offs, axis=-1)

    def heads(t, n):
        return t.reshape(B, T, n, -1)

    q_a = partial_rope(heads(q_a, DSA_HEADS), pos)
    kv = rms_norm(c_kv, kv_norm_g) @ w_kv_up
    k_a, v_a = jnp.split(kv, 2, axis=-1)
    k_a = partial_rope(heads(k_a, DSA_HEADS), pos)
    v_a = heads(v_a, DSA_HEADS)
    q_i = partial_rope(heads(q_i, IDX_HEADS), pos)
    k_i = partial_rope(k_i[:, :, None, :], pos)[:, :, 0]
    w_i = w_i * (IDX_HEADS ** -0.5)
    q_f, k_f, v_f = heads(q_f, FOX_HEADS), heads(k_f, FOX_HEADS), heads(v_f, FOX_HEADS)
    log_f = jax.nn.log_sigmoid((f_lg + b_forget).astype(jnp.float32))

    def pf(t):
        return jnp.pad(t, [(0, 0), (pad, 0)] + [(0, 0)] * (t.ndim - 2))

    y_a = dsa_attention(pf(q_a), pf(k_a), pf(v_a), pf(q_i), pf(k_i), pf(w_i), T)[:, pad:].reshape(B, T, DSA_W)
    y_f = fox_attention(pf(q_f), pf(k_f), pf(v_f), pf(log_f), T)[:, pad:].reshape(B, T, FOX_W)
    merged = jax.nn.sigmoid(g_a) * (y_a @ w_branch_dsa) + jax.nn.sigmoid(g_f) * (y_f @ w_branch_fox)
    return merged @ w_out


def hierarchical_moe(h, w_rg, b_rg, w_re, b_re, w_gate, w_up, w_down):
    B, T, D = h.shape
    xf = h.reshape(-1, D)
    N = xf.shape[0]
    g_logits = (xf @ w_rg).astype(jnp.float32) + b_rg
    g_prob = jax.nn.softmax(g_logits, axis=-1)
    g_sel = jnp.argmax(g_logits, axis=-1)
    rows = jnp.arange(N)
    p_group = g_prob[rows, g_sel][:, None]
    e_logits = ((xf @ w_re).astype(jnp.float32) + b_re).reshape(N, N_GROUPS, EXPERTS_PER_GROUP)
    e_logits = e_logits[rows, g_sel]
    top_l, top_i = lax.top_k(e_logits, TOPK_IN_GROUP)
    gate = p_group * jax.nn.softmax(top_l, axis=-1)
    eid = (g_sel[:, None] * EXPERTS_PER_GROUP + top_i).reshape(-1)
    tok = jnp.repeat(rows, TOPK_IN_GROUP)
    gw = gate.reshape(-1)
    A = N * TOPK_IN_GROUP
    order = jnp.argsort(eid)
    eid_s, tok_s, gw_s = eid[order], tok[order], gw[order]
    counts = jnp.zeros((N_EXPERTS,), jnp.int32).at[eid].add(1)
    start = jnp.cumsum(counts) - counts
    pcounts = (counts + MOE_BLOCK - 1) // MOE_BLOCK * MOE_BLOCK
    pend = jnp.cumsum(pcounts)
    pstart = pend - pcounts
    dest = pstart[eid_s] + jnp.arange(A) - start[eid_s]
    n_blocks = (A + N_EXPERTS * (MOE_BLOCK - 1) + MOE_BLOCK - 1) // MOE_BLOCK
    R = n_blocks * MOE_BLOCK
    buf_x = jnp.zeros((R, D), h.dtype).at[dest].set(xf[tok_s])
    buf_w = jnp.zeros((R,), jnp.float32).at[dest].set(gw_s.astype(jnp.float32))
    buf_tok = jnp.zeros((R,), jnp.int32).at[dest].set(tok_s)
    blk_e = jnp.minimum(jnp.searchsorted(pend, jnp.arange(n_blocks) * MOE_BLOCK, side='right'), N_EXPERTS - 1)

    def expert_block(args):
        xb, e = args
        return (jax.nn.silu(xb @ w_gate[e]) * (xb @ w_up[e])) @ w_down[e]

    y = lax.map(expert_block, (buf_x.reshape(n_blocks, MOE_BLOCK, D), blk_e)).reshape(R, D)
    out = jnp.zeros((N, D), jnp.float32).at[buf_tok].add(y.astype(jnp.float32) * buf_w[:, None])
    return out.astype(h.dtype).reshape(B, T, D)


def setup_inputs(seed: int = 0) -> dict:
    key = jax.random.key(seed)
    ks = jax.random.split(key, 24)
    n = jax.random.normal
    f32 = jnp.float32
    L = DEPTH
    return {
        "x": n(ks[0], (BATCH, SEQ, D_MODEL), f32),
        "meta_tokens": n(ks[1], (N_META, D_MODEL), f32),
        "emb_ln_g": 1.0 + 0.02 * n(ks[2], (D_MODEL,), f32),
        "emb_ln_b": 0.02 * n(ks[3], (D_MODEL,), f32),
        "w_in": n(ks[4], (L, D_MODEL, IN_WIDTH), f32) * D_MODEL ** -0.5,
        "b_forget": 3.0 + 0.5 * n(ks[5], (L, FOX_HEADS), f32),
        "kv_norm_g": 1.0 + 0.02 * n(ks[6], (L, KV_RANK), f32),
        "w_kv_up": n(ks[7], (L, KV_RANK, 2 * DSA_W), f32) * KV_RANK ** -0.5,
        "w_branch_dsa": n(ks[8], (L, DSA_W, D_MODEL), f32) * DSA_W ** -0.5,
        "w_branch_fox": n(ks[9], (L, FOX_W, D_MODEL), f32) * FOX_W ** -0.5,
        "w_out": n(ks[10], (L, D_MODEL, D_MODEL), f32) * (D_MODEL ** -0.5 * DN_BETA),
        "ln1_g": 1.0 + 0.02 * n(ks[11], (L, D_MODEL), f32),
        "ln1_b": 0.02 * n(ks[12], (L, D_MODEL), f32),
        "w_router_group": n(ks[13], (L, D_MODEL, N_GROUPS), f32) * D_MODEL ** -0.5,
        "b_router_group": 0.01 * n(ks[14], (L, N_GROUPS), f32),
        "w_router_expert": n(ks[15], (L, D_MODEL, N_EXPERTS), f32) * D_MODEL ** -0.5,
        "b_router_expert": 0.01 * n(ks[16], (L, N_EXPERTS), f32),
        "w_gate": n(ks[17], (L, N_EXPERTS, D_MODEL, D_EXPERT), f32) * D_MODEL ** -0.5,
        "w_up": n(ks[18], (L, N_EXPERTS, D_MODEL, D_EXPERT), f32) * D_MODEL ** -0.5,
        "w_down": n(ks[19], (L, N_EXPERTS, D_EXPERT, D_MODEL), f32) * (D_EXPERT ** -0.5 * DN_BETA),
        "ln2_g": 1.0 + 0.02 * n(ks[20], (L, D_MODEL), f32),
        "ln2_b": 0.02 * n(ks[21], (L, D_MODEL), f32),
    }


def reference(x, meta_tokens, emb_ln_g, emb_ln_b, w_in, b_forget, kv_norm_g, w_kv_up, w_branch_dsa,
              w_branch_fox, w_out, ln1_g, ln1_b, w_router_group, b_router_group, w_router_expert,
              b_router_expert, w_gate, w_up, w_down, ln2_g, ln2_b):
    B = x.shape[0]
    meta = jnp.broadcast_to(meta_tokens.astype(x.dtype)[None], (B, N_META, x.shape[-1]))
    h = layer_norm(jnp.concatenate([meta, x], axis=1), emb_ln_g, emb_ln_b)
    pos = jnp.arange(h.shape[1])
    for l in range(DEPTH):
        mix = token_mixer(h, pos, w_in[l], b_forget[l], kv_norm_g[l], w_kv_up[l],
                          w_branch_dsa[l], w_branch_fox[l], w_out[l])
        h = layer_norm(DN_ALPHA * h + mix, ln1_g[l], ln1_b[l])
        ffn = hierarchical_moe(h, w_router_group[l], b_router_group[l], w_router_expert[l],
                               b_router_expert[l], w_gate[l], w_up[l], w_down[l])
        h = layer_norm(DN_ALPHA * h + ffn, ln2_g[l], ln2_b[l])
    return h[:, N_META:]
```

```python
import os
import numpy as np
import ml_dtypes
from contextlib import ExitStack
import concourse.bass as bass
import concourse.mybir as mybir
from concourse.bass_utils import run_bass_kernel_spmd

F32 = mybir.dt.float32
BF16 = mybir.dt.bfloat16
AF = mybir.ActivationFunctionType
ALU = mybir.AluOpType
AX = mybir.AxisListType

D = 1024
SEQ = 8192
NMETA = 16
PAD = 112
TP = SEQ + NMETA + PAD
NKB = TP // 128
NSLOT = 16
NQ = NSLOT * 128
NBIS = 24
ALPHA = 2.0 ** 0.25
NEGM = -30000.0
NEXP = 32
VW = 68
NEXP_RUN = 32

WK_COLS = 256 + 128 + 512 + 512 + 8
WQ_COLS = 512 * 3 + 8


class Sched:
    ENG = ("pe", "act", "dve", "pool", "sp")
    NDMA = 8

    def __init__(self):
        self.ops = {e: [] for e in self.ENG}
        self.count = {e: 0 for e in self.ENG}
        self.seen = {e: {} for e in self.ENG}
        self.res = {}
        self.dma_rr = {e: 0 for e in self.ENG}
        self.dma_val = {}

    def op(self, eng, fn, reads=(), writes=(), dma=False):
        self.nops = getattr(self, "nops", 0) + 1
        if self.nops > getattr(self, "maxops", 10 ** 9):
            return None
        need = {}

        def add(tok):
            if tok is None:
                return
            k, v = tok
            if need.get(k, 0) < v:
                need[k] = v

        reads = list(reads)
        writes = list(writes)
        for r in list(reads):
            if isinstance(r, tuple) and r and r[0] in ("PF", "PB") and r not in writes:
                writes.append(r)
        for r in reads:
            st = self.res.get(r)
            if st:
                add(st["w"])
        for w in writes:
            st = self.res.get(w)
            if st:
                add(st["w"])
                for k, v in st["r"].items():
                    add((k, v))
        waits = []
        for k, v in need.items():
            if eng == "pe" and k == "pe":
                continue
            if self.seen[eng].get(k, 0) >= v:
                continue
            waits.append((k, v))
            self.seen[eng][k] = v
        if dma:
            idx = self.dma_rr[eng]
            self.dma_rr[eng] = (idx + 1) % self.NDMA
            key = ("dma", eng, idx)
            prev = self.dma_val.get(key, 0)
            if prev > 0 and self.seen[eng].get(key, 0) < prev:
                waits.append((key, prev))
                self.seen[eng][key] = prev
            val = prev + 16
            self.dma_val[key] = val
            tok = (key, val)
        else:
            self.count[eng] += 1
            tok = (eng, self.count[eng])
        self.ops[eng].append((waits, fn, tok))
        for r in reads:
            st = self.res.get(r)
            if not st:
                st = {"w": None, "r": {}}
                self.res[r] = st
            if st["r"].get(tok[0], 0) < tok[1]:
                st["r"][tok[0]] = tok[1]
        for w in writes:
            self.res[w] = {"w": tok, "r": {}}
        return tok

    def barrier(self):
        toks = [(e, self.count[e]) for e in self.ENG if self.count[e] > 0]
        toks += [(k, v) for k, v in self.dma_val.items()]
        for e in self.ENG:
            waits = []
            for k, v in toks:
                if k == e and e == "pe":
                    continue
                if self.seen[e].get(k, 0) >= v:
                    continue
                waits.append((k, v))
                self.seen[e][k] = v
            if waits:
                self.ops[e].append((waits, None, None))

    def finish(self):
        waits = [(k, v) for k, v in self.dma_val.items() if self.seen["sp"].get(k, 0) < v]
        self.ops["sp"].append((waits, None, None))

    def prepare(self):
        import bisect as _b
        sig = {e: set() for e in self.ENG}
        for e in self.ENG:
            for waits, fn, tok in self.ops[e]:
                for k, v in waits:
                    if k in sig and k != e:
                        sig[k].add(v)
        self.sig = {e: sorted(v) for e, v in sig.items()}
        self._b = _b

    def rank(self, k, v):
        lst = self.sig[k]
        i = self._b.bisect_right(lst, v)
        assert i > 0 and lst[i - 1] == v, (k, v)
        return i

    def replay(self, name, e, sems):
        sigset = set(self.sig[name])
        for waits, fn, tok in self.ops[name]:
            for k, v in waits:
                if k == name:
                    e.drain()
                elif k in self.sig:
                    e.wait_ge(sems[k], self.rank(k, v))
                else:
                    e.wait_ge(sems[k], v)
            if fn is None:
                continue
            ins = fn(e)
            if tok[0][0] == "dma":
                ins.then_inc(sems[tok[0]], 16)
            elif tok[1] in sigset:
                ins.then_inc(sems[tok[0]], 1)


def build(nslot=NSLOT, nexp=NEXP, debug=False, nkb=NKB, stop_after=9):
    nc = bass.Bass("TRN2", target_bir_lowering=False)
    S = Sched()
    import os
    S.maxops = int(os.environ.get("MAXOPS", 10 ** 9))
    nq = nslot * 128

    def din(name, shape, dt=F32):
        return nc.dram_tensor(name, list(shape), dt, kind="ExternalInput").ap()

    def dscr(name, shape, dt=BF16):
        return nc.dram_tensor(name, list(shape), dt, kind="Internal").ap()

    xk = din("xk", [TP, D])
    xq = din("xq", [nq, D])
    wk_d = din("wk", [D, WK_COLS])
    wq_d = din("wq", [D, WQ_COLS])
    wg_d = din("wg", [D, 2048])
    wkv_d = din("wkv", [256, 1024])
    wba_d = din("wba", [512, 1024])
    wbf_d = din("wbf", [512, 1024])
    wo_d = din("wo", [D, D])
    wr_d = din("wr", [D, 36])
    wgate_d = din("wgate", [nexp, D, 256])
    wup_d = din("wup", [nexp, D, 256])
    wdn_d = din("wdn", [nexp, 256, D])
    lnp_d = din("lnp", [128, 6 * D])
    kvg_d = din("kvg", [128, 256])
    bfg_d = din("bfg", [128, 8])
    brt_d = din("brt", [128, 36])
    ck_d = din("ck", [128, TP])
    sk_d = din("sk", [128, TP])
    cq_d = din("cq", [128, nq])
    sq_d = din("sq", [128, nq])
    cf32_d = din("cf32", [128, 128 * 3 + 512 + 4 + NBIS + 1])
    cbf_d = din("cbf", [128, 128 + 512 + 128 + 512], BF16)
    out_d = nc.dram_tensor("out", [nq, D], F32, kind="ExternalOutput").ap()

    kaT_d = dscr("kaT_d", [4, 128, TP])
    kfT_d = dscr("kfT_d", [4, 128, TP])
    va_d = dscr("va_d", [NKB, 128, 8 * VW])
    vf_d = dscr("vf_d", [NKB, 128, 8 * VW])
    qaT_d = dscr("qaT_d", [4, 128, nq])
    qiT_d = dscr("qiT_d", [4, 128, nq])
    qfT_d = dscr("qfT_d", [4, 128, nq])
    hqT_d = dscr("hqT_d", [8, 128, nq])
    h1_d = dscr("h1_d", [nq, D], F32)
    h1T_d = dscr("h1T_d", [8, 128, nq])

    es = ExitStack()
    with es:
        def sb(name, cols, dt=F32, parts=128):
            return es.enter_context(nc.sbuf_tensor(name, [parts, cols], dt))

        CF = sb("CF", 128 * 3 + 512 + 4 + NBIS + 1)
        CB = sb("CB", 128 + 512 + 128 + 512, BF16)
        LNP = sb("LNP", 6 * D)
        KVG = sb("KVG", 256)
        BFG = sb("BFG", 8)
        BRT = sb("BRT", 36)
        identF = CF[:, 0:128]
        utri = CF[:, 128:256]
        onesF = CF[:, 256:384]
        cmadd = CF[:, 384:896]
        selr = CF[:, 896:900]
        pow2 = CF[:, 900:900 + NBIS]
        padmask = CF[:, 900 + NBIS:901 + NBIS]
        identB = CB[:, 0:128]
        ident4 = CB[:, 128:640]
        ropeP = CB[:, 640:768]
        cmneg = CB[:, 768:1280]

        ARENA_COLS = 38 * 1024
        ARENA = sb("ARENA", ARENA_COLS)
        ARENA_B = ARENA[:, :].bitcast(BF16)

        class Bump:
            def __init__(self):
                self.off = 0

            def f32(self, cols):
                o = self.off // 4
                self.off += cols * 4
                assert self.off <= ARENA_COLS * 4, self.off
                return ARENA[:, o:o + cols]

            def bf(self, cols):
                cols2 = (cols + 1) // 2 * 2
                o = self.off // 2
                self.off += cols2 * 2
                assert self.off <= ARENA_COLS * 4, self.off
                return ARENA_B[:, o:o + cols]

        PF = [es.enter_context(nc.psum_tensor(f"PF{i}", [128, 512], F32)) for i in range(6)]
        PB = [es.enter_context(nc.psum_tensor(f"PB{i}", [128, 1024], BF16)) for i in range(2)]
        rr = {"n": 0, "b": 0}

        def pf(lo=0, hi=6):
            i = lo + rr["n"] % (hi - lo)
            rr["n"] += 1
            return PF[i], ("PF", i)

        def pb():
            i = rr["b"] % 2
            rr["b"] += 1
            return PB[i], ("PB", i)

        sems = {}
        for e in Sched.ENG:
            sems[e] = es.enter_context(nc.semaphore(f"s_{e}"))
        for e in ("sp", "pool", "act"):
            for i in range(Sched.NDMA):
                sems[("dma", e, i)] = es.enter_context(nc.semaphore(f"d_{e}{i}"))

        def dma(q, out, in_, reads, writes):
            S.op(q, lambda e, o=out, i=in_: e.dma_start(out=o, in_=i), reads, writes, dma=True)

        def mm(out, lhsT, rhs, start, stop, skip=True):
            return lambda e: e.matmul(out, lhsT, rhs, start=start, stop=stop, skip_group_check=skip)

        def mm_chain(items):
            def fn(e):
                ins = None
                for (o, l, r, st, sp_) in items:
                    ins = e.matmul(o, l, r, start=st, stop=sp_, skip_group_check=True)
                return ins
            return fn

        def act(out, in_, func, reads, writes, bias=0.0, scale=1.0, accum=None):
            if accum is None:
                S.op("act", lambda e: e.activation(out=out, in_=in_, func=func, bias=bias, scale=scale), reads, writes)
            else:
                S.op("act", lambda e: e.activation(out=out, in_=in_, func=func, bias=bias, scale=scale, accum_out=accum), reads, writes)

        def tt(eng, out, in0, in1, op, reads, writes):
            S.op(eng, lambda e: e.tensor_tensor(out=out, in0=in0, in1=in1, op=op), reads, writes)

        def ts(eng, out, in0, s1, s2, op0, op1, reads, writes, accum=None):
            if accum is None:
                if op1 is None:
                    S.op(eng, lambda e: e.tensor_scalar(out, in0, s1, None, op0), reads, writes)
                else:
                    S.op(eng, lambda e: e.tensor_scalar(out, in0, s1, s2, op0, op1), reads, writes)
            else:
                S.op(eng, lambda e: e.tensor_scalar(out, in0, s1, s2, op0, op1, accum), reads, writes)

        def stt(eng, out, in0, scalar, in1, op0, op1, reads, writes):
            S.op(eng, lambda e: e.scalar_tensor_tensor(out=out, in0=in0, scalar=scalar, in1=in1, op0=op0, op1=op1), reads, writes)

        def copy(eng, out, in_, reads, writes):
            if eng == "act":
                S.op("act", lambda e: e.copy(out, in_), reads, writes)
            else:
                S.op(eng, lambda e: e.tensor_copy(out, in_), reads, writes)

        def memset(eng, ap, val, writes):
            S.op(eng, lambda e: e.memset(ap, val), (), writes)

        def transpose_to(out_ps, in_sb, ident):
            return lambda e: e.transpose(out_ps, in_sb, ident)

        dma("sp", CF[:, :], cf32_d, (), ["CF"])
        dma("sp", CB[:, :], cbf_d, (), ["CB"])
        dma("sp", LNP[:, :], lnp_d, (), ["LNP"])
        dma("sp", KVG[:, :], kvg_d, (), ["KVG"])
        dma("sp", BFG[:, :], bfg_d, (), ["BFG"])
        dma("sp", BRT[:, :], brt_d, (), ["BRT"])
        CONST = ["CF", "CB", "LNP", "KVG", "BFG", "BRT"]

        uid = {"n": 0}

        def layer_norm_tile(x32, gcol, bcol, out_ap, tag, rx, wout, eps=1e-5, prescale=None):
            uid["n"] += 1
            u = uid["n"] % 2
            st = LNS[u]
            rs = ("LNS", u)
            rx = rx if isinstance(rx, list) else [rx]
            S.op("dve", lambda e: e.bn_stats(st[:, 0:6], x32[:, 0:512]), rx, [rs])
            S.op("dve", lambda e: e.bn_stats(st[:, 6:12], x32[:, 512:1024]), rx, [(rs, 1)])
            S.op("dve", lambda e: e.bn_aggr(st[:, 12:14], st[:, 0:12]), [rs, (rs, 1)], [(rs, 2)])
            ts("dve", st[:, 11:12], st[:, 13:14], eps, None, ALU.add, None, [(rs, 2), (rs, 1)], [(rs, 5)])
            act(st[:, 10:11], st[:, 11:12], AF.Ln, [(rs, 5)], [(rs, 6)])
            act(st[:, 14:15], st[:, 10:11], AF.Exp, [(rs, 6)], [(rs, 3)], scale=-0.5)
            stt("dve", st[:, 15:16], st[:, 12:13], -1.0, st[:, 14:15], ALU.mult, ALU.mult, [(rs, 2), (rs, 3)], [(rs, 4)])
            xn = LNX[u]
            rxn = ("LNX", u)
            act(xn[:, :], x32, AF.Identity, rx + [(rs, 3), (rs, 4)], [rxn], bias=st[:, 15:16], scale=st[:, 14:15])
            tt("dve", xn[:, :], xn[:, :], LNP[:, gcol * D:(gcol + 1) * D], ALU.mult, [rxn, "LNP"], [rxn])
            tt("pool", out_ap, xn[:, :], LNP[:, bcol * D:(bcol + 1) * D], ALU.add, [rxn, "LNP"], wout)

        LNS = [sb(f"LNS{i}", 16) for i in range(2)]
        LNX = [sb(f"LNX{i}", D) for i in range(2)]

        A = Bump()
        KIT = A.bf(TP)
        LOGF = A.f32(NKB * 8)
        WIQ = A.f32(nslot * 8)
        CC = A.f32(NKB * 8)
        PI = A.f32(NKB * 8)
        CREF = A.f32(nslot * 8)
        P1_END = A.off
        WKs = A.bf(8 * WK_COLS)
        WK = WKs.rearrange("p (k n) -> p k n", k=8)
        WQs = A.bf(8 * WQ_COLS)
        WQ = WQs.rearrange("p (k n) -> p k n", k=8)
        WKVs = A.bf(2 * 1024)
        WKV = WKVs.rearrange("p (k n) -> p k n", k=2)
        XT = [A.f32(D) for _ in range(2)]
        HB = [A.bf(D) for _ in range(2)]
        HT = [A.bf(8 * 512).rearrange("p (k n) -> p k n", k=8) for _ in range(2)]
        FMB = [A.bf(512) for _ in range(2)]
        RT1 = [A.f32(512) for _ in range(2)]
        RT2 = [A.f32(512) for _ in range(2)]
        CS = [A.f32(1024) for _ in range(2)]
        OUTB = [A.bf(512) for _ in range(3)]
        VST = [A.bf(8 * VW) for _ in range(2)]
        CKV32 = [A.f32(256) for _ in range(2)]
        CKVB = [A.bf(256) for _ in range(2)]
        CKVT = [A.bf(2 * 512).rearrange("p (k n) -> p k n", k=2) for _ in range(2)]
        SM = [A.f32(32) for _ in range(2)]

        dma("pool", WK, wk_d.rearrange("(k p) n -> p k n", p=128), (), ["WK"])
        dma("pool", WQ, wq_d.rearrange("(k p) n -> p k n", p=128), (), ["WQ"])
        dma("pool", WKV, wkv_d.rearrange("(k p) n -> p k n", p=128), (), ["WKV"])

        cnt = {"fm": 0, "ob": 0, "vs": 0, "ck": 0}
        if nkb < NKB:
            memset("pool", LOGF[:, :], 0.0, [("LOGF", kb) for kb in range(NKB)])

        def rope_store(ps, rps, n, cs_ap, rcs, dst_dram, rdst, dup_dst=None, rdup=None):
            i = cnt["fm"] % 2
            cnt["fm"] += 1
            copy("act", FMB[i][:, :n], ps[:, :n], [rps], [("FMB", i)])
            tt("dve", RT1[i][:, :n], ps[:, :n], cs_ap[:, 0:n], ALU.mult, [rps, rcs], [("RT1", i)])
            pp, rpp = pf()
            S.op("pe", mm(pp[:, :n], ropeP, FMB[i][:, :n], True, True), ["CB", ("FMB", i)], [rpp])
            tt("dve", RT2[i][:, :n], pp[:, :n], cs_ap[:, 512:512 + n], ALU.mult, [rpp, rcs], [("RT2", i)])
            o = cnt["ob"] % 3
            cnt["ob"] += 1
            if dup_dst is not None:
                tt("pool", dup_dst, RT1[i][:, :n], RT2[i][:, :n], ALU.add, [("RT1", i), ("RT2", i)], [rdup])
            else:
                tt("pool", OUTB[o][:, :n], RT1[i][:, :n], RT2[i][:, :n], ALU.add, [("RT1", i), ("RT2", i)], [("OUTB", o)])
                dma("sp", dst_dram, OUTB[o][:, :n], [("OUTB", o)], [rdst])

        def plain_store(ps, rps, n, dst_dram, rdst):
            o = cnt["ob"] % 3
            cnt["ob"] += 1
            copy("act", OUTB[o][:, :n], ps[:, :n], [rps], [("OUTB", o)])
            dma("sp", dst_dram, OUTB[o][:, :n], [("OUTB", o)], [rdst])

        def load_ln_transpose(src_rows, ti, hti, col0):
            b = ti % 2
            dma("sp", XT[b][:, :], src_rows, (), [("XT", b)])
            layer_norm_tile(XT[b], 0, 1, HB[b][:, :], "emb", ("XT", b), [("HB", b)])
            for half in range(2):
                tp_, rtp = pb()
                def fn(e, tp_=tp_, b=b, half=half):
                    ins = None
                    for kc in range(4):
                        k = half * 4 + kc
                        ins = e.transpose(tp_[:, kc * 128:(kc + 1) * 128], HB[b][:, k * 128:(k + 1) * 128], identB)
                    return ins
                S.op("pe", fn, [("HB", b), "CB"], [rtp])
                dst = HT[hti][:, half * 4:half * 4 + 4, col0:col0 + 128]
                src = tp_[:, 0:512].rearrange("p (k n) -> p k n", k=4)
                copy("act" if half == 0 else "dve", dst, src, [rtp], [("HT", hti, half, col0)])

        def ht_res(hti, ntile):
            return [("HT", hti, half, c * 128) for half in range(2) for c in range(ntile)]

        def proj_fm(W, col0, hti, n, rw):
            ps, rps = pf()
            items = [(ps[:, :n], W[:, kc, col0:col0 + 128], HT[hti][:, kc, :n], kc == 0, kc == 7) for kc in range(8)]
            S.op("pe", mm_chain(items), [rw] + ht_res(hti, (n + 127) // 128), [rps])
            return ps, rps

        nchunks = (nkb + 3) // 4 if stop_after >= 1 else 0
        for ch in range(nchunks):
            t0 = ch * 4
            ntile = min(4, nkb - t0)
            n = ntile * 128
            k0 = t0 * 128
            hti = ch % 2
            for t in range(ntile):
                load_ln_transpose(xk[(t0 + t) * 128:(t0 + t + 1) * 128, :], t0 + t, hti, t * 128)
            ci = ch % 2
            dma("sp", CS[ci][:, 0:n], ck_d[:, k0:k0 + n], (), [("CS", ci, 0)])
            dma("sp", CS[ci][:, 512:512 + n], sk_d[:, k0:k0 + n], (), [("CS", ci, 1)])
            csr = [("CS", ci, 0), ("CS", ci, 1)]
            ps, rps = proj_fm(WK, 256, hti, n, "WK")
            i = cnt["fm"] % 2
            cnt["fm"] += 1
            copy("act", FMB[i][:, :n], ps[:, :n], [rps], [("FMB", i)])
            tt("dve", RT1[i][:, :n], ps[:, :n], CS[ci][:, 0:n], ALU.mult, [rps] + csr, [("RT1", i)])
            pp, rpp = pf()
            S.op("pe", mm(pp[:, :n], ropeP, FMB[i][:, :n], True, True), ["CB", ("FMB", i)], [rpp])
            tt("dve", RT2[i][:, :n], pp[:, :n], CS[ci][:, 512:512 + n], ALU.mult, [rpp] + csr, [("RT2", i)])
            tt("pool", KIT[:, k0:k0 + n], RT1[i][:, :n], RT2[i][:, :n], ALU.add, [("RT1", i), ("RT2", i)], [("KIT", ch)])
            for ot in range(4):
                ps, rps = proj_fm(WK, 384 + ot * 128, hti, n, "WK")
                plain_store(ps, rps, n, kfT_d[ot, :, k0:k0 + n], ("kfT_d", ot, ch))
            for t in range(ntile):
                kb = t0 + t
                hres = [("HT", hti, half, t * 128) for half in range(2)]
                ps, rps = pf()
                items = [(ps[:, 0:256], HT[hti][:, kc, t * 128:(t + 1) * 128], WK[:, kc, 0:256], kc == 0, kc == 7) for kc in range(8)]
                S.op("pe", mm_chain(items), ["WK"] + hres, [rps])
                c = cnt["ck"] % 2
                cnt["ck"] += 1
                act(CKV32[c][:, :], ps[:, 0:256], AF.Square, [rps], [("CKV32", c)], accum=SM[c][:, 0:1])
                ts("dve", SM[c][:, 1:2], SM[c][:, 0:1], 1.0 / 256.0, 1e-6, ALU.mult, ALU.add, [("CKV32", c)], [("SM", c, 1)])
                act(SM[c][:, 3:4], SM[c][:, 1:2], AF.Ln, [("SM", c, 1)], [("SM", c, 3)])
                act(SM[c][:, 2:3], SM[c][:, 3:4], AF.Exp, [("SM", c, 3)], [("SM", c, 2)], scale=-0.5)
                stt("dve", CKVB[c][:, :], ps[:, 0:256], SM[c][:, 2:3], KVG[:, :], ALU.mult, ALU.mult, [rps, ("SM", c, 2), "KVG"], [("CKVB", c)])
                tp_, rtp = pb()
                def fn(e, tp_=tp_, c=c):
                    ins = None
                    for kc in range(2):
                        ins = e.transpose(tp_[:, kc * 128:(kc + 1) * 128], CKVB[c][:, kc * 128:(kc + 1) * 128], identB)
                    return ins
                S.op("pe", fn, [("CKVB", c), "CB"], [rtp])
                copy("act", CKVT[hti][:, :, t * 128:(t + 1) * 128], tp_[:, 0:256].rearrange("p (k n) -> p k n", k=2), [rtp], [("CKVT", hti, t)])
                ps, rps = pf()
                items = [(ps[:, 0:512], CKVT[hti][:, kc, t * 128:(t + 1) * 128], WKV[:, kc, 512:1024], kc == 0, kc == 1) for kc in range(2)]
                S.op("pe", mm_chain(items), ["WKV", ("CKVT", hti, t)], [rps])
                v = cnt["vs"] % 2
                cnt["vs"] += 1
                VV = VST[v].rearrange("p (h c) -> p h c", h=8)
                memset("pool", VST[v][:, :], 1.0, [("VST", v)])
                if kb == 0:
                    ts("dve", VV[:, :, 0:64], ps[:, 0:512].rearrange("p (h c) -> p h c", h=8), padmask, None, ALU.mult, None, [rps, ("VST", v), "CF"], [("VST", v)])
                    ts("dve", VV[:, :, 64:65], VV[:, :, 64:65], padmask, None, ALU.mult, None, [("VST", v), "CF"], [("VST", v)])
                else:
                    copy("act", VV[:, :, 0:64], ps[:, 0:512].rearrange("p (h c) -> p h c", h=8), [rps, ("VST", v)], [("VST", v)])
                dma("sp", va_d[kb], VST[v][:, :], [("VST", v)], [("va_d", kb)])
                ps, rps = pf()
                items = [(ps[:, 0:512], HT[hti][:, kc, t * 128:(t + 1) * 128], WK[:, kc, 896:1408], kc == 0, kc == 7) for kc in range(8)]
                S.op("pe", mm_chain(items), ["WK"] + hres, [rps])
                v = cnt["vs"] % 2
                cnt["vs"] += 1
                VV = VST[v].rearrange("p (h c) -> p h c", h=8)
                memset("pool", VST[v][:, :], 1.0, [("VST", v)])
                if kb == 0:
                    ts("dve", VV[:, :, 0:64], ps[:, 0:512].rearrange("p (h c) -> p h c", h=8), padmask, None, ALU.mult, None, [rps, ("VST", v), "CF"], [("VST", v)])
                    ts("dve", VV[:, :, 64:65], VV[:, :, 64:65], padmask, None, ALU.mult, None, [("VST", v), "CF"], [("VST", v)])
                else:
                    copy("act", VV[:, :, 0:64], ps[:, 0:512].rearrange("p (h c) -> p h c", h=8), [rps, ("VST", v)], [("VST", v)])
                dma("sp", vf_d[kb], VST[v][:, :], [("VST", v)], [("vf_d", kb)])
                ps, rps = pf()
                items = [(ps[:, 0:8], HT[hti][:, kc, t * 128:(t + 1) * 128], WK[:, kc, 1408:1416], kc == 0, kc == 7) for kc in range(8)]
                S.op("pe", mm_chain(items), ["WK"] + hres, [rps])
                c2 = cnt["ck"] % 2
                tt("dve", SM[c2][:, 8:16], ps[:, 0:8], BFG[:, :], ALU.add, [rps, "BFG"], [("SM", c2, 8)])
                act(SM[c2][:, 16:24], SM[c2][:, 8:16], AF.Exp, [("SM", c2, 8)], [("SM", c2, 16)], scale=-1.0)
                act(SM[c2][:, 24:32], SM[c2][:, 16:24], AF.Ln, [("SM", c2, 16)], [("SM", c2, 24)], bias=1.0)
                if kb == 0:
                    stt("dve", LOGF[:, kb * 8:(kb + 1) * 8], SM[c2][:, 24:32], -1.0, padmask.to_broadcast([128, 8]), ALU.mult, ALU.mult, [("SM", c2, 24), "CF"], [("LOGF", kb)])
                else:
                    ts("dve", LOGF[:, kb * 8:(kb + 1) * 8], SM[c2][:, 24:32], -1.0, None, ALU.mult, None, [("SM", c2, 24)], [("LOGF", kb)])
            for ot in range(4):
                ps, rps = pf()
                items = [(ps[:, :n], WKV[:, kc, ot * 128:(ot + 1) * 128], CKVT[hti][:, kc, :n], kc == 0, kc == 1) for kc in range(2)]
                S.op("pe", mm_chain(items), ["WKV"] + [("CKVT", hti, t) for t in range(ntile)], [rps])
                i = cnt["fm"] % 2
                cnt["fm"] += 1
                copy("act", FMB[i][:, :n], ps[:, :n], [rps], [("FMB", i)])
                tt("dve", RT1[i][:, :n], ps[:, :n], CS[ci][:, 0:n], ALU.mult, [rps] + csr, [("RT1", i)])
                pp, rpp = pf()
                S.op("pe", mm(pp[:, :n], ropeP, FMB[i][:, :n], True, True), ["CB", ("FMB", i)], [rpp])
                tt("dve", RT2[i][:, :n], pp[:, :n], CS[ci][:, 512:512 + n], ALU.mult, [rpp] + csr, [("RT2", i)])
                o = cnt["ob"] % 3
                cnt["ob"] += 1
                tt("pool", OUTB[o][:, :n], RT1[i][:, :n], RT2[i][:, :n], ALU.add, [("RT1", i), ("RT2", i)], [("OUTB", o)])
                dma("sp", kaT_d[ot, :, k0:k0 + n], OUTB[o][:, :n], [("OUTB", o)], [("kaT_d", ot, ch)])

        logf_res = [("LOGF", kb) for kb in range(NKB)]
        nb8 = NKB * 8
        for half in range(2):
            c0 = half * 264
            c1 = min(nb8, c0 + 264)
            ps, rps = pf()
            S.op("pe", mm(ps[:, 0:c1 - c0], utri, LOGF[:, c0:c1], True, True), ["CF"] + logf_res, [rps])
            copy("dve", CC[:, c0:c1], ps[:, 0:c1 - c0], [rps], [("CC", half)])
            ps2, rps2 = pf()
            S.op("pe", mm(ps2[:, 0:c1 - c0], onesF, LOGF[:, c0:c1], True, True), ["CF"] + logf_res, [rps2])
            copy("dve", PI[:, c0:c1], ps2[:, 0:c1 - c0], [rps2], [("PI", half)])
        for kb in range(1, NKB):
            tt("dve", PI[:, kb * 8:(kb + 1) * 8], PI[:, kb * 8:(kb + 1) * 8], PI[:, (kb - 1) * 8:kb * 8], ALU.add,
               [("PI", 0), ("PI", 1), ("PIx", kb - 1)], [("PIx", kb)])
        for kb in range(1, NKB):
            tt("pool", CC[:, kb * 8:(kb + 1) * 8], CC[:, kb * 8:(kb + 1) * 8], PI[:, (kb - 1) * 8:kb * 8], ALU.add,
               [("CC", 0), ("CC", 1), ("PIx", kb - 1), ("PIx", max(kb - 2, 0))], [("CCx", kb)])
        cc_res = [("CC", 0), ("CC", 1)] + [("CCx", kb) for kb in range(1, NKB)]
        pi_res = [("PI", 0), ("PI", 1)] + [("PIx", kb) for kb in range(1, NKB)]
        for j in range(nslot):
            ts("dve", CREF[:, j * 8:(j + 1) * 8], PI[:, (4 * j + 1) * 8:(4 * j + 2) * 8], selr[:, 0:1], None, ALU.mult, None, pi_res + ["CF"], [("CREF", j)])
            for t in range(1, 4):
                stt("dve", CREF[:, j * 8:(j + 1) * 8], PI[:, (4 * j + 1 + t) * 8:(4 * j + 2 + t) * 8], selr[:, t:t + 1], CREF[:, j * 8:(j + 1) * 8],
                    ALU.mult, ALU.add, pi_res + ["CF", ("CREF", j)], [("CREF", j)])

        for ch in range(nslot // 4 if nslot >= 4 else 1):
            ntile = min(4, nslot)
            n = ntile * 128
            q0 = ch * 512
            hti = ch % 2
            for t in range(ntile):
                load_ln_transpose(xq[q0 + t * 128:q0 + (t + 1) * 128, :], t, hti, t * 128)
            for kc in range(8):
                dma("sp", hqT_d[kc, :, q0:q0 + n], HT[hti][:, kc, :n], ht_res(hti, ntile), [("hqT_d", ch, kc)])
            ci = ch % 2
            dma("sp", CS[ci][:, 0:n], cq_d[:, q0:q0 + n], (), [("CS", ci, 0)])
            dma("sp", CS[ci][:, 512:512 + n], sq_d[:, q0:q0 + n], (), [("CS", ci, 1)])
            csr = [("CS", ci, 0), ("CS", ci, 1)]
            for grp, dst in ((0, qaT_d), (1, qiT_d)):
                for ot in range(4):
                    ps, rps = proj_fm(WQ, grp * 512 + ot * 128, hti, n, "WQ")
                    i = cnt["fm"] % 2
                    cnt["fm"] += 1
                    copy("act", FMB[i][:, :n], ps[:, :n], [rps], [("FMB", i)])
                    tt("dve", RT1[i][:, :n], ps[:, :n], CS[ci][:, 0:n], ALU.mult, [rps] + csr, [("RT1", i)])
                    pp, rpp = pf()
                    S.op("pe", mm(pp[:, :n], ropeP, FMB[i][:, :n], True, True), ["CB", ("FMB", i)], [rpp])
                    tt("dve", RT2[i][:, :n], pp[:, :n], CS[ci][:, 512:512 + n], ALU.mult, [rpp] + csr, [("RT2", i)])
                    o = cnt["ob"] % 3
                    cnt["ob"] += 1
                    tt("pool", OUTB[o][:, :n], RT1[i][:, :n], RT2[i][:, :n], ALU.add, [("RT1", i), ("RT2", i)], [("OUTB", o)])
                    dma("sp", dst[ot, :, q0:q0 + n], OUTB[o][:, :n], [("OUTB", o)], [(id(dst), ot, ch)])
            for ot in range(4):
                ps, rps = proj_fm(WQ, 1024 + ot * 128, hti, n, "WQ")
                plain_store(ps, rps, n, qfT_d[ot, :, q0:q0 + n], ("qfT_d", ot, ch))
            for t in range(ntile):
                sl = ch * 4 + t
                hres = [("HT", hti, half, t * 128) for half in range(2)]
                ps, rps = pf()
                items = [(ps[:, 0:8], HT[hti][:, kc, t * 128:(t + 1) * 128], WQ[:, kc, 1536:1544], kc == 0, kc == 7) for kc in range(8)]
                S.op("pe", mm_chain(items), ["WQ"] + hres, [rps])
                copy("dve", WIQ[:, sl * 8:(sl + 1) * 8], ps[:, 0:8], [rps], [("WIQ", sl)])

        S.barrier()

        A.off = P1_END
        SIDX = A.f32(TP)
        MNEG = A.bf(TP)
        RL = [A.bf(512) for _ in range(8)]
        DW = A.bf(8 * 128)
        QI = A.bf(4 * 128).rearrange("p (t n) -> p t n", t=4)
        QA = A.bf(4 * 128).rearrange("p (t n) -> p t n", t=4)
        QF = A.bf(4 * 128).rearrange("p (t n) -> p t n", t=4)
        KS = [A.bf(4 * 512).rearrange("p (t n) -> p t n", t=4) for _ in range(2)]
        VS = [A.bf(4 * 8 * VW).rearrange("p (b n) -> p b n", b=4) for _ in range(2)]
        PT = [A.bf(1024) for _ in range(2)]
        BIASK = A.f32(NKB * 8)
        BS = A.f32(64)
        HW = A.f32(NBIS)
        OSB = A.f32(8 * VW)
        RCP = A.f32(8)
        YB = A.bf(512)
        YT = A.bf(4 * 128)
        yaT_d = dscr("yaT_d", [4, 128, nq])
        yfT_d = dscr("yfT_d", [4, 128, nq])
        P2_END = A.off

        O_A = (PF[4], ("PF", 4))
        O_B = (PF[5], ("PF", 5))
        sc = {"k": 0}

        def attention(j, nk, kT_dram, v_dram, Qt, rQ, fox, yT_dram, tagname):
            memset("dve", O_A[0][:, :], 0.0, [O_A[1]])
            memset("dve", O_B[0][:, :], 0.0, [O_B[1]])
            nch = (nk + 3) // 4
            for ch in range(nch):
                nb = min(4, nk - ch * 4)
                w = nb * 128
                k0 = ch * 512
                b = sc["k"] % 2
                sc["k"] += 1
                kres = [(kT_dram_name(kT_dram), ot, ch) for ot in range(4)]
                dma("sp", KS[b][:, :, 0:w], kT_dram[:, :, k0:k0 + w].rearrange("t p k -> p t k"),
                    [(tagname + "kT", ot, ch) for ot in range(4)], [("KS", b)])
                dma("sp", VS[b][:, 0:nb, :], v_dram[ch * 4:ch * 4 + nb].rearrange("b p c -> p b c"),
                    [(tagname + "v", kb) for kb in range(ch * 4, ch * 4 + nb)], [("VS", b)])
                for kbl in range(nb):
                    kb = ch * 4 + kbl
                    tail = kb - (nk - 4)
                    pt = sc["k"] % 2
                    for par in range(2):
                        st_, rst = pf(0, 4)
                        items = []
                        if not fox:
                            items.append((st_[:, :], MNEG[:, kb * 128:(kb + 1) * 128], ident4, True, False))
                        elif tail >= 0:
                            items.append((st_[:, :], cmneg[:, tail * 128:(tail + 1) * 128], ident4, True, False))
                        masked = len(items) > 0
                        p0 = par * 64
                        for hh in range(4):
                            items.append((st_[:, hh * 128:(hh + 1) * 128],
                                          KS[b][p0:p0 + 64, hh, kbl * 128:(kbl + 1) * 128],
                                          Qt[p0:p0 + 64, hh, :], not masked, True))
                        reads = [("KS", b), rQ, "CB"]
                        if not fox:
                            reads.append("MNEG")
                        S.op("pe", mm_chain(items), reads, [rst])
                        if fox:
                            for hh in range(4):
                                h = 2 * hh + par
                                sl = par * 4 + hh
                                act(PTcur(kb)[:, sl * 128:(sl + 1) * 128], st_[:, hh * 128:(hh + 1) * 128], AF.Exp,
                                    [rst, "BIASK"], [("PT", kb % 2, par)], bias=BIASK[:, kb * 8 + h:kb * 8 + h + 1], scale=0.125)
                        else:
                            act(PTcur(kb)[:, par * 512:(par + 1) * 512], st_[:, :], AF.Exp, [rst], [("PT", kb % 2, par)], scale=0.125)
                    for hg, (O_, rO) in enumerate((O_A, O_B)):
                        items = []
                        for hh in range(4):
                            h = hg * 4 + hh
                            sl = (h % 2) * 4 + h // 2
                            items.append((O_[:, hh * VW:hh * VW + 65], PTcur(kb)[:, sl * 128:(sl + 1) * 128],
                                          VS[b][:, kbl, h * VW:h * VW + 65], False, False))
                        S.op("pe", mm_chain(items), [("PT", kb % 2, 0), ("PT", kb % 2, 1), ("VS", b)], [rO])
            for hg, (O_, rO) in enumerate((O_A, O_B)):
                copy("dve", OSB[:, hg * 4 * VW:(hg + 1) * 4 * VW], O_[:, 0:4 * VW], [rO], [("OSB", hg)])
            OV = OSB.rearrange("p (h c) -> p h c", h=8)
            S.op("dve", lambda e: e.reciprocal(RCP[:, :], OV[:, :, 64]), [("OSB", 0), ("OSB", 1)], ["RCP"])
            for h in range(8):
                ts("dve", YB[:, h * 64:(h + 1) * 64], OV[:, h, 0:64], RCP[:, h:h + 1], None, ALU.mult, None,
                   [("OSB", 0), ("OSB", 1), "RCP"], [("YB", h)])
            tp_, rtp = pb()
            def fn(e, tp_=tp_):
                ins = None
                for kc in range(4):
                    ins = e.transpose(tp_[:, kc * 128:(kc + 1) * 128], YB[:, kc * 128:(kc + 1) * 128], identB)
                return ins
            S.op("pe", fn, [("YB", h) for h in range(8)] + ["CB"], [rtp])
            copy("act", YT[:, :], tp_[:, 0:512], [rtp], ["YT"])
            dma("sp", yT_dram[:, :, j * 128:(j + 1) * 128].rearrange("t p n -> p t n"), YT.rearrange("p (t n) -> p t n", t=4),
                ["YT"], [(tagname + "yT", j)])

        def kT_dram_name(x):
            return id(x)

        def PTcur(kb):
            return PT[kb % 2]

        for j in range(nslot if stop_after >= 2 else 0):
            nk = 4 * j + 5
            n = nk * 128
            dma("sp", QI, qiT_d[:, :, j * 128:(j + 1) * 128].rearrange("t p n -> p t n"), (), ["QI"])
            dma("sp", QA, qaT_d[:, :, j * 128:(j + 1) * 128].rearrange("t p n -> p t n"), (), ["QA"])
            dma("sp", QF, qfT_d[:, :, j * 128:(j + 1) * 128].rearrange("t p n -> p t n"), (), ["QF"])
            for h in range(8):
                ts("dve", DW[:, h * 128:(h + 1) * 128], identB, WIQ[:, j * 8 + h:j * 8 + h + 1], None, ALU.mult, None,
                   ["CB", ("WIQ", j)], [("DW", h)])
            nch = (nk + 3) // 4
            for ch in range(nch):
                w = min(512, n - ch * 512)
                k0 = ch * 512
                for h in range(8):
                    p0 = (h % 2) * 64
                    z, rz = pf(0, 4)
                    S.op("pe", mm(z[:, :w], QI[p0:p0 + 64, h // 2, :], KIT[p0:p0 + 64, k0:k0 + w], True, True), ["QI", "KITall"], [rz])
                    act(RL[h][:, :w], z[:, :w], AF.Relu, [rz], [("RL", h)])
                acc, racc = pf(4, 6)
                items = [(acc[:, :w], DW[:, h * 128:(h + 1) * 128], RL[h][:, :w], h == 0, h == 7) for h in range(8)]
                S.op("pe", mm_chain(items), [("DW", h) for h in range(8)] + [("RL", h) for h in range(8)], [racc])
                copy("dve", SIDX[:, k0:k0 + w], acc[:, :w], [racc], [("SIDX", ch)])
            sres = [("SIDX", ch) for ch in range(nch)]
            S.op("dve", lambda e, n=n: e.tensor_reduce(BS[:, 0:1], SIDX[:, 0:n], AX.X, ALU.max, True), sres, [("BS", 0)])
            memset("dve", SIDX[:, 0:PAD], -1e30, [("SIDX", 0)])
            tt("dve", SIDX[:, n - 512:n], SIDX[:, n - 512:n], cmadd, ALU.add, sres + ["CF"], sres)
            ts("dve", BS[:, 2:3], BS[:, 0:1], -1.0, None, ALU.mult, None, [("BS", 0)], [("BS", 2)])
            ts("dve", BS[:, 1:2], BS[:, 0:1], 2.0, None, ALU.mult, None, [("BS", 0)], [("BS", 1)])
            ts("dve", HW[:, :], pow2, BS[:, 1:2], None, ALU.mult, None, ["CF", ("BS", 1)], ["HW"])
            for it in range(NBIS):
                tt("dve", BS[:, 3:4], BS[:, 2:3], HW[:, it:it + 1], ALU.add, [("BS", 2), "HW"], [("BS", 3)])
                ts("dve", MNEG[:, 0:n], SIDX[:, 0:n], BS[:, 3:4], 0.0, ALU.is_ge, ALU.add, sres + [("BS", 3)], ["MNEG", ("BS", 4)], accum=BS[:, 4:5])
                stt("dve", BS[:, 5:6], BS[:, 4:5], 255.5, HW[:, it:it + 1], ALU.is_ge, ALU.mult, [("BS", 4), "HW"], [("BS", 5)])
                tt("dve", BS[:, 2:3], BS[:, 2:3], BS[:, 5:6], ALU.add, [("BS", 2), ("BS", 5)], [("BS", 2)])
            ts("dve", MNEG[:, 0:n], SIDX[:, 0:n], BS[:, 2:3], NEGM, ALU.is_lt, ALU.mult, sres + [("BS", 2)], ["MNEG"])
            attention(j, nk, kaT_d, va_d, QA, "QA", False, yaT_d, "a")
            BK = BIASK[:, 0:nk * 8].rearrange("p (k h) -> p k h", h=8)
            tt("dve", BK, CREF[:, j * 8:(j + 1) * 8].unsqueeze(1).to_broadcast([128, nk, 8]),
               CC[:, 0:nk * 8].rearrange("p (k h) -> p k h", h=8), ALU.subtract, [("CREF", j)] + cc_res, ["BIASK"])
            attention(j, nk, kfT_d, vf_d, QF, "QF", True, yfT_d, "f")

        S.barrier()

        A.off = 0
        GATE = A.f32(nslot * 32)
        P3_KEEP = A.off
        WBA = A.bf(4 * 1024).rearrange("p (k n) -> p k n", k=4)
        WBF = A.bf(4 * 1024).rearrange("p (k n) -> p k n", k=4)
        WG = A.bf(8 * 2048).rearrange("p (k n) -> p k n", k=8)
        WO = A.bf(8 * 1024).rearrange("p (k n) -> p k n", k=8)
        WR = A.f32(8 * 36).rearrange("p (k n) -> p k n", k=8)
        H1TS = [A.bf(D).rearrange("p (k n) -> p k n", k=8) for _ in range(2)]
        YAT = [A.bf(512).rearrange("p (t n) -> p t n", t=4) for _ in range(2)]
        YFT = [A.bf(512).rearrange("p (t n) -> p t n", t=4) for _ in range(2)]
        HQ = [A.bf(1024).rearrange("p (k n) -> p k n", k=8) for _ in range(2)]
        XQ = [A.f32(D) for _ in range(2)]
        H32 = [A.f32(D) for _ in range(2)]
        GA = [A.f32(D) for _ in range(2)]
        GF = [A.f32(D) for _ in range(2)]
        MGB = [A.bf(D) for _ in range(2)]
        MGT = [A.bf(D).rearrange("p (k n) -> p k n", k=8) for _ in range(2)]
        H1P = [A.f32(D) for _ in range(2)]
        H1 = [A.f32(D) for _ in range(2)]
        H1B = [A.bf(D) for _ in range(2)]
        H1T32 = [A.f32(D).rearrange("p (k n) -> p k n", k=8) for _ in range(2)]
        RS = [A.f32(256) for _ in range(2)]

        dma("pool", WBA, wba_d.rearrange("(k p) n -> p k n", p=128), (), ["WBA"])
        dma("pool", WBF, wbf_d.rearrange("(k p) n -> p k n", p=128), (), ["WBF"])
        dma("pool", WG, wg_d.rearrange("(k p) n -> p k n", p=128), (), ["WG"])
        dma("pool", WO, wo_d.rearrange("(k p) n -> p k n", p=128), (), ["WO"])
        dma("sp", WR, wr_d.rearrange("(k p) n -> p k n", p=128), (), ["WR"])

        for i in range(nslot if stop_after >= 3 else 0):
            b = i % 2
            dma("sp", YAT[b], yaT_d[:, :, i * 128:(i + 1) * 128].rearrange("t p n -> p t n"), [("ayT", i)], [("YAT", b)])
            dma("sp", YFT[b], yfT_d[:, :, i * 128:(i + 1) * 128].rearrange("t p n -> p t n"), [("fyT", i)], [("YFT", b)])
            dma("sp", HQ[b], hqT_d[:, :, i * 128:(i + 1) * 128].rearrange("k p n -> p k n"),
                [("hqT_d", i // 4, kc) for kc in range(8)], [("HQ", b)])
            dma("sp", XQ[b][:, :], xq[i * 128:(i + 1) * 128, :], (), [("XQ", b)])
            layer_norm_tile(XQ[b], 0, 1, H32[b][:, :], "emb", ("XQ", b), [("H32", b)])
            for half in range(2):
                c0 = half * 512
                for (Wcol, GT, nm) in ((0, GA, "GA"), (1024, GF, "GF")):
                    ps, rps = pf()
                    items = [(ps[:, :], HQ[b][:, kc, :], WG[:, kc, Wcol + c0:Wcol + c0 + 512], kc == 0, kc == 7) for kc in range(8)]
                    S.op("pe", mm_chain(items), ["WG", ("HQ", b)], [rps])
                    act(GT[b][:, c0:c0 + 512], ps[:, :], AF.Sigmoid, [rps], [(nm, b, half)])
                ps, rps = pf()
                items = [(ps[:, :], YAT[b][:, kc, :], WBA[:, kc, c0:c0 + 512], kc == 0, kc == 3) for kc in range(4)]
                S.op("pe", mm_chain(items), ["WBA", ("YAT", b)], [rps])
                tt("dve", GA[b][:, c0:c0 + 512], ps[:, :], GA[b][:, c0:c0 + 512], ALU.mult, [rps, ("GA", b, half)], [("GA", b, half)])
                ps, rps = pf()
                items = [(ps[:, :], YFT[b][:, kc, :], WBF[:, kc, c0:c0 + 512], kc == 0, kc == 3) for kc in range(4)]
                S.op("pe", mm_chain(items), ["WBF", ("YFT", b)], [rps])
                tt("dve", GF[b][:, c0:c0 + 512], ps[:, :], GF[b][:, c0:c0 + 512], ALU.mult, [rps, ("GF", b, half)], [("GF", b, half)])
                tt("pool", MGB[b][:, c0:c0 + 512], GA[b][:, c0:c0 + 512], GF[b][:, c0:c0 + 512], ALU.add,
                   [("GA", b, half), ("GF", b, half)], [("MGB", b, half)])
            for half in range(2):
                tp_, rtp = pb()
                def fn(e, tp_=tp_, b=b, half=half):
                    ins = None
                    for kc in range(4):
                        k = half * 4 + kc
                        ins = e.transpose(tp_[:, kc * 128:(kc + 1) * 128], MGB[b][:, k * 128:(k + 1) * 128], identB)
                    return ins
                S.op("pe", fn, [("MGB", b, 0), ("MGB", b, 1), "CB"], [rtp])
                copy("act", MGT[b][:, half * 4:half * 4 + 4, :], tp_[:, 0:512].rearrange("p (k n) -> p k n", k=4), [rtp], [("MGT", b, half)])
            for half in range(2):
                c0 = half * 512
                ps, rps = pf()
                items = [(ps[:, :], MGT[b][:, kc, :], WO[:, kc, c0:c0 + 512], kc == 0, kc == 7) for kc in range(8)]
                S.op("pe", mm_chain(items), ["WO", ("MGT", b, 0), ("MGT", b, 1)], [rps])
                stt("dve", H1P[b][:, c0:c0 + 512], H32[b][:, c0:c0 + 512], ALPHA, ps[:, :], ALU.mult, ALU.add, [("H32", b), rps], [("H1P", b, half)])
            layer_norm_tile(H1P[b], 2, 3, H1[b][:, :], "ln1", [("H1P", b, 0), ("H1P", b, 1)], [("H1", b)])
            dma("sp", h1_d[i * 128:(i + 1) * 128, :], H1[b][:, :], [("H1", b)], [("h1_d", i)])
            copy("act", H1B[b][:, :], H1[b][:, :], [("H1", b)], [("H1B", b)])
            for half in range(2):
                tp_, rtp = pb()
                def fn(e, tp_=tp_, b=b, half=half):
                    ins = None
                    for kc in range(4):
                        k = half * 4 + kc
                        ins = e.transpose(tp_[:, kc * 128:(kc + 1) * 128], H1B[b][:, k * 128:(k + 1) * 128], identB)
                    return ins
                S.op("pe", fn, [("H1B", b), "CB"], [rtp])
                copy("act", H1TS[b][:, half * 4:half * 4 + 4, :], tp_[:, 0:512].rearrange("p (k n) -> p k n", k=4), [rtp], [("H1TS", b, half)])
            dma("sp", h1T_d[:, :, i * 128:(i + 1) * 128].rearrange("k p n -> p k n"), H1TS[b], [("H1TS", b, 0), ("H1TS", b, 1)], [("h1T_d", i)])
            for half in range(2):
                ps, rps = pf()
                def fn(e, ps=ps, b=b, half=half):
                    ins = None
                    for kc in range(4):
                        k = half * 4 + kc
                        ins = e.transpose(ps[:, kc * 128:(kc + 1) * 128], H1[b][:, k * 128:(k + 1) * 128], identF)
                    return ins
                S.op("pe", fn, [("H1", b), "CF"], [rps])
                copy("dve", H1T32[b][:, half * 4:half * 4 + 4, :], ps[:, :].rearrange("p (k n) -> p k n", k=4), [rps], [("H1T32", b, half)])
            ps, rps = pf()
            items = [(ps[:, 0:36], H1T32[b][:, kc, :], WR[:, kc, :], kc == 0, kc == 7) for kc in range(8)]
            S.op("pe", mm_chain(items), ["WR", ("H1T32", b, 0), ("H1T32", b, 1)], [rps])
            R = RS[b]
            rR = ("RS", b)
            k_ = [0]

            def rr_(nm):
                return ("RS", b, nm)
            tt("dve", R[:, 0:36], ps[:, 0:36], BRT[:, :], ALU.add, [rps, "BRT"], [rr_("lg")])
            S.op("dve", lambda e, R=R: e.tensor_reduce(R[:, 40:41], R[:, 0:4], AX.X, ALU.max), [rr_("lg")], [rr_("gmax")])
            ts("dve", R[:, 44:48], R[:, 0:4], R[:, 40:41], None, ALU.is_ge, None, [rr_("lg"), rr_("gmax")], [rr_("goh")])
            ts("dve", R[:, 41:42], R[:, 40:41], -1.0, None, ALU.mult, None, [rr_("gmax")], [rr_("ngmax")])
            act(R[:, 48:52], R[:, 0:4], AF.Exp, [rr_("lg"), rr_("ngmax")], [rr_("gexp"), rr_("gsum")], bias=R[:, 41:42], scale=1.0, accum=R[:, 42:43])
            S.op("dve", lambda e, R=R: e.reciprocal(R[:, 43:44], R[:, 42:43]), [rr_("gsum")], [rr_("pg")])
            ts("dve", R[:, 52:56], R[:, 44:48], -1.0, 1e30, ALU.add, ALU.mult, [rr_("goh")], [rr_("gpen")])
            tt("dve", R[:, 64:96].rearrange("p (g e) -> p g e", g=4), R[:, 4:36].rearrange("p (g e) -> p g e", g=4),
               R[:, 52:56].unsqueeze(2).to_broadcast([128, 4, 8]), ALU.add, [rr_("lg"), rr_("gpen")], [rr_("em")])
            S.op("dve", lambda e, R=R: e.tensor_reduce(R[:, 56:57], R[:, 64:96], AX.X, ALU.max), [rr_("em")], [rr_("m1")])
            ts("dve", R[:, 96:128], R[:, 64:96], R[:, 56:57], None, ALU.is_ge, None, [rr_("em"), rr_("m1")], [rr_("oh1")])
            stt("dve", R[:, 128:160], R[:, 96:128], -1e30, R[:, 64:96], ALU.mult, ALU.add, [rr_("oh1"), rr_("em")], [rr_("em2")])
            S.op("dve", lambda e, R=R: e.tensor_reduce(R[:, 57:58], R[:, 128:160], AX.X, ALU.max), [rr_("em2")], [rr_("m2")])
            ts("dve", R[:, 160:192], R[:, 128:160], R[:, 57:58], None, ALU.is_ge, None, [rr_("em2"), rr_("m2")], [rr_("oh2")])
            tt("dve", R[:, 58:59], R[:, 57:58], R[:, 56:57], ALU.subtract, [rr_("m1"), rr_("m2")], [rr_("dm")])
            act(R[:, 59:60], R[:, 58:59], AF.Exp, [rr_("dm")], [rr_("edm")])
            ts("dve", R[:, 60:61], R[:, 59:60], 1.0, None, ALU.add, None, [rr_("edm")], [rr_("den")])
            S.op("dve", lambda e, R=R: e.reciprocal(R[:, 61:62], R[:, 60:61]), [rr_("den")], [rr_("w1")])
            tt("dve", R[:, 62:63], R[:, 61:62], R[:, 43:44], ALU.mult, [rr_("w1"), rr_("pg")], [rr_("g1")])
            tt("dve", R[:, 63:64], R[:, 43:44], R[:, 62:63], ALU.subtract, [rr_("pg"), rr_("g1")], [rr_("g2")])
            ts("dve", R[:, 192:224], R[:, 96:128], R[:, 62:63], None, ALU.mult, None, [rr_("oh1"), rr_("g1")], [rr_("t1")])
            stt("dve", GATE[:, i * 32:(i + 1) * 32], R[:, 160:192], R[:, 63:64], R[:, 192:224], ALU.mult, ALU.add,
                [rr_("oh2"), rr_("g2"), rr_("t1")], [("GATE", i)])

        S.barrier()

        A.off = P3_KEEP
        H1T = A.bf(8 * nq).rearrange("p (k n) -> p k n", k=8)
        ACC = A.f32(nslot * D)
        dma("sp", H1T, h1T_d.rearrange("k p n -> p k n"), [("h1T_d", i) for i in range(nslot)], ["H1Tall"])
        WGT = [A.bf(8 * 256).rearrange("p (k n) -> p k n", k=8) for _ in range(2)]
        WUP = [A.bf(8 * 256).rearrange("p (k n) -> p k n", k=8) for _ in range(2)]
        WDN = [A.bf(2 * 1024).rearrange("p (k n) -> p k n", k=2) for _ in range(2)]
        SG = [A.f32(512) for _ in range(2)]
        AT = [A.bf(2 * 512).rearrange("p (f n) -> p f n", f=2) for _ in range(2)]
        FX = [A.f32(D) for _ in range(2)]
        FO = [A.f32(D) for _ in range(2)]

        memset("pool", ACC[:, :], 0.0, ["ACCall"])
        nchq = max(1, nq // 512)
        ac = {"n": 0}
        for ex in range(nexp if stop_after >= 4 else 0):
            b = ex % 2
            dma("pool", WGT[b], wgate_d[ex].rearrange("(k p) n -> p k n", p=128), (), [("WGT", b)])
            dma("pool", WUP[b], wup_d[ex].rearrange("(k p) n -> p k n", p=128), (), [("WUP", b)])
            dma("pool", WDN[b], wdn_d[ex].rearrange("(k p) n -> p k n", p=128), (), [("WDN", b)])
            for ch in range(nchq):
                n = min(512, nq)
                q0 = ch * 512
                a = ac["n"] % 2
                ac["n"] += 1
                hres = ["H1Tall"]
                for ft in range(2):
                    pg_, rpg = pf()
                    items = [(pg_[:, :n], WGT[b][:, kc, ft * 128:(ft + 1) * 128], H1T[:, kc, q0:q0 + n], kc == 0, kc == 7) for kc in range(8)]
                    S.op("pe", mm_chain(items), [("WGT", b)] + hres, [rpg])
                    pu_, rpu = pf()
                    items = [(pu_[:, :n], WUP[b][:, kc, ft * 128:(ft + 1) * 128], H1T[:, kc, q0:q0 + n], kc == 0, kc == 7) for kc in range(8)]
                    S.op("pe", mm_chain(items), [("WUP", b)] + hres, [rpu])
                    s = (ac["n"] + ft) % 2
                    act(SG[s][:, :n], pg_[:, :n], AF.Silu, [rpg], [("SG", s)])
                    tt("dve", AT[a][:, ft, :n], SG[s][:, :n], pu_[:, :n], ALU.mult, [("SG", s), rpu], [("AT", a, ft)])
                for t in range(n // 128):
                    i = ch * 4 + t
                    for half in range(2):
                        c0 = half * 512
                        py, rpy = pf()
                        items = [(py[:, :], AT[a][:, ft, t * 128:(t + 1) * 128], WDN[b][:, ft, c0:c0 + 512], ft == 0, ft == 1) for ft in range(2)]
                        S.op("pe", mm_chain(items), [("WDN", b), ("AT", a, 0), ("AT", a, 1)], [rpy])
                        stt("dve", ACC[:, i * D + c0:i * D + c0 + 512], py[:, :], GATE[:, i * 32 + ex:i * 32 + ex + 1],
                            ACC[:, i * D + c0:i * D + c0 + 512], ALU.mult, ALU.add,
                            [rpy, ("GATE", i), "ACCall", ("ACC", i, half)], [("ACC", i, half)])
        for i in range(nslot):
            b = i % 2
            dma("sp", FX[b][:, :], h1_d[i * 128:(i + 1) * 128, :], [("h1_d", i)], [("FX", b)])
            stt("dve", FX[b][:, :], FX[b][:, :], ALPHA, ACC[:, i * D:(i + 1) * D], ALU.mult, ALU.add,
                [("FX", b), ("ACC", i, 0), ("ACC", i, 1), "ACCall"], [("FX", b)])
            layer_norm_tile(FX[b], 4, 5, FO[b][:, :], "ln2", ("FX", b), [("FO", b)])
            dma("sp", out_d[i * 128:(i + 1) * 128, :], FO[b][:, :], [("FO", b)], [("out", i)])
        S.finish()
        S.prepare()

        with nc.Block() as block:
            @block.tensor
            def _(e):
                S.replay("pe", e, sems)

            @block.scalar
            def _(e):
                S.replay("act", e, sems)

            @block.vector
            def _(e):
                S.replay("dve", e, sems)

            @block.gpsimd
            def _(e):
                S.replay("pool", e, sems)

            @block.sync
            def _(e):
                S.replay("sp", e, sems)
    return nc


def _consts(r):
    bf = ml_dtypes.bfloat16
    ident = np.eye(128, dtype=np.float32)
    k = np.arange(128)
    utri = (k[:, None] <= k[None, :]).astype(np.float32)
    ones = np.ones((128, 128), np.float32)
    q = np.arange(128)[:, None]
    kk = np.arange(128)[None, :]
    tri_ok = kk <= q
    cm = np.zeros((128, 4, 128), bool)
    for t in range(4):
        if t < r:
            cm[:, t, :] = True
        elif t == r:
            cm[:, t, :] = tri_ok
    cmadd = np.where(cm, 0.0, -1e30).astype(np.float32).reshape(128, 512)
    cmneg = np.where(cm, 0.0, NEGM).astype(np.float32).reshape(128, 512)
    selr = np.zeros((128, 4), np.float32)
    selr[:, r] = 1.0
    pow2 = np.tile((2.0 ** -(np.arange(NBIS) + 1.0)).astype(np.float32)[None], (128, 1))
    padmask = (np.arange(128) >= PAD).astype(np.float32)[:, None]
    cf = np.concatenate([ident, utri, ones, cmadd, selr, pow2, padmask], axis=1).astype(np.float32)
    P = np.zeros((128, 128), np.float32)
    for hb in (0, 64):
        for d in range(8):
            P[hb + d + 8, hb + d] = -1.0
            P[hb + d, hb + d + 8] = 1.0
    cb = np.concatenate([ident, np.tile(ident, (1, 4)), P, cmneg], axis=1).astype(bf)
    return cf, cb


def _rope_tables(pos):
    half = 8
    inv = (500000.0 ** (-np.arange(half, dtype=np.float32) / half)).astype(np.float32)
    ang = pos.astype(np.float32)[None, :] * inv[:, None]
    c8, s8 = np.cos(ang).astype(np.float32), np.sin(ang).astype(np.float32)
    n = pos.shape[0]
    C = np.ones((128, n), np.float32)
    Sn = np.zeros((128, n), np.float32)
    for hb in (0, 64):
        C[hb:hb + 8] = c8
        C[hb + 8:hb + 16] = c8
        Sn[hb:hb + 8] = s8
        Sn[hb + 8:hb + 16] = s8
    return C, Sn


_NC_CACHE = {}


def kernel(x, meta_tokens, emb_ln_g, emb_ln_b, w_in, b_forget, kv_norm_g, w_kv_up, w_branch_dsa,
           w_branch_fox, w_out, ln1_g, ln1_b, w_router_group, b_router_group, w_router_expert,
           b_router_expert, w_gate, w_up, w_down, ln2_g, ln2_b):
    f = lambda a: np.ascontiguousarray(np.asarray(a, dtype=np.float32))
    x = f(x)
    w_in0 = f(w_in)[0]
    offs = np.cumsum([0, 512, 256, 512, 64, 8, 512, 512, 512, 8, 1024, 1024])
    seg = lambda i: w_in0[:, offs[i]:offs[i + 1]]
    q_a, c_kv, q_i, k_i, w_i, q_f, k_f, v_f, f_lg, g_a, g_f = [seg(i) for i in range(11)]
    wk = f(np.concatenate([c_kv, k_i, k_i, k_f, v_f, f_lg], axis=1))
    wq = f(np.concatenate([q_a, q_i, q_f, w_i], axis=1))
    wg = f(np.concatenate([g_a, g_f], axis=1))
    rep = lambda v: f(np.tile(np.asarray(v, np.float32).reshape(1, -1), (128, 1)))
    lnp = f(np.concatenate([rep(emb_ln_g), rep(emb_ln_b), rep(ln1_g[0]), rep(ln1_b[0]), rep(ln2_g[0]), rep(ln2_b[0])], axis=1))
    wr = f(np.concatenate([f(w_router_group)[0], f(w_router_expert)[0]], axis=1))
    brt = rep(np.concatenate([f(b_router_group)[0], f(b_router_expert)[0]]))
    common = dict(
        wk=wk, wq=wq, wg=wg, wkv=f(w_kv_up)[0], wba=f(w_branch_dsa)[0], wbf=f(w_branch_fox)[0], wo=f(w_out)[0],
        wr=wr, wgate=f(w_gate)[0][:NEXP_RUN], wup=f(w_up)[0][:NEXP_RUN], wdn=f(w_down)[0][:NEXP_RUN], lnp=lnp, kvg=rep(kv_norm_g[0]),
        bfg=rep(b_forget[0]), brt=brt,
    )
    kpos = np.arange(TP) - PAD
    ck, sk = _rope_tables(kpos)
    in_maps = []
    own = []
    for c in range(8):
        b, r = c // 4, c % 4
        xkc = np.zeros((TP, D), np.float32)
        xkc[PAD:PAD + NMETA] = f(meta_tokens)
        xkc[PAD + NMETA:] = x[b]
        blocks = [4 * j + r for j in range(NSLOT)]
        rows = np.concatenate([np.arange(bl * 128, (bl + 1) * 128) for bl in blocks])
        own.append((b, rows))
        cq, sq = _rope_tables(rows + NMETA)
        cf, cb = _consts(r)
        m = dict(common)
        m.update(xk=xkc, xq=f(x[b][rows]), ck=ck, sk=sk, cq=cq, sq=sq, cf32=cf, cbf=cb)
        in_maps.append(m)
    if "nc" not in _NC_CACHE:
        _NC_CACHE["nc"] = build()
    nc = _NC_CACHE["nc"]
    res = run_bass_kernel_spmd(nc, in_maps, core_ids=list(range(8)))
    out = np.zeros((2, SEQ, D), np.float32)
    for c in range(8):
        b, rows = own[c]
        out[b, rows] = np.asarray(res.results[c]["out"], dtype=np.float32)
    return out
```

```python
import os
import numpy as np
import ml_dtypes
from contextlib import ExitStack
import concourse.bass as bass
import concourse.mybir as mybir
from concourse.bass_utils import run_bass_kernel_spmd

F32 = mybir.dt.float32
BF16 = mybir.dt.bfloat16
AF = mybir.ActivationFunctionType
ALU = mybir.AluOpType
AX = mybir.AxisListType

D = 1024
SEQ = 8192
NMETA = 16
PAD = 112
TP = SEQ + NMETA + PAD
NKB = TP // 128
NSLOT = 16
NQ = NSLOT * 128
NBIS = 24
ALPHA = 2.0 ** 0.25
NEGM = -30000.0
NEXP = 32
VW = 68
NEXP_RUN = 32

WK_COLS = 256 + 128 + 512 + 512 + 8
WQ_COLS = 512 * 3 + 8


class Sched:
    ENG = ("pe", "act", "dve", "pool", "sp")
    NDMA = 8

    def __init__(self):
        self.ops = {e: [] for e in self.ENG}
        self.count = {e: 0 for e in self.ENG}
        self.seen = {e: {} for e in self.ENG}
        self.res = {}
        self.dma_rr = {e: 0 for e in self.ENG}
        self.dma_val = {}

    def op(self, eng, fn, reads=(), writes=(), dma=False):
        self.nops = getattr(self, "nops", 0) + 1
        if self.nops > getattr(self, "maxops", 10 ** 9):
            return None
        need = {}

        def add(tok):
            if tok is None:
                return
            k, v = tok
            if need.get(k, 0) < v:
                need[k] = v

        reads = list(reads)
        writes = list(writes)
        for r in list(reads):
            if isinstance(r, tuple) and r and r[0] in ("PF", "PB") and r not in writes:
                writes.append(r)
        for r in reads:
            st = self.res.get(r)
            if st:
                add(st["w"])
        for w in writes:
            st = self.res.get(w)
            if st:
                add(st["w"])
                for k, v in st["r"].items():
                    add((k, v))
        waits = []
        for k, v in need.items():
            if eng == "pe" and k == "pe":
                continue
            if self.seen[eng].get(k, 0) >= v:
                continue
            waits.append((k, v))
            self.seen[eng][k] = v
        if dma:
            idx = self.dma_rr[eng]
            self.dma_rr[eng] = (idx + 1) % self.NDMA
            key = ("dma", eng, idx)
            prev = self.dma_val.get(key, 0)
            if prev > 0 and self.seen[eng].get(key, 0) < prev:
                waits.append((key, prev))
                self.seen[eng][key] = prev
            val = prev + 16
            self.dma_val[key] = val
            tok = (key, val)
        else:
            self.count[eng] += 1
            tok = (eng, self.count[eng])
        self.ops[eng].append((waits, fn, tok))
        for r in reads:
            st = self.res.get(r)
            if not st:
                st = {"w": None, "r": {}}
                self.res[r] = st
            if st["r"].get(tok[0], 0) < tok[1]:
                st["r"][tok[0]] = tok[1]
        for w in writes:
            self.res[w] = {"w": tok, "r": {}}
        return tok

    def barrier(self):
        toks = [(e, self.count[e]) for e in self.ENG if self.count[e] > 0]
        toks += [(k, v) for k, v in self.dma_val.items()]
        for e in self.ENG:
            waits = []
            for k, v in toks:
                if k == e and e == "pe":
                    continue
                if self.seen[e].get(k, 0) >= v:
                    continue
                waits.append((k, v))
                self.seen[e][k] = v
            if waits:
                self.ops[e].append((waits, None, None))

    def finish(self):
        waits = [(k, v) for k, v in self.dma_val.items() if self.seen["sp"].get(k, 0) < v]
        self.ops["sp"].append((waits, None, None))

    def prepare(self):
        import bisect as _b
        sig = {e: set() for e in self.ENG}
        for e in self.ENG:
            for waits, fn, tok in self.ops[e]:
                for k, v in waits:
                    if k in sig and k != e:
                        sig[k].add(v)
        self.sig = {e: sorted(v) for e, v in sig.items()}
        self._b = _b

    def rank(self, k, v):
        lst = self.sig[k]
        i = self._b.bisect_right(lst, v)
        assert i > 0 and lst[i - 1] == v, (k, v)
        return i

    def replay(self, name, e, sems):
        sigset = set(self.sig[name])
        for waits, fn, tok in self.ops[name]:
            for k, v in waits:
                if k == name:
                    e.drain()
                elif k in self.sig:
                    e.wait_ge(sems[k], self.rank(k, v))
                else:
                    e.wait_ge(sems[k], v)
            if fn is None:
                continue
            ins = fn(e)
            if tok[0][0] == "dma":
                ins.then_inc(sems[tok[0]], 16)
            elif tok[1] in sigset:
                ins.then_inc(sems[tok[0]], 1)


def build(nslot=NSLOT, nexp=NEXP, debug=False, nkb=NKB, stop_after=9):
    nc = bass.Bass("TRN2", target_bir_lowering=False)
    S = Sched()
    import os
    S.maxops = int(os.environ.get("MAXOPS", 10 ** 9))
    nq = nslot * 128

    def din(name, shape, dt=F32):
        return nc.dram_tensor(name, list(shape), dt, kind="ExternalInput").ap()

    def dscr(name, shape, dt=BF16):
        return nc.dram_tensor(name, list(shape), dt, kind="Internal").ap()

    xk = din("xk", [TP, D])
    xq = din("xq", [nq, D])
    wk_d = din("wk", [D, WK_COLS])
    wq_d = din("wq", [D, WQ_COLS])
    wg_d = din("wg", [D, 2048])
    wkv_d = din("wkv", [256, 1024])
    wba_d = din("wba", [512, 1024])
    wbf_d = din("wbf", [512, 1024])
    wo_d = din("wo", [D, D])
    wr_d = din("wr", [D, 36])
    wgate_d = din("wgate", [nexp, D, 256])
    wup_d = din("wup", [nexp, D, 256])
    wdn_d = din("wdn", [nexp, 256, D])
    lnp_d = din("lnp", [128, 6 * D])
    kvg_d = din("kvg", [128, 256])
    bfg_d = din("bfg", [128, 8])
    brt_d = din("brt", [128, 36])
    ck_d = din("ck", [128, TP])
    sk_d = din("sk", [128, TP])
    cq_d = din("cq", [128, nq])
    sq_d = din("sq", [128, nq])
    cf32_d = din("cf32", [128, 128 * 3 + 512 + 4 + NBIS + 1])
    cbf_d = din("cbf", [128, 128 + 512 + 128 + 512], BF16)
    out_d = nc.dram_tensor("out", [nq, D], F32, kind="ExternalOutput").ap()

    kaT_d = dscr("kaT_d", [4, 128, TP])
    kfT_d = dscr("kfT_d", [4, 128, TP])
    va_d = dscr("va_d", [NKB, 128, 8 * VW])
    vf_d = dscr("vf_d", [NKB, 128, 8 * VW])
    qaT_d = dscr("qaT_d", [4, 128, nq])
    qiT_d = dscr("qiT_d", [4, 128, nq])
    qfT_d = dscr("qfT_d", [4, 128, nq])
    hqT_d = dscr("hqT_d", [8, 128, nq])
    h1_d = dscr("h1_d", [nq, D], F32)
    h1T_d = dscr("h1T_d", [8, 128, nq])

    es = ExitStack()
    with es:
        def sb(name, cols, dt=F32, parts=128):
            return es.enter_context(nc.sbuf_tensor(name, [parts, cols], dt))

        CF = sb("CF", 128 * 3 + 512 + 4 + NBIS + 1)
        CB = sb("CB", 128 + 512 + 128 + 512, BF16)
        LNP = sb("LNP", 6 * D)
        KVG = sb("KVG", 256)
        BFG = sb("BFG", 8)
        BRT = sb("BRT", 36)
        identF = CF[:, 0:128]
        utri = CF[:, 128:256]
        onesF = CF[:, 256:384]
        cmadd = CF[:, 384:896]
        selr = CF[:, 896:900]
        pow2 = CF[:, 900:900 + NBIS]
        padmask = CF[:, 900 + NBIS:901 + NBIS]
        identB = CB[:, 0:128]
        ident4 = CB[:, 128:640]
        ropeP = CB[:, 640:768]
        cmneg = CB[:, 768:1280]

        ARENA_COLS = 38 * 1024
        ARENA = sb("ARENA", ARENA_COLS)
        ARENA_B = ARENA[:, :].bitcast(BF16)

        class Bump:
            def __init__(self):
                self.off = 0

            def f32(self, cols):
                o = self.off // 4
                self.off += cols * 4
                assert self.off <= ARENA_COLS * 4, self.off
                return ARENA[:, o:o + cols]

            def bf(self, cols):
                cols2 = (cols + 1) // 2 * 2
                o = self.off // 2
                self.off += cols2 * 2
                assert self.off <= ARENA_COLS * 4, self.off
                return ARENA_B[:, o:o + cols]

        PF = [es.enter_context(nc.psum_tensor(f"PF{i}", [128, 512], F32)) for i in range(6)]
        PB = [es.enter_context(nc.psum_tensor(f"PB{i}", [128, 1024], BF16)) for i in range(2)]
        rr = {"n": 0, "b": 0}

        def pf(lo=0, hi=6):
            i = lo + rr["n"] % (hi - lo)
            rr["n"] += 1
            return PF[i], ("PF", i)

        def pb():
            i = rr["b"] % 2
            rr["b"] += 1
            return PB[i], ("PB", i)

        sems = {}
        for e in Sched.ENG:
            sems[e] = es.enter_context(nc.semaphore(f"s_{e}"))
        for e in ("sp", "pool", "act"):
            for i in range(Sched.NDMA):
                sems[("dma", e, i)] = es.enter_context(nc.semaphore(f"d_{e}{i}"))

        def dma(q, out, in_, reads, writes):
            S.op(q, lambda e, o=out, i=in_: e.dma_start(out=o, in_=i), reads, writes, dma=True)

        def mm(out, lhsT, rhs, start, stop, skip=True):
            return lambda e: e.matmul(out, lhsT, rhs, start=start, stop=stop, skip_group_check=skip)

        def mm_chain(items):
            def fn(e):
                ins = None
                for (o, l, r, st, sp_) in items:
                    ins = e.matmul(o, l, r, start=st, stop=sp_, skip_group_check=True)
                return ins
            return fn

        def act(out, in_, func, reads, writes, bias=0.0, scale=1.0, accum=None):
            if accum is None:
                S.op("act", lambda e: e.activation(out=out, in_=in_, func=func, bias=bias, scale=scale), reads, writes)
            else:
                S.op("act", lambda e: e.activation(out=out, in_=in_, func=func, bias=bias, scale=scale, accum_out=accum), reads, writes)

        def tt(eng, out, in0, in1, op, reads, writes):
            S.op(eng, lambda e: e.tensor_tensor(out=out, in0=in0, in1=in1, op=op), reads, writes)

        def ts(eng, out, in0, s1, s2, op0, op1, reads, writes, accum=None):
            if accum is None:
                if op1 is None:
                    S.op(eng, lambda e: e.tensor_scalar(out, in0, s1, None, op0), reads, writes)
                else:
                    S.op(eng, lambda e: e.tensor_scalar(out, in0, s1, s2, op0, op1), reads, writes)
            else:
                S.op(eng, lambda e: e.tensor_scalar(out, in0, s1, s2, op0, op1, accum), reads, writes)

        def stt(eng, out, in0, scalar, in1, op0, op1, reads, writes):
            S.op(eng, lambda e: e.scalar_tensor_tensor(out=out, in0=in0, scalar=scalar, in1=in1, op0=op0, op1=op1), reads, writes)

        def copy(eng, out, in_, reads, writes):
            if eng == "act":
                S.op("act", lambda e: e.copy(out, in_), reads, writes)
            else:
                S.op(eng, lambda e: e.tensor_copy(out, in_), reads, writes)

        def memset(eng, ap, val, writes):
            S.op(eng, lambda e: e.memset(ap, val), (), writes)

        def transpose_to(out_ps, in_sb, ident):
            return lambda e: e.transpose(out_ps, in_sb, ident)

        dma("sp", CF[:, :], cf32_d, (), ["CF"])
        dma("sp", CB[:, :], cbf_d, (), ["CB"])
        dma("sp", LNP[:, :], lnp_d, (), ["LNP"])
        dma("sp", KVG[:, :], kvg_d, (), ["KVG"])
        dma("sp", BFG[:, :], bfg_d, (), ["BFG"])
        dma("sp", BRT[:, :], brt_d, (), ["BRT"])
        CONST = ["CF", "CB", "LNP", "KVG", "BFG", "BRT"]

        uid = {"n": 0}

        def layer_norm_tile(x32, gcol, bcol, out_ap, tag, rx, wout, eps=1e-5, prescale=None):
            uid["n"] += 1
            u = uid["n"] % 2
            st = LNS[u]
            rs = ("LNS", u)
            rx = rx if isinstance(rx, list) else [rx]
            S.op("dve", lambda e: e.bn_stats(st[:, 0:6], x32[:, 0:512]), rx, [rs])
            S.op("dve", lambda e: e.bn_stats(st[:, 6:12], x32[:, 512:1024]), rx, [(rs, 1)])
            S.op("dve", lambda e: e.bn_aggr(st[:, 12:14], st[:, 0:12]), [rs, (rs, 1)], [(rs, 2)])
            ts("dve", st[:, 11:12], st[:, 13:14], eps, None, ALU.add, None, [(rs, 2), (rs, 1)], [(rs, 5)])
            act(st[:, 10:11], st[:, 11:12], AF.Ln, [(rs, 5)], [(rs, 6)])
            act(st[:, 14:15], st[:, 10:11], AF.Exp, [(rs, 6)], [(rs, 3)], scale=-0.5)
            stt("dve", st[:, 15:16], st[:, 12:13], -1.0, st[:, 14:15], ALU.mult, ALU.mult, [(rs, 2), (rs, 3)], [(rs, 4)])
            xn = LNX[u]
            rxn = ("LNX", u)
            act(xn[:, :], x32, AF.Identity, rx + [(rs, 3), (rs, 4)], [rxn], bias=st[:, 15:16], scale=st[:, 14:15])
            tt("dve", xn[:, :], xn[:, :], LNP[:, gcol * D:(gcol + 1) * D], ALU.mult, [rxn, "LNP"], [rxn])
            tt("pool", out_ap, xn[:, :], LNP[:, bcol * D:(bcol + 1) * D], ALU.add, [rxn, "LNP"], wout)

        LNS = [sb(f"LNS{i}", 16) for i in range(2)]
        LNX = [sb(f"LNX{i}", D) for i in range(2)]

        A = Bump()
        KIT = A.bf(TP)
        LOGF = A.f32(NKB * 8)
        WIQ = A.f32(nslot * 8)
        CC = A.f32(NKB * 8)
        PI = A.f32(NKB * 8)
        CREF = A.f32(nslot * 8)
        P1_END = A.off
        WKs = A.bf(8 * WK_COLS)
        WK = WKs.rearrange("p (k n) -> p k n", k=8)
        WQs = A.bf(8 * WQ_COLS)
        WQ = WQs.rearrange("p (k n) -> p k n", k=8)
        WKVs = A.bf(2 * 1024)
        WKV = WKVs.rearrange("p (k n) -> p k n", k=2)
        XT = [A.f32(D) for _ in range(2)]
        HB = [A.bf(D) for _ in range(2)]
        HT = [A.bf(8 * 512).rearrange("p (k n) -> p k n", k=8) for _ in range(2)]
        FMB = [A.bf(512) for _ in range(2)]
        RT1 = [A.f32(512) for _ in range(2)]
        RT2 = [A.f32(512) for _ in range(2)]
        CS = [A.f32(1024) for _ in range(2)]
        OUTB = [A.bf(512) for _ in range(3)]
        VST = [A.bf(8 * VW) for _ in range(2)]
        CKV32 = [A.f32(256) for _ in range(2)]
        CKVB = [A.bf(256) for _ in range(2)]
        CKVT = [A.bf(2 * 512).rearrange("p (k n) -> p k n", k=2) for _ in range(2)]
        SM = [A.f32(32) for _ in range(2)]

        dma("pool", WK, wk_d.rearrange("(k p) n -> p k n", p=128), (), ["WK"])
        dma("pool", WQ, wq_d.rearrange("(k p) n -> p k n", p=128), (), ["WQ"])
        dma("pool", WKV, wkv_d.rearrange("(k p) n -> p k n", p=128), (), ["WKV"])

        cnt = {"fm": 0, "ob": 0, "vs": 0, "ck": 0}
        if nkb < NKB:
            memset("pool", LOGF[:, :], 0.0, [("LOGF", kb) for kb in range(NKB)])

        def rope_store(ps, rps, n, cs_ap, rcs, dst_dram, rdst, dup_dst=None, rdup=None):
            i = cnt["fm"] % 2
            cnt["fm"] += 1
            copy("act", FMB[i][:, :n], ps[:, :n], [rps], [("FMB", i)])
            tt("dve", RT1[i][:, :n], ps[:, :n], cs_ap[:, 0:n], ALU.mult, [rps, rcs], [("RT1", i)])
            pp, rpp = pf()
            S.op("pe", mm(pp[:, :n], ropeP, FMB[i][:, :n], True, True), ["CB", ("FMB", i)], [rpp])
            tt("dve", RT2[i][:, :n], pp[:, :n], cs_ap[:, 512:512 + n], ALU.mult, [rpp, rcs], [("RT2", i)])
            o = cnt["ob"] % 3
            cnt["ob"] += 1
            if dup_dst is not None:
                tt("pool", dup_dst, RT1[i][:, :n], RT2[i][:, :n], ALU.add, [("RT1", i), ("RT2", i)], [rdup])
            else:
                tt("pool", OUTB[o][:, :n], RT1[i][:, :n], RT2[i][:, :n], ALU.add, [("RT1", i), ("RT2", i)], [("OUTB", o)])
                dma("sp", dst_dram, OUTB[o][:, :n], [("OUTB", o)], [rdst])

        def plain_store(ps, rps, n, dst_dram, rdst):
            o = cnt["ob"] % 3
            cnt["ob"] += 1
            copy("act", OUTB[o][:, :n], ps[:, :n], [rps], [("OUTB", o)])
            dma("sp", dst_dram, OUTB[o][:, :n], [("OUTB", o)], [rdst])

        def load_ln_transpose(src_rows, ti, hti, col0):
            b = ti % 2
            dma("sp", XT[b][:, :], src_rows, (), [("XT", b)])
            layer_norm_tile(XT[b], 0, 1, HB[b][:, :], "emb", ("XT", b), [("HB", b)])
            for half in range(2):
                tp_, rtp = pb()
                def fn(e, tp_=tp_, b=b, half=half):
                    ins = None
                    for kc in range(4):
                        k = half * 4 + kc
                        ins = e.transpose(tp_[:, kc * 128:(kc + 1) * 128], HB[b][:, k * 128:(k + 1) * 128], identB)
                    return ins
                S.op("pe", fn, [("HB", b), "CB"], [rtp])
                dst = HT[hti][:, half * 4:half * 4 + 4, col0:col0 + 128]
                src = tp_[:, 0:512].rearrange("p (k n) -> p k n", k=4)
                copy("act" if half == 0 else "dve", dst, src, [rtp], [("HT", hti, half, col0)])

        def ht_res(hti, ntile):
            return [("HT", hti, half, c * 128) for half in range(2) for c in range(ntile)]

        def proj_fm(W, col0, hti, n, rw):
            ps, rps = pf()
            items = [(ps[:, :n], W[:, kc, col0:col0 + 128], HT[hti][:, kc, :n], kc == 0, kc == 7) for kc in range(8)]
            S.op("pe", mm_chain(items), [rw] + ht_res(hti, (n + 127) // 128), [rps])
            return ps, rps

        nchunks = (nkb + 3) // 4 if stop_after >= 1 else 0
        for ch in range(nchunks):
            t0 = ch * 4
            ntile = min(4, nkb - t0)
            n = ntile * 128
            k0 = t0 * 128
            hti = ch % 2
            for t in range(ntile):
                load_ln_transpose(xk[(t0 + t) * 128:(t0 + t + 1) * 128, :], t0 + t, hti, t * 128)
            ci = ch % 2
            dma("sp", CS[ci][:, 0:n], ck_d[:, k0:k0 + n], (), [("CS", ci, 0)])
            dma("sp", CS[ci][:, 512:512 + n], sk_d[:, k0:k0 + n], (), [("CS", ci, 1)])
            csr = [("CS", ci, 0), ("CS", ci, 1)]
            ps, rps = proj_fm(WK, 256, hti, n, "WK")
            i = cnt["fm"] % 2
            cnt["fm"] += 1
            copy("act", FMB[i][:, :n], ps[:, :n], [rps], [("FMB", i)])
            tt("dve", RT1[i][:, :n], ps[:, :n], CS[ci][:, 0:n], ALU.mult, [rps] + csr, [("RT1", i)])
            pp, rpp = pf()
            S.op("pe", mm(pp[:, :n], ropeP, FMB[i][:, :n], True, True), ["CB", ("FMB", i)], [rpp])
            tt("dve", RT2[i][:, :n], pp[:, :n], CS[ci][:, 512:512 + n], ALU.mult, [rpp] + csr, [("RT2", i)])
            tt("pool", KIT[:, k0:k0 + n], RT1[i][:, :n], RT2[i][:, :n], ALU.add, [("RT1", i), ("RT2", i)], [("KIT", ch)])
            for ot in range(4):
                ps, rps = proj_fm(WK, 384 + ot * 128, hti, n, "WK")
                plain_store(ps, rps, n, kfT_d[ot, :, k0:k0 + n], ("kfT_d", ot, ch))
            for t in range(ntile):
                kb = t0 + t
                hres = [("HT", hti, half, t * 128) for half in range(2)]
                ps, rps = pf()
                items = [(ps[:, 0:256], HT[hti][:, kc, t * 128:(t + 1) * 128], WK[:, kc, 0:256], kc == 0, kc == 7) for kc in range(8)]
                S.op("pe", mm_chain(items), ["WK"] + hres, [rps])
                c = cnt["ck"] % 2
                cnt["ck"] += 1
                act(CKV32[c][:, :], ps[:, 0:256], AF.Square, [rps], [("CKV32", c)], accum=SM[c][:, 0:1])
                ts("dve", SM[c][:, 1:2], SM[c][:, 0:1], 1.0 / 256.0, 1e-6, ALU.mult, ALU.add, [("CKV32", c)], [("SM", c, 1)])
                act(SM[c][:, 3:4], SM[c][:, 1:2], AF.Ln, [("SM", c, 1)], [("SM", c, 3)])
                act(SM[c][:, 2:3], SM[c][:, 3:4], AF.Exp, [("SM", c, 3)], [("SM", c, 2)], scale=-0.5)
                stt("dve", CKVB[c][:, :], ps[:, 0:256], SM[c][:, 2:3], KVG[:, :], ALU.mult, ALU.mult, [rps, ("SM", c, 2), "KVG"], [("CKVB", c)])
                tp_, rtp = pb()
                def fn(e, tp_=tp_, c=c):
                    ins = None
                    for kc in range(2):
                        ins = e.transpose(tp_[:, kc * 128:(kc + 1) * 128], CKVB[c][:, kc * 128:(kc + 1) * 128], identB)
                    return ins
                S.op("pe", fn, [("CKVB", c), "CB"], [rtp])
                copy("act", CKVT[hti][:, :, t * 128:(t + 1) * 128], tp_[:, 0:256].rearrange("p (k n) -> p k n", k=2), [rtp], [("CKVT", hti, t)])
                ps, rps = pf()
                items = [(ps[:, 0:512], CKVT[hti][:, kc, t * 128:(t + 1) * 128], WKV[:, kc, 512:1024], kc == 0, kc == 1) for kc in range(2)]
                S.op("pe", mm_chain(items), ["WKV", ("CKVT", hti, t)], [rps])
                v = cnt["vs"] % 2
                cnt["vs"] += 1
                VV = VST[v].rearrange("p (h c) -> p h c", h=8)
                memset("pool", VST[v][:, :], 1.0, [("VST", v)])
                if kb == 0:
                    ts("dve", VV[:, :, 0:64], ps[:, 0:512].rearrange("p (h c) -> p h c", h=8), padmask, None, ALU.mult, None, [rps, ("VST", v), "CF"], [("VST", v)])
                    ts("dve", VV[:, :, 64:65], VV[:, :, 64:65], padmask, None, ALU.mult, None, [("VST", v), "CF"], [("VST", v)])
                else:
                    copy("act", VV[:, :, 0:64], ps[:, 0:512].rearrange("p (h c) -> p h c", h=8), [rps, ("VST", v)], [("VST", v)])
                dma("sp", va_d[kb], VST[v][:, :], [("VST", v)], [("va_d", kb)])
                ps, rps = pf()
                items = [(ps[:, 0:512], HT[hti][:, kc, t * 128:(t + 1) * 128], WK[:, kc, 896:1408], kc == 0, kc == 7) for kc in range(8)]
                S.op("pe", mm_chain(items), ["WK"] + hres, [rps])
                v = cnt["vs"] % 2
                cnt["vs"] += 1
                VV = VST[v].rearrange("p (h c) -> p h c", h=8)
                memset("pool", VST[v][:, :], 1.0, [("VST", v)])
                if kb == 0:
                    ts("dve", VV[:, :, 0:64], ps[:, 0:512].rearrange("p (h c) -> p h c", h=8), padmask, None, ALU.mult, None, [rps, ("VST", v), "CF"], [("VST", v)])
                    ts("dve", VV[:, :, 64:65], VV[:, :, 64:65], padmask, None, ALU.mult, None, [("VST", v), "CF"], [("VST", v)])
                else:
                    copy("act", VV[:, :, 0:64], ps[:, 0:512].rearrange("p (h c) -> p h c", h=8), [rps, ("VST", v)], [("VST", v)])
                dma("sp", vf_d[kb], VST[v][:, :], [("VST", v)], [("vf_d", kb)])
                ps, rps = pf()
                items = [(ps[:, 0:8], HT[hti][:, kc, t * 128:(t + 1) * 128], WK[:, kc, 1408:1416], kc == 0, kc == 7) for kc in range(8)]
                S.op("pe", mm_chain(items), ["WK"] + hres, [rps])
                c2 = cnt["ck"] % 2
                tt("dve", SM[c2][:, 8:16], ps[:, 0:8], BFG[:, :], ALU.add, [rps, "BFG"], [("SM", c2, 8)])
                act(SM[c2][:, 16:24], SM[c2][:, 8:16], AF.Exp, [("SM", c2, 8)], [("SM", c2, 16)], scale=-1.0)
                act(SM[c2][:, 24:32], SM[c2][:, 16:24], AF.Ln, [("SM", c2, 16)], [("SM", c2, 24)], bias=1.0)
                if kb == 0:
                    stt("dve", LOGF[:, kb * 8:(kb + 1) * 8], SM[c2][:, 24:32], -1.0, padmask.to_broadcast([128, 8]), ALU.mult, ALU.mult, [("SM", c2, 24), "CF"], [("LOGF", kb)])
                else:
                    ts("dve", LOGF[:, kb * 8:(kb + 1) * 8], SM[c2][:, 24:32], -1.0, None, ALU.mult, None, [("SM", c2, 24)], [("LOGF", kb)])
            for ot in range(4):
                ps, rps = pf()
                items = [(ps[:, :n], WKV[:, kc, ot * 128:(ot + 1) * 128], CKVT[hti][:, kc, :n], kc == 0, kc == 1) for kc in range(2)]
                S.op("pe", mm_chain(items), ["WKV"] + [("CKVT", hti, t) for t in range(ntile)], [rps])
                i = cnt["fm"] % 2
                cnt["fm"] += 1
                copy("act", FMB[i][:, :n], ps[:, :n], [rps], [("FMB", i)])
                tt("dve", RT1[i][:, :n], ps[:, :n], CS[ci][:, 0:n], ALU.mult, [rps] + csr, [("RT1", i)])
                pp, rpp = pf()
                S.op("pe", mm(pp[:, :n], ropeP, FMB[i][:, :n], True, True), ["CB", ("FMB", i)], [rpp])
                tt("dve", RT2[i][:, :n], pp[:, :n], CS[ci][:, 512:512 + n], ALU.mult, [rpp] + csr, [("RT2", i)])
                o = cnt["ob"] % 3
                cnt["ob"] += 1
                tt("pool", OUTB[o][:, :n], RT1[i][:, :n], RT2[i][:, :n], ALU.add, [("RT1", i), ("RT2", i)], [("OUTB", o)])
                dma("sp", kaT_d[ot, :, k0:k0 + n], OUTB[o][:, :n], [("OUTB", o)], [("kaT_d", ot, ch)])

        logf_res = [("LOGF", kb) for kb in range(NKB)]
        nb8 = NKB * 8
        for half in range(2):
            c0 = half * 264
            c1 = min(nb8, c0 + 264)
            ps, rps = pf()
            S.op("pe", mm(ps[:, 0:c1 - c0], utri, LOGF[:, c0:c1], True, True), ["CF"] + logf_res, [rps])
            copy("dve", CC[:, c0:c1], ps[:, 0:c1 - c0], [rps], [("CC", half)])
            ps2, rps2 = pf()
            S.op("pe", mm(ps2[:, 0:c1 - c0], onesF, LOGF[:, c0:c1], True, True), ["CF"] + logf_res, [rps2])
            copy("dve", PI[:, c0:c1], ps2[:, 0:c1 - c0], [rps2], [("PI", half)])
        for kb in range(1, NKB):
            tt("dve", PI[:, kb * 8:(kb + 1) * 8], PI[:, kb * 8:(kb + 1) * 8], PI[:, (kb - 1) * 8:kb * 8], ALU.add,
               [("PI", 0), ("PI", 1), ("PIx", kb - 1)], [("PIx", kb)])
        for kb in range(1, NKB):
            tt("pool", CC[:, kb * 8:(kb + 1) * 8], CC[:, kb * 8:(kb + 1) * 8], PI[:, (kb - 1) * 8:kb * 8], ALU.add,
               [("CC", 0), ("CC", 1), ("PIx", kb - 1), ("PIx", max(kb - 2, 0))], [("CCx", kb)])
        cc_res = [("CC", 0), ("CC", 1)] + [("CCx", kb) for kb in range(1, NKB)]
        pi_res = [("PI", 0), ("PI", 1)] + [("PIx", kb) for kb in range(1, NKB)]
        for j in range(nslot):
            ts("dve", CREF[:, j * 8:(j + 1) * 8], PI[:, (4 * j + 1) * 8:(4 * j + 2) * 8], selr[:, 0:1], None, ALU.mult, None, pi_res + ["CF"], [("CREF", j)])
            for t in range(1, 4):
                stt("dve", CREF[:, j * 8:(j + 1) * 8], PI[:, (4 * j + 1 + t) * 8:(4 * j + 2 + t) * 8], selr[:, t:t + 1], CREF[:, j * 8:(j + 1) * 8],
                    ALU.mult, ALU.add, pi_res + ["CF", ("CREF", j)], [("CREF", j)])

        for ch in range(nslot // 4 if nslot >= 4 else 1):
            ntile = min(4, nslot)
            n = ntile * 128
            q0 = ch * 512
            hti = ch % 2
            for t in range(ntile):
                load_ln_transpose(xq[q0 + t * 128:q0 + (t + 1) * 128, :], t, hti, t * 128)
            for kc in range(8):
                dma("sp", hqT_d[kc, :, q0:q0 + n], HT[hti][:, kc, :n], ht_res(hti, ntile), [("hqT_d", ch, kc)])
            ci = ch % 2
            dma("sp", CS[ci][:, 0:n], cq_d[:, q0:q0 + n], (), [("CS", ci, 0)])
            dma("sp", CS[ci][:, 512:512 + n], sq_d[:, q0:q0 + n], (), [("CS", ci, 1)])
            csr = [("CS", ci, 0), ("CS", ci, 1)]
            for grp, dst in ((0, qaT_d), (1, qiT_d)):
                for ot in range(4):
                    ps, rps = proj_fm(WQ, grp * 512 + ot * 128, hti, n, "WQ")
                    i = cnt["fm"] % 2
                    cnt["fm"] += 1
                    copy("act", FMB[i][:, :n], ps[:, :n], [rps], [("FMB", i)])
                    tt("dve", RT1[i][:, :n], ps[:, :n], CS[ci][:, 0:n], ALU.mult, [rps] + csr, [("RT1", i)])
                    pp, rpp = pf()
                    S.op("pe", mm(pp[:, :n], ropeP, FMB[i][:, :n], True, True), ["CB", ("FMB", i)], [rpp])
                    tt("dve", RT2[i][:, :n], pp[:, :n], CS[ci][:, 512:512 + n], ALU.mult, [rpp] + csr, [("RT2", i)])
                    o = cnt["ob"] % 3
                    cnt["ob"] += 1
                    tt("pool", OUTB[o][:, :n], RT1[i][:, :n], RT2[i][:, :n], ALU.add, [("RT1", i), ("RT2", i)], [("OUTB", o)])
                    dma("sp", dst[ot, :, q0:q0 + n], OUTB[o][:, :n], [("OUTB", o)], [(id(dst), ot, ch)])
            for ot in range(4):
                ps, rps = proj_fm(WQ, 1024 + ot * 128, hti, n, "WQ")
                plain_store(ps, rps, n, qfT_d[ot, :, q0:q0 + n], ("qfT_d", ot, ch))
            for t in range(ntile):
                sl = ch * 4 + t
                hres = [("HT", hti, half, t * 128) for half in range(2)]
                ps, rps = pf()
                items = [(ps[:, 0:8], HT[hti][:, kc, t * 128:(t + 1) * 128], WQ[:, kc, 1536:1544], kc == 0, kc == 7) for kc in range(8)]
                S.op("pe", mm_chain(items), ["WQ"] + hres, [rps])
                copy("dve", WIQ[:, sl * 8:(sl + 1) * 8], ps[:, 0:8], [rps], [("WIQ", sl)])

        S.barrier()

        A.off = P1_END
        SIDX = A.f32(TP)
        MNEGS = [A.bf(TP) for _ in range(2)]
        RL = [A.bf(512) for _ in range(8)]
        DW = A.bf(8 * 128)
        QI = A.bf(4 * 128).rearrange("p (t n) -> p t n", t=4)
        QA = A.bf(4 * 128).rearrange("p (t n) -> p t n", t=4)
        QF = A.bf(4 * 128).rearrange("p (t n) -> p t n", t=4)
        KS = [A.bf(4 * 512).rearrange("p (t n) -> p t n", t=4) for _ in range(2)]
        VS = [A.bf(4 * 8 * VW).rearrange("p (b n) -> p b n", b=4) for _ in range(2)]
        PT = [A.bf(1024) for _ in range(2)]
        BIASK = A.f32(NKB * 8)
        BS = A.f32(64)
        HW = A.f32(NBIS)
        OSB = A.f32(8 * VW)
        RCP = A.f32(8)
        YB = A.bf(512)
        YT = A.bf(4 * 128)
        yaT_d = dscr("yaT_d", [4, 128, nq])
        yfT_d = dscr("yfT_d", [4, 128, nq])
        P2_END = A.off

        O_A = (PF[4], ("PF", 4))
        O_B = (PF[5], ("PF", 5))
        sc = {"k": 0}

        def attention(j, nk, kT_dram, v_dram, Qt, rQ, fox, yT_dram, tagname, hook):
            memset("dve", O_A[0][:, :], 0.0, [O_A[1]])
            memset("dve", O_B[0][:, :], 0.0, [O_B[1]])
            if hook is not None:
                hook()
            MNEG = MNEGS[j % 2]
            nch = (nk + 3) // 4
            for ch in range(nch):
                nb = min(4, nk - ch * 4)
                w = nb * 128
                k0 = ch * 512
                b = sc["k"] % 2
                sc["k"] += 1
                kres = [(kT_dram_name(kT_dram), ot, ch) for ot in range(4)]
                dma("sp", KS[b][:, :, 0:w], kT_dram[:, :, k0:k0 + w].rearrange("t p k -> p t k"),
                    [(tagname + "kT", ot, ch) for ot in range(4)], [("KS", b)])
                dma("sp", VS[b][:, 0:nb, :], v_dram[ch * 4:ch * 4 + nb].rearrange("b p c -> p b c"),
                    [(tagname + "v", kb) for kb in range(ch * 4, ch * 4 + nb)], [("VS", b)])
                for kbl in range(nb):
                    kb = ch * 4 + kbl
                    tail = kb - (nk - 4)
                    pt = sc["k"] % 2
                    for par in range(2):
                        st_, rst = pf(0, 4)
                        items = []
                        if not fox:
                            items.append((st_[:, :], MNEG[:, kb * 128:(kb + 1) * 128], ident4, True, False))
                        elif tail >= 0:
                            items.append((st_[:, :], cmneg[:, tail * 128:(tail + 1) * 128], ident4, True, False))
                        masked = len(items) > 0
                        p0 = par * 64
                        for hh in range(4):
                            items.append((st_[:, hh * 128:(hh + 1) * 128],
                                          KS[b][p0:p0 + 64, hh, kbl * 128:(kbl + 1) * 128],
                                          Qt[p0:p0 + 64, hh, :], not masked, True))
                        reads = [("KS", b), rQ, "CB"]
                        if not fox:
                            reads.append(("MNEG", j % 2))
                        S.op("pe", mm_chain(items), reads, [rst])
                        if fox:
                            for hh in range(4):
                                h = 2 * hh + par
                                sl = par * 4 + hh
                                act(PTcur(kb)[:, sl * 128:(sl + 1) * 128], st_[:, hh * 128:(hh + 1) * 128], AF.Exp,
                                    [rst, "BIASK"], [("PT", kb % 2, par)], bias=BIASK[:, kb * 8 + h:kb * 8 + h + 1], scale=0.125)
                        else:
                            act(PTcur(kb)[:, par * 512:(par + 1) * 512], st_[:, :], AF.Exp, [rst], [("PT", kb % 2, par)], scale=0.125)
                    for hg, (O_, rO) in enumerate((O_A, O_B)):
                        items = []
                        for hh in range(4):
                            h = hg * 4 + hh
                            sl = (h % 2) * 4 + h // 2
                            items.append((O_[:, hh * VW:hh * VW + 65], PTcur(kb)[:, sl * 128:(sl + 1) * 128],
                                          VS[b][:, kbl, h * VW:h * VW + 65], False, False))
                        S.op("pe", mm_chain(items), [("PT", kb % 2, 0), ("PT", kb % 2, 1), ("VS", b)], [rO])
            for hg, (O_, rO) in enumerate((O_A, O_B)):
                copy("dve", OSB[:, hg * 4 * VW:(hg + 1) * 4 * VW], O_[:, 0:4 * VW], [rO], [("OSB", hg)])
            OV = OSB.rearrange("p (h c) -> p h c", h=8)
            S.op("dve", lambda e: e.reciprocal(RCP[:, :], OV[:, :, 64]), [("OSB", 0), ("OSB", 1)], ["RCP"])
            for h in range(8):
                ts("dve", YB[:, h * 64:(h + 1) * 64], OV[:, h, 0:64], RCP[:, h:h + 1], None, ALU.mult, None,
                   [("OSB", 0), ("OSB", 1), "RCP"], [("YB", h)])
            tp_, rtp = pb()
            def fn(e, tp_=tp_):
                ins = None
                for kc in range(4):
                    ins = e.transpose(tp_[:, kc * 128:(kc + 1) * 128], YB[:, kc * 128:(kc + 1) * 128], identB)
                return ins
            S.op("pe", fn, [("YB", h) for h in range(8)] + ["CB"], [rtp])
            copy("act", YT[:, :], tp_[:, 0:512], [rtp], ["YT"])
            dma("sp", yT_dram[:, :, j * 128:(j + 1) * 128].rearrange("t p n -> p t n"), YT.rearrange("p (t n) -> p t n", t=4),
                ["YT"], [(tagname + "yT", j)])

        def kT_dram_name(x):
            return id(x)

        def PTcur(kb):
            return PT[kb % 2]

        def emit_idx(j):
            nk = 4 * j + 5
            n = nk * 128
            dma("sp", QI, qiT_d[:, :, j * 128:(j + 1) * 128].rearrange("t p n -> p t n"), (), ["QI"])
            for h in range(8):
                ts("dve", DW[:, h * 128:(h + 1) * 128], identB, WIQ[:, j * 8 + h:j * 8 + h + 1], None, ALU.mult, None,
                   ["CB", ("WIQ", j)], [("DW", h)])
            nch = (nk + 3) // 4
            for ch in range(nch):
                w = min(512, n - ch * 512)
                k0 = ch * 512
                for h in range(8):
                    p0 = (h % 2) * 64
                    z, rz = pf(0, 4)
                    S.op("pe", mm(z[:, :w], QI[p0:p0 + 64, h // 2, :], KIT[p0:p0 + 64, k0:k0 + w], True, True), ["QI", "KITall"], [rz])
                    act(RL[h][:, :w], z[:, :w], AF.Relu, [rz], [("RL", h)])
                acc, racc = pf(4, 6)
                items = [(acc[:, :w], DW[:, h * 128:(h + 1) * 128], RL[h][:, :w], h == 0, h == 7) for h in range(8)]
                S.op("pe", mm_chain(items), [("DW", h) for h in range(8)] + [("RL", h) for h in range(8)], [racc])
                copy("dve", SIDX[:, k0:k0 + w], acc[:, :w], [racc], [("SIDX", ch)])

        def emit_bis(j):
            nk = 4 * j + 5
            n = nk * 128
            nch = (nk + 3) // 4
            MN = MNEGS[j % 2]
            rm = ("MNEG", j % 2)
            sres = [("SIDX", ch) for ch in range(nch)]
            S.op("dve", lambda e, n=n: e.tensor_reduce(BS[:, 0:1], SIDX[:, 0:n], AX.X, ALU.max, True), sres, [("BS", 0)])
            memset("dve", SIDX[:, 0:PAD], -1e30, [("SIDX", 0)])
            tt("dve", SIDX[:, n - 512:n], SIDX[:, n - 512:n], cmadd, ALU.add, sres + ["CF"], sres)
            ts("dve", BS[:, 2:3], BS[:, 0:1], -1.0, None, ALU.mult, None, [("BS", 0)], [("BS", 2)])
            ts("dve", BS[:, 1:2], BS[:, 0:1], 2.0, None, ALU.mult, None, [("BS", 0)], [("BS", 1)])
            ts("dve", HW[:, :], pow2, BS[:, 1:2], None, ALU.mult, None, ["CF", ("BS", 1)], ["HW"])
            for it in range(NBIS):
                tt("dve", BS[:, 3:4], BS[:, 2:3], HW[:, it:it + 1], ALU.add, [("BS", 2), "HW"], [("BS", 3)])
                ts("dve", MN[:, 0:n], SIDX[:, 0:n], BS[:, 3:4], 0.0, ALU.is_ge, ALU.add, sres + [("BS", 3)], [rm, ("BS", 4)], accum=BS[:, 4:5])
                stt("dve", BS[:, 5:6], BS[:, 4:5], 255.5, HW[:, it:it + 1], ALU.is_ge, ALU.mult, [("BS", 4), "HW"], [("BS", 5)])
                tt("dve", BS[:, 2:3], BS[:, 2:3], BS[:, 5:6], ALU.add, [("BS", 2), ("BS", 5)], [("BS", 2)])
            ts("dve", MN[:, 0:n], SIDX[:, 0:n], BS[:, 2:3], NEGM, ALU.is_lt, ALU.mult, sres + [("BS", 2)], [rm])

        nsl = nslot if stop_after >= 2 else 0
        if nsl:
            emit_idx(0)
            emit_bis(0)
        for j in range(nsl):
            nk = 4 * j + 5
            dma("sp", QA, qaT_d[:, :, j * 128:(j + 1) * 128].rearrange("t p n -> p t n"), (), ["QA"])
            dma("sp", QF, qfT_d[:, :, j * 128:(j + 1) * 128].rearrange("t p n -> p t n"), (), ["QF"])
            hook = None
            if j + 1 < nsl:
                emit_idx(j + 1)
                hook = (lambda jj=j + 1: emit_bis(jj))
            attention(j, nk, kaT_d, va_d, QA, "QA", False, yaT_d, "a", hook)
            BK = BIASK[:, 0:nk * 8].rearrange("p (k h) -> p k h", h=8)
            tt("dve", BK, CREF[:, j * 8:(j + 1) * 8].unsqueeze(1).to_broadcast([128, nk, 8]),
               CC[:, 0:nk * 8].rearrange("p (k h) -> p k h", h=8), ALU.subtract, [("CREF", j)] + cc_res, ["BIASK"])
            attention(j, nk, kfT_d, vf_d, QF, "QF", True, yfT_d, "f", None)

        S.barrier()

        A.off = 0
        GATE = A.f32(nslot * 32)
        P3_KEEP = A.off
        WBA = A.bf(4 * 1024).rearrange("p (k n) -> p k n", k=4)
        WBF = A.bf(4 * 1024).rearrange("p (k n) -> p k n", k=4)
        WG = A.bf(8 * 2048).rearrange("p (k n) -> p k n", k=8)
        WO = A.bf(8 * 1024).rearrange("p (k n) -> p k n", k=8)
        WR = A.f32(8 * 36).rearrange("p (k n) -> p k n", k=8)
        H1TS = [A.bf(D).rearrange("p (k n) -> p k n", k=8) for _ in range(2)]
        YAT = [A.bf(512).rearrange("p (t n) -> p t n", t=4) for _ in range(2)]
        YFT = [A.bf(512).rearrange("p (t n) -> p t n", t=4) for _ in range(2)]
        HQ = [A.bf(1024).rearrange("p (k n) -> p k n", k=8) for _ in range(2)]
        XQ = [A.f32(D) for _ in range(2)]
        H32 = [A.f32(D) for _ in range(2)]
        GA = [A.f32(D) for _ in range(2)]
        GF = [A.f32(D) for _ in range(2)]
        MGB = [A.bf(D) for _ in range(2)]
        MGT = [A.bf(D).rearrange("p (k n) -> p k n", k=8) for _ in range(2)]
        H1P = [A.f32(D) for _ in range(2)]
        H1 = [A.f32(D) for _ in range(2)]
        H1B = [A.bf(D) for _ in range(2)]
        H1T32 = [A.f32(D).rearrange("p (k n) -> p k n", k=8) for _ in range(2)]
        RS = [A.f32(256) for _ in range(2)]

        dma("pool", WBA, wba_d.rearrange("(k p) n -> p k n", p=128), (), ["WBA"])
        dma("pool", WBF, wbf_d.rearrange("(k p) n -> p k n", p=128), (), ["WBF"])
        dma("pool", WG, wg_d.rearrange("(k p) n -> p k n", p=128), (), ["WG"])
        dma("pool", WO, wo_d.rearrange("(k p) n -> p k n", p=128), (), ["WO"])
        dma("sp", WR, wr_d.rearrange("(k p) n -> p k n", p=128), (), ["WR"])

        for i in range(nslot if stop_after >= 3 else 0):
            b = i % 2
            dma("sp", YAT[b], yaT_d[:, :, i * 128:(i + 1) * 128].rearrange("t p n -> p t n"), [("ayT", i)], [("YAT", b)])
            dma("sp", YFT[b], yfT_d[:, :, i * 128:(i + 1) * 128].rearrange("t p n -> p t n"), [("fyT", i)], [("YFT", b)])
            dma("sp", HQ[b], hqT_d[:, :, i * 128:(i + 1) * 128].rearrange("k p n -> p k n"),
                [("hqT_d", i // 4, kc) for kc in range(8)], [("HQ", b)])
            dma("sp", XQ[b][:, :], xq[i * 128:(i + 1) * 128, :], (), [("XQ", b)])
            layer_norm_tile(XQ[b], 0, 1, H32[b][:, :], "emb", ("XQ", b), [("H32", b)])
            for half in range(2):
                c0 = half * 512
                for (Wcol, GT, nm) in ((0, GA, "GA"), (1024, GF, "GF")):
                    ps, rps = pf()
                    items = [(ps[:, :], HQ[b][:, kc, :], WG[:, kc, Wcol + c0:Wcol + c0 + 512], kc == 0, kc == 7) for kc in range(8)]
                    S.op("pe", mm_chain(items), ["WG", ("HQ", b)], [rps])
                    act(GT[b][:, c0:c0 + 512], ps[:, :], AF.Sigmoid, [rps], [(nm, b, half)])
                ps, rps = pf()
                items = [(ps[:, :], YAT[b][:, kc, :], WBA[:, kc, c0:c0 + 512], kc == 0, kc == 3) for kc in range(4)]
                S.op("pe", mm_chain(items), ["WBA", ("YAT", b)], [rps])
                tt("dve", GA[b][:, c0:c0 + 512], ps[:, :], GA[b][:, c0:c0 + 512], ALU.mult, [rps, ("GA", b, half)], [("GA", b, half)])
                ps, rps = pf()
                items = [(ps[:, :], YFT[b][:, kc, :], WBF[:, kc, c0:c0 + 512], kc == 0, kc == 3) for kc in range(4)]
                S.op("pe", mm_chain(items), ["WBF", ("YFT", b)], [rps])
                tt("dve", GF[b][:, c0:c0 + 512], ps[:, :], GF[b][:, c0:c0 + 512], ALU.mult, [rps, ("GF", b, half)], [("GF", b, half)])
                tt("pool", MGB[b][:, c0:c0 + 512], GA[b][:, c0:c0 + 512], GF[b][:, c0:c0 + 512], ALU.add,
                   [("GA", b, half), ("GF", b, half)], [("MGB", b, half)])
            for half in range(2):
                tp_, rtp = pb()
                def fn(e, tp_=tp_, b=b, half=half):
                    ins = None
                    for kc in range(4):
                        k = half * 4 + kc
                        ins = e.transpose(tp_[:, kc * 128:(kc + 1) * 128], MGB[b][:, k * 128:(k + 1) * 128], identB)
                    return ins
                S.op("pe", fn, [("MGB", b, 0), ("MGB", b, 1), "CB"], [rtp])
                copy("act", MGT[b][:, half * 4:half * 4 + 4, :], tp_[:, 0:512].rearrange("p (k n) -> p k n", k=4), [rtp], [("MGT", b, half)])
            for half in range(2):
                c0 = half * 512
                ps, rps = pf()
                items = [(ps[:, :], MGT[b][:, kc, :], WO[:, kc, c0:c0 + 512], kc == 0, kc == 7) for kc in range(8)]
                S.op("pe", mm_chain(items), ["WO", ("MGT", b, 0), ("MGT", b, 1)], [rps])
                stt("dve", H1P[b][:, c0:c0 + 512], H32[b][:, c0:c0 + 512], ALPHA, ps[:, :], ALU.mult, ALU.add, [("H32", b), rps], [("H1P", b, half)])
            layer_norm_tile(H1P[b], 2, 3, H1[b][:, :], "ln1", [("H1P", b, 0), ("H1P", b, 1)], [("H1", b)])
            dma("sp", h1_d[i * 128:(i + 1) * 128, :], H1[b][:, :], [("H1", b)], [("h1_d", i)])
            copy("act", H1B[b][:, :], H1[b][:, :], [("H1", b)], [("H1B", b)])
            for half in range(2):
                tp_, rtp = pb()
                def fn(e, tp_=tp_, b=b, half=half):
                    ins = None
                    for kc in range(4):
                        k = half * 4 + kc
                        ins = e.transpose(tp_[:, kc * 128:(kc + 1) * 128], H1B[b][:, k * 128:(k + 1) * 128], identB)
                    return ins
                S.op("pe", fn, [("H1B", b), "CB"], [rtp])
                copy("act", H1TS[b][:, half * 4:half * 4 + 4, :], tp_[:, 0:512].rearrange("p (k n) -> p k n", k=4), [rtp], [("H1TS", b, half)])
            dma("sp", h1T_d[:, :, i * 128:(i + 1) * 128].rearrange("k p n -> p k n"), H1TS[b], [("H1TS", b, 0), ("H1TS", b, 1)], [("h1T_d", i)])
            for half in range(2):
                ps, rps = pf()
                def fn(e, ps=ps, b=b, half=half):
                    ins = None
                    for kc in range(4):
                        k = half * 4 + kc
                        ins = e.transpose(ps[:, kc * 128:(kc + 1) * 128], H1[b][:, k * 128:(k + 1) * 128], identF)
                    return ins
                S.op("pe", fn, [("H1", b), "CF"], [rps])
                copy("dve", H1T32[b][:, half * 4:half * 4 + 4, :], ps[:, :].rearrange("p (k n) -> p k n", k=4), [rps], [("H1T32", b, half)])
            ps, rps = pf()
            items = [(ps[:, 0:36], H1T32[b][:, kc, :], WR[:, kc, :], kc == 0, kc == 7) for kc in range(8)]
            S.op("pe", mm_chain(items), ["WR", ("H1T32", b, 0), ("H1T32", b, 1)], [rps])
            R = RS[b]
            rR = ("RS", b)
            k_ = [0]

            def rr_(nm):
                return ("RS", b, nm)
            tt("dve", R[:, 0:36], ps[:, 0:36], BRT[:, :], ALU.add, [rps, "BRT"], [rr_("lg")])
            S.op("dve", lambda e, R=R: e.tensor_reduce(R[:, 40:41], R[:, 0:4], AX.X, ALU.max), [rr_("lg")], [rr_("gmax")])
            ts("dve", R[:, 44:48], R[:, 0:4], R[:, 40:41], None, ALU.is_ge, None, [rr_("lg"), rr_("gmax")], [rr_("goh")])
            ts("dve", R[:, 41:42], R[:, 40:41], -1.0, None, ALU.mult, None, [rr_("gmax")], [rr_("ngmax")])
            act(R[:, 48:52], R[:, 0:4], AF.Exp, [rr_("lg"), rr_("ngmax")], [rr_("gexp"), rr_("gsum")], bias=R[:, 41:42], scale=1.0, accum=R[:, 42:43])
            S.op("dve", lambda e, R=R: e.reciprocal(R[:, 43:44], R[:, 42:43]), [rr_("gsum")], [rr_("pg")])
            ts("dve", R[:, 52:56], R[:, 44:48], -1.0, 1e30, ALU.add, ALU.mult, [rr_("goh")], [rr_("gpen")])
            tt("dve", R[:, 64:96].rearrange("p (g e) -> p g e", g=4), R[:, 4:36].rearrange("p (g e) -> p g e", g=4),
               R[:, 52:56].unsqueeze(2).to_broadcast([128, 4, 8]), ALU.add, [rr_("lg"), rr_("gpen")], [rr_("em")])
            S.op("dve", lambda e, R=R: e.tensor_reduce(R[:, 56:57], R[:, 64:96], AX.X, ALU.max), [rr_("em")], [rr_("m1")])
            ts("dve", R[:, 96:128], R[:, 64:96], R[:, 56:57], None, ALU.is_ge, None, [rr_("em"), rr_("m1")], [rr_("oh1")])
            stt("dve", R[:, 128:160], R[:, 96:128], -1e30, R[:, 64:96], ALU.mult, ALU.add, [rr_("oh1"), rr_("em")], [rr_("em2")])
            S.op("dve", lambda e, R=R: e.tensor_reduce(R[:, 57:58], R[:, 128:160], AX.X, ALU.max), [rr_("em2")], [rr_("m2")])
            ts("dve", R[:, 160:192], R[:, 128:160], R[:, 57:58], None, ALU.is_ge, None, [rr_("em2"), rr_("m2")], [rr_("oh2")])
            tt("dve", R[:, 58:59], R[:, 57:58], R[:, 56:57], ALU.subtract, [rr_("m1"), rr_("m2")], [rr_("dm")])
            act(R[:, 59:60], R[:, 58:59], AF.Exp, [rr_("dm")], [rr_("edm")])
            ts("dve", R[:, 60:61], R[:, 59:60], 1.0, None, ALU.add, None, [rr_("edm")], [rr_("den")])
            S.op("dve", lambda e, R=R: e.reciprocal(R[:, 61:62], R[:, 60:61]), [rr_("den")], [rr_("w1")])
            tt("dve", R[:, 62:63], R[:, 61:62], R[:, 43:44], ALU.mult, [rr_("w1"), rr_("pg")], [rr_("g1")])
            tt("dve", R[:, 63:64], R[:, 43:44], R[:, 62:63], ALU.subtract, [rr_("pg"), rr_("g1")], [rr_("g2")])
            ts("dve", R[:, 192:224], R[:, 96:128], R[:, 62:63], None, ALU.mult, None, [rr_("oh1"), rr_("g1")], [rr_("t1")])
            stt("dve", GATE[:, i * 32:(i + 1) * 32], R[:, 160:192], R[:, 63:64], R[:, 192:224], ALU.mult, ALU.add,
                [rr_("oh2"), rr_("g2"), rr_("t1")], [("GATE", i)])

        S.barrier()

        A.off = P3_KEEP
        H1T = A.bf(8 * nq).rearrange("p (k n) -> p k n", k=8)
        ACC = A.f32(nslot * D)
        dma("sp", H1T, h1T_d.rearrange("k p n -> p k n"), [("h1T_d", i) for i in range(nslot)], ["H1Tall"])
        WGT = [A.bf(8 * 256).rearrange("p (k n) -> p k n", k=8) for _ in range(2)]
        WUP = [A.bf(8 * 256).rearrange("p (k n) -> p k n", k=8) for _ in range(2)]
        WDN = [A.bf(2 * 1024).rearrange("p (k n) -> p k n", k=2) for _ in range(2)]
        SG = [A.f32(512) for _ in range(2)]
        AT = [A.bf(2 * 512).rearrange("p (f n) -> p f n", f=2) for _ in range(2)]
        FX = [A.f32(D) for _ in range(2)]
        FO = [A.f32(D) for _ in range(2)]

        memset("pool", ACC[:, :], 0.0, ["ACCall"])
        nchq = max(1, nq // 512)
        ac = {"n": 0}
        for ex in range(nexp if stop_after >= 4 else 0):
            b = ex % 2
            dma("pool", WGT[b], wgate_d[ex].rearrange("(k p) n -> p k n", p=128), (), [("WGT", b)])
            dma("pool", WUP[b], wup_d[ex].rearrange("(k p) n -> p k n", p=128), (), [("WUP", b)])
            dma("pool", WDN[b], wdn_d[ex].rearrange("(k p) n -> p k n", p=128), (), [("WDN", b)])
            for ch in range(nchq):
                n = min(512, nq)
                q0 = ch * 512
                a = ac["n"] % 2
                ac["n"] += 1
                hres = ["H1Tall"]
                for ft in range(2):
                    pg_, rpg = pf()
                    items = [(pg_[:, :n], WGT[b][:, kc, ft * 128:(ft + 1) * 128], H1T[:, kc, q0:q0 + n], kc == 0, kc == 7) for kc in range(8)]
                    S.op("pe", mm_chain(items), [("WGT", b)] + hres, [rpg])
                    pu_, rpu = pf()
                    items = [(pu_[:, :n], WUP[b][:, kc, ft * 128:(ft + 1) * 128], H1T[:, kc, q0:q0 + n], kc == 0, kc == 7) for kc in range(8)]
                    S.op("pe", mm_chain(items), [("WUP", b)] + hres, [rpu])
                    s = (ac["n"] + ft) % 2
                    act(SG[s][:, :n], pg_[:, :n], AF.Silu, [rpg], [("SG", s)])
                    tt("dve", AT[a][:, ft, :n], SG[s][:, :n], pu_[:, :n], ALU.mult, [("SG", s), rpu], [("AT", a, ft)])
                for t in range(n // 128):
                    i = ch * 4 + t
                    for half in range(2):
                        c0 = half * 512
                        py, rpy = pf()
                        items = [(py[:, :], AT[a][:, ft, t * 128:(t + 1) * 128], WDN[b][:, ft, c0:c0 + 512], ft == 0, ft == 1) for ft in range(2)]
                        S.op("pe", mm_chain(items), [("WDN", b), ("AT", a, 0), ("AT", a, 1)], [rpy])
                        stt("dve", ACC[:, i * D + c0:i * D + c0 + 512], py[:, :], GATE[:, i * 32 + ex:i * 32 + ex + 1],
                            ACC[:, i * D + c0:i * D + c0 + 512], ALU.mult, ALU.add,
                            [rpy, ("GATE", i), "ACCall", ("ACC", i, half)], [("ACC", i, half)])
        for i in range(nslot):
            b = i % 2
            dma("sp", FX[b][:, :], h1_d[i * 128:(i + 1) * 128, :], [("h1_d", i)], [("FX", b)])
            stt("dve", FX[b][:, :], FX[b][:, :], ALPHA, ACC[:, i * D:(i + 1) * D], ALU.mult, ALU.add,
                [("FX", b), ("ACC", i, 0), ("ACC", i, 1), "ACCall"], [("FX", b)])
            layer_norm_tile(FX[b], 4, 5, FO[b][:, :], "ln2", ("FX", b), [("FO", b)])
            dma("sp", out_d[i * 128:(i + 1) * 128, :], FO[b][:, :], [("FO", b)], [("out", i)])
        S.finish()
        S.prepare()

        with nc.Block() as block:
            @block.tensor
            def _(e):
                S.replay("pe", e, sems)

            @block.scalar
            def _(e):
                S.replay("act", e, sems)

            @block.vector
            def _(e):
                S.replay("dve", e, sems)

            @block.gpsimd
            def _(e):
                S.replay("pool", e, sems)

            @block.sync
            def _(e):
                S.replay("sp", e, sems)
    return nc


def _consts(r):
    bf = ml_dtypes.bfloat16
    ident = np.eye(128, dtype=np.float32)
    k = np.arange(128)
    utri = (k[:, None] <= k[None, :]).astype(np.float32)
    ones = np.ones((128, 128), np.float32)
    q = np.arange(128)[:, None]
    kk = np.arange(128)[None, :]
    tri_ok = kk <= q
    cm = np.zeros((128, 4, 128), bool)
    for t in range(4):
        if t < r:
            cm[:, t, :] = True
        elif t == r:
            cm[:, t, :] = tri_ok
    cmadd = np.where(cm, 0.0, -1e30).astype(np.float32).reshape(128, 512)
    cmneg = np.where(cm, 0.0, NEGM).astype(np.float32).reshape(128, 512)
    selr = np.zeros((128, 4), np.float32)
    selr[:, r] = 1.0
    pow2 = np.tile((2.0 ** -(np.arange(NBIS) + 1.0)).astype(np.float32)[None], (128, 1))
    padmask = (np.arange(128) >= PAD).astype(np.float32)[:, None]
    cf = np.concatenate([ident, utri, ones, cmadd, selr, pow2, padmask], axis=1).astype(np.float32)
    P = np.zeros((128, 128), np.float32)
    for hb in (0, 64):
        for d in range(8):
            P[hb + d + 8, hb + d] = -1.0
            P[hb + d, hb + d + 8] = 1.0
    cb = np.concatenate([ident, np.tile(ident, (1, 4)), P, cmneg], axis=1).astype(bf)
    return cf, cb


def _rope_tables(pos):
    half = 8
    inv = (500000.0 ** (-np.arange(half, dtype=np.float32) / half)).astype(np.float32)
    ang = pos.astype(np.float32)[None, :] * inv[:, None]
    c8, s8 = np.cos(ang).astype(np.float32), np.sin(ang).astype(np.float32)
    n = pos.shape[0]
    C = np.ones((128, n), np.float32)
    Sn = np.zeros((128, n), np.float32)
    for hb in (0, 64):
        C[hb:hb + 8] = c8
        C[hb + 8:hb + 16] = c8
        Sn[hb:hb + 8] = s8
        Sn[hb + 8:hb + 16] = s8
    return C, Sn


_NC_CACHE = {}


def kernel(x, meta_tokens, emb_ln_g, emb_ln_b, w_in, b_forget, kv_norm_g, w_kv_up, w_branch_dsa,
           w_branch_fox, w_out, ln1_g, ln1_b, w_router_group, b_router_group, w_router_expert,
           b_router_expert, w_gate, w_up, w_down, ln2_g, ln2_b):
    f = lambda a: np.ascontiguousarray(np.asarray(a, dtype=np.float32))
    x = f(x)
    w_in0 = f(w_in)[0]
    offs = np.cumsum([0, 512, 256, 512, 64, 8, 512, 512, 512, 8, 1024, 1024])
    seg = lambda i: w_in0[:, offs[i]:offs[i + 1]]
    q_a, c_kv, q_i, k_i, w_i, q_f, k_f, v_f, f_lg, g_a, g_f = [seg(i) for i in range(11)]
    wk = f(np.concatenate([c_kv, k_i, k_i, k_f, v_f, f_lg], axis=1))
    wq = f(np.concatenate([q_a, q_i, q_f, w_i], axis=1))
    wg = f(np.concatenate([g_a, g_f], axis=1))
    rep = lambda v: f(np.tile(np.asarray(v, np.float32).reshape(1, -1), (128, 1)))
    lnp = f(np.concatenate([rep(emb_ln_g), rep(emb_ln_b), rep(ln1_g[0]), rep(ln1_b[0]), rep(ln2_g[0]), rep(ln2_b[0])], axis=1))
    wr = f(np.concatenate([f(w_router_group)[0], f(w_router_expert)[0]], axis=1))
    brt = rep(np.concatenate([f(b_router_group)[0], f(b_router_expert)[0]]))
    common = dict(
        wk=wk, wq=wq, wg=wg, wkv=f(w_kv_up)[0], wba=f(w_branch_dsa)[0], wbf=f(w_branch_fox)[0], wo=f(w_out)[0],
        wr=wr, wgate=f(w_gate)[0][:NEXP_RUN], wup=f(w_up)[0][:NEXP_RUN], wdn=f(w_down)[0][:NEXP_RUN], lnp=lnp, kvg=rep(kv_norm_g[0]),
        bfg=rep(b_forget[0]), brt=brt,
    )
    kpos = np.arange(TP) - PAD
    ck, sk = _rope_tables(kpos)
    in_maps = []
    own = []
    for c in range(8):
        b, r = c // 4, c % 4
        xkc = np.zeros((TP, D), np.float32)
        xkc[PAD:PAD + NMETA] = f(meta_tokens)
        xkc[PAD + NMETA:] = x[b]
        blocks = [4 * j + r for j in range(NSLOT)]
        rows = np.concatenate([np.arange(bl * 128, (bl + 1) * 128) for bl in blocks])
        own.append((b, rows))
        cq, sq = _rope_tables(rows + NMETA)
        cf, cb = _consts(r)
        m = dict(common)
        m.update(xk=xkc, xq=f(x[b][rows]), ck=ck, sk=sk, cq=cq, sq=sq, cf32=cf, cbf=cb)
        in_maps.append(m)
    if "nc" not in _NC_CACHE:
        _NC_CACHE["nc"] = build()
    nc = _NC_CACHE["nc"]
    res = run_bass_kernel_spmd(nc, in_maps, core_ids=list(range(8)))
    out = np.zeros((2, SEQ, D), np.float32)
    for c in range(8):
        b, rows = own[c]
        out[b, rows] = np.asarray(res.results[c]["out"], dtype=np.float32)
    return out
```

```python
import os
import numpy as np
import ml_dtypes
from contextlib import ExitStack
import concourse.bass as bass
import concourse.mybir as mybir
from concourse.bass_utils import run_bass_kernel_spmd

F32 = mybir.dt.float32
BF16 = mybir.dt.bfloat16
AF = mybir.ActivationFunctionType
ALU = mybir.AluOpType
AX = mybir.AxisListType

D = 1024
SEQ = 8192
NMETA = 16
PAD = 112
TP = SEQ + NMETA + PAD
NKB = TP // 128
NSLOT = 16
NQ = NSLOT * 128
NBIS = 20
ALPHA = 2.0 ** 0.25
NEGM = -30000.0
NEXP = 32
VW = 68
NEXP_RUN = 32

WK_COLS = 256 + 128 + 512 + 512 + 8
WQ_COLS = 512 * 3 + 8


class Sched:
    ENG = ("pe", "act", "dve", "pool", "sp")
    NDMA = 8

    def __init__(self):
        self.ops = {e: [] for e in self.ENG}
        self.count = {e: 0 for e in self.ENG}
        self.seen = {e: {} for e in self.ENG}
        self.res = {}
        self.dma_rr = {e: 0 for e in self.ENG}
        self.dma_val = {}

    def op(self, eng, fn, reads=(), writes=(), dma=False):
        self.nops = getattr(self, "nops", 0) + 1
        if self.nops > getattr(self, "maxops", 10 ** 9):
            return None
        need = {}

        def add(tok):
            if tok is None:
                return
            k, v = tok
            if need.get(k, 0) < v:
                need[k] = v

        reads = list(reads)
        writes = list(writes)
        for r in list(reads):
            if isinstance(r, tuple) and r and r[0] in ("PF", "PB") and r not in writes:
                writes.append(r)
        for r in reads:
            st = self.res.get(r)
            if st:
                add(st["w"])
        for w in writes:
            st = self.res.get(w)
            if st:
                add(st["w"])
                for k, v in st["r"].items():
                    add((k, v))
        waits = []
        for k, v in need.items():
            if eng == "pe" and k == "pe":
                continue
            if self.seen[eng].get(k, 0) >= v:
                continue
            waits.append((k, v))
            self.seen[eng][k] = v
        if dma:
            idx = self.dma_rr[eng]
            self.dma_rr[eng] = (idx + 1) % self.NDMA
            key = ("dma", eng, idx)
            prev = self.dma_val.get(key, 0)
            if prev > 0 and self.seen[eng].get(key, 0) < prev:
                waits.append((key, prev))
                self.seen[eng][key] = prev
            val = prev + 16
            self.dma_val[key] = val
            tok = (key, val)
        else:
            self.count[eng] += 1
            tok = (eng, self.count[eng])
        self.ops[eng].append((waits, fn, tok))
        for r in reads:
            st = self.res.get(r)
            if not st:
                st = {"w": None, "r": {}}
                self.res[r] = st
            if st["r"].get(tok[0], 0) < tok[1]:
                st["r"][tok[0]] = tok[1]
        for w in writes:
            self.res[w] = {"w": tok, "r": {}}
        return tok

    def barrier(self):
        toks = [(e, self.count[e]) for e in self.ENG if self.count[e] > 0]
        toks += [(k, v) for k, v in self.dma_val.items()]
        for e in self.ENG:
            waits = []
            for k, v in toks:
                if k == e and e == "pe":
                    continue
                if self.seen[e].get(k, 0) >= v:
                    continue
                waits.append((k, v))
                self.seen[e][k] = v
            if waits:
                self.ops[e].append((waits, None, None))

    def finish(self):
        waits = [(k, v) for k, v in self.dma_val.items() if self.seen["sp"].get(k, 0) < v]
        self.ops["sp"].append((waits, None, None))

    def prepare(self):
        import bisect as _b
        sig = {e: set() for e in self.ENG}
        for e in self.ENG:
            for waits, fn, tok in self.ops[e]:
                for k, v in waits:
                    if k in sig and k != e:
                        sig[k].add(v)
        self.sig = {e: sorted(v) for e, v in sig.items()}
        self._b = _b

    def rank(self, k, v):
        lst = self.sig[k]
        i = self._b.bisect_right(lst, v)
        assert i > 0 and lst[i - 1] == v, (k, v)
        return i

    def replay(self, name, e, sems):
        sigset = set(self.sig[name])
        for waits, fn, tok in self.ops[name]:
            for k, v in waits:
                if k == name:
                    e.drain()
                elif k in self.sig:
                    e.wait_ge(sems[k], self.rank(k, v))
                else:
                    e.wait_ge(sems[k], v)
            if fn is None:
                continue
            ins = fn(e)
            if tok[0][0] == "dma":
                ins.then_inc(sems[tok[0]], 16)
            elif tok[1] in sigset:
                ins.then_inc(sems[tok[0]], 1)


def build(nslot=NSLOT, nexp=NEXP, debug=False, nkb=NKB, stop_after=9):
    nc = bass.Bass("TRN2", target_bir_lowering=False)
    S = Sched()
    import os
    S.maxops = int(os.environ.get("MAXOPS", 10 ** 9))
    nq = nslot * 128

    def din(name, shape, dt=F32):
        return nc.dram_tensor(name, list(shape), dt, kind="ExternalInput").ap()

    def dscr(name, shape, dt=BF16):
        return nc.dram_tensor(name, list(shape), dt, kind="Internal").ap()

    xk = din("xk", [TP, D])
    xq = din("xq", [nq, D])
    wk_d = din("wk", [D, WK_COLS])
    wq_d = din("wq", [D, WQ_COLS])
    wg_d = din("wg", [D, 2048])
    wkv_d = din("wkv", [256, 1024])
    wba_d = din("wba", [512, 1024])
    wbf_d = din("wbf", [512, 1024])
    wo_d = din("wo", [D, D])
    wr_d = din("wr", [D, 36])
    wgate_d = din("wgate", [nexp, D, 256])
    wup_d = din("wup", [nexp, D, 256])
    wdn_d = din("wdn", [nexp, 256, D])
    lnp_d = din("lnp", [128, 6 * D])
    kvg_d = din("kvg", [128, 256])
    bfg_d = din("bfg", [128, 8])
    brt_d = din("brt", [128, 36])
    ck_d = din("ck", [128, TP])
    sk_d = din("sk", [128, TP])
    cq_d = din("cq", [128, nq])
    sq_d = din("sq", [128, nq])
    cf32_d = din("cf32", [128, 128 * 3 + 512 + 4 + NBIS + 1])
    cbf_d = din("cbf", [128, 128 + 512 + 128 + 512], BF16)
    out_d = nc.dram_tensor("out", [nq, D], F32, kind="ExternalOutput").ap()

    kaT_d = dscr("kaT_d", [4, 128, TP])
    kfT_d = dscr("kfT_d", [4, 128, TP])
    va_d = dscr("va_d", [NKB, 128, 8 * VW])
    vf_d = dscr("vf_d", [NKB, 128, 8 * VW])
    qaT_d = dscr("qaT_d", [4, 128, nq])
    qiT_d = dscr("qiT_d", [4, 128, nq])
    qfT_d = dscr("qfT_d", [4, 128, nq])
    hqT_d = dscr("hqT_d", [8, 128, nq])
    h1_d = dscr("h1_d", [nq, D], F32)
    h1T_d = dscr("h1T_d", [8, 128, nq])

    es = ExitStack()
    with es:
        def sb(name, cols, dt=F32, parts=128):
            return es.enter_context(nc.sbuf_tensor(name, [parts, cols], dt))

        CF = sb("CF", 128 * 3 + 512 + 4 + NBIS + 1)
        CB = sb("CB", 128 + 512 + 128 + 512, BF16)
        LNP = sb("LNP", 6 * D)
        KVG = sb("KVG", 256)
        BFG = sb("BFG", 8)
        BRT = sb("BRT", 36)
        identF = CF[:, 0:128]
        utri = CF[:, 128:256]
        onesF = CF[:, 256:384]
        cmadd = CF[:, 384:896]
        selr = CF[:, 896:900]
        pow2 = CF[:, 900:900 + NBIS]
        padmask = CF[:, 900 + NBIS:901 + NBIS]
        identB = CB[:, 0:128]
        ident4 = CB[:, 128:640]
        ropeP = CB[:, 640:768]
        cmneg = CB[:, 768:1280]

        ARENA_COLS = 38 * 1024
        ARENA = sb("ARENA", ARENA_COLS)
        ARENA_B = ARENA[:, :].bitcast(BF16)

        class Bump:
            def __init__(self):
                self.off = 0

            def f32(self, cols):
                o = self.off // 4
                self.off += cols * 4
                assert self.off <= ARENA_COLS * 4, self.off
                return ARENA[:, o:o + cols]

            def bf(self, cols):
                cols2 = (cols + 1) // 2 * 2
                o = self.off // 2
                self.off += cols2 * 2
                assert self.off <= ARENA_COLS * 4, self.off
                return ARENA_B[:, o:o + cols]

        PF = [es.enter_context(nc.psum_tensor(f"PF{i}", [128, 512], F32)) for i in range(6)]
        PB = [es.enter_context(nc.psum_tensor(f"PB{i}", [128, 1024], BF16)) for i in range(2)]
        rr = {"n": 0, "b": 0}

        def pf(lo=0, hi=6):
            i = lo + rr["n"] % (hi - lo)
            rr["n"] += 1
            return PF[i], ("PF", i)

        def pb():
            i = rr["b"] % 2
            rr["b"] += 1
            return PB[i], ("PB", i)

        sems = {}
        for e in Sched.ENG:
            sems[e] = es.enter_context(nc.semaphore(f"s_{e}"))
        for e in ("sp", "pool", "act"):
            for i in range(Sched.NDMA):
                sems[("dma", e, i)] = es.enter_context(nc.semaphore(f"d_{e}{i}"))

        def dma(q, out, in_, reads, writes):
            S.op(q, lambda e, o=out, i=in_: e.dma_start(out=o, in_=i), reads, writes, dma=True)

        def mm(out, lhsT, rhs, start, stop, skip=True):
            return lambda e: e.matmul(out, lhsT, rhs, start=start, stop=stop, skip_group_check=skip)

        def mm_chain(items):
            def fn(e):
                ins = None
                for (o, l, r, st, sp_) in items:
                    ins = e.matmul(o, l, r, start=st, stop=sp_, skip_group_check=True)
                return ins
            return fn

        def act(out, in_, func, reads, writes, bias=0.0, scale=1.0, accum=None):
            if accum is None:
                S.op("act", lambda e: e.activation(out=out, in_=in_, func=func, bias=bias, scale=scale), reads, writes)
            else:
                S.op("act", lambda e: e.activation(out=out, in_=in_, func=func, bias=bias, scale=scale, accum_out=accum), reads, writes)

        def tt(eng, out, in0, in1, op, reads, writes):
            S.op(eng, lambda e: e.tensor_tensor(out=out, in0=in0, in1=in1, op=op), reads, writes)

        def ts(eng, out, in0, s1, s2, op0, op1, reads, writes, accum=None):
            if accum is None:
                if op1 is None:
                    S.op(eng, lambda e: e.tensor_scalar(out, in0, s1, None, op0), reads, writes)
                else:
                    S.op(eng, lambda e: e.tensor_scalar(out, in0, s1, s2, op0, op1), reads, writes)
            else:
                S.op(eng, lambda e: e.tensor_scalar(out, in0, s1, s2, op0, op1, accum), reads, writes)

        def stt(eng, out, in0, scalar, in1, op0, op1, reads, writes):
            S.op(eng, lambda e: e.scalar_tensor_tensor(out=out, in0=in0, scalar=scalar, in1=in1, op0=op0, op1=op1), reads, writes)

        def copy(eng, out, in_, reads, writes):
            if eng == "act":
                S.op("act", lambda e: e.copy(out, in_), reads, writes)
            else:
                S.op(eng, lambda e: e.tensor_copy(out, in_), reads, writes)

        def memset(eng, ap, val, writes):
            S.op(eng, lambda e: e.memset(ap, val), (), writes)

        def transpose_to(out_ps, in_sb, ident):
            return lambda e: e.transpose(out_ps, in_sb, ident)

        dma("sp", CF[:, :], cf32_d, (), ["CF"])
        dma("sp", CB[:, :], cbf_d, (), ["CB"])
        dma("sp", LNP[:, :], lnp_d, (), ["LNP"])
        dma("sp", KVG[:, :], kvg_d, (), ["KVG"])
        dma("sp", BFG[:, :], bfg_d, (), ["BFG"])
        dma("sp", BRT[:, :], brt_d, (), ["BRT"])
        CONST = ["CF", "CB", "LNP", "KVG", "BFG", "BRT"]

        uid = {"n": 0}

        def layer_norm_tile(x32, gcol, bcol, out_ap, tag, rx, wout, eps=1e-5, prescale=None):
            uid["n"] += 1
            u = uid["n"] % 2
            st = LNS[u]
            rs = ("LNS", u)
            rx = rx if isinstance(rx, list) else [rx]
            S.op("dve", lambda e: e.bn_stats(st[:, 0:6], x32[:, 0:512]), rx, [rs])
            S.op("dve", lambda e: e.bn_stats(st[:, 6:12], x32[:, 512:1024]), rx, [(rs, 1)])
            S.op("dve", lambda e: e.bn_aggr(st[:, 12:14], st[:, 0:12]), [rs, (rs, 1)], [(rs, 2)])
            ts("dve", st[:, 11:12], st[:, 13:14], eps, None, ALU.add, None, [(rs, 2), (rs, 1)], [(rs, 5)])
            act(st[:, 10:11], st[:, 11:12], AF.Ln, [(rs, 5)], [(rs, 6)])
            act(st[:, 14:15], st[:, 10:11], AF.Exp, [(rs, 6)], [(rs, 3)], scale=-0.5)
            stt("dve", st[:, 15:16], st[:, 12:13], -1.0, st[:, 14:15], ALU.mult, ALU.mult, [(rs, 2), (rs, 3)], [(rs, 4)])
            xn = LNX[u]
            rxn = ("LNX", u)
            act(xn[:, :], x32, AF.Identity, rx + [(rs, 3), (rs, 4)], [rxn], bias=st[:, 15:16], scale=st[:, 14:15])
            tt("dve", xn[:, :], xn[:, :], LNP[:, gcol * D:(gcol + 1) * D], ALU.mult, [rxn, "LNP"], [rxn])
            tt("pool", out_ap, xn[:, :], LNP[:, bcol * D:(bcol + 1) * D], ALU.add, [rxn, "LNP"], wout)

        LNS = [sb(f"LNS{i}", 16) for i in range(2)]
        LNX = [sb(f"LNX{i}", D) for i in range(2)]

        A = Bump()
        KIT = A.bf(TP)
        LOGF = A.f32(NKB * 8)
        WIQ = A.f32(nslot * 8)
        CC = A.f32(NKB * 8)
        PI = A.f32(NKB * 8)
        CREF = A.f32(nslot * 8)
        P1_END = A.off
        WKs = A.bf(8 * WK_COLS)
        WK = WKs.rearrange("p (k n) -> p k n", k=8)
        WQs = A.bf(8 * WQ_COLS)
        WQ = WQs.rearrange("p (k n) -> p k n", k=8)
        WKVs = A.bf(2 * 1024)
        WKV = WKVs.rearrange("p (k n) -> p k n", k=2)
        XT = [A.f32(D) for _ in range(2)]
        HB = [A.bf(D) for _ in range(2)]
        HT = [A.bf(8 * 512).rearrange("p (k n) -> p k n", k=8) for _ in range(2)]
        FMB = [A.bf(512) for _ in range(2)]
        RT1 = [A.f32(512) for _ in range(2)]
        RT2 = [A.f32(512) for _ in range(2)]
        CS = [A.f32(1024) for _ in range(2)]
        OUTB = [A.bf(512) for _ in range(3)]
        VST = [A.bf(8 * VW) for _ in range(2)]
        CKV32 = [A.f32(256) for _ in range(2)]
        CKVB = [A.bf(256) for _ in range(2)]
        CKVT = [A.bf(2 * 512).rearrange("p (k n) -> p k n", k=2) for _ in range(2)]
        SM = [A.f32(32) for _ in range(2)]

        dma("pool", WK, wk_d.rearrange("(k p) n -> p k n", p=128), (), ["WK"])
        dma("pool", WQ, wq_d.rearrange("(k p) n -> p k n", p=128), (), ["WQ"])
        dma("pool", WKV, wkv_d.rearrange("(k p) n -> p k n", p=128), (), ["WKV"])

        cnt = {"fm": 0, "ob": 0, "vs": 0, "ck": 0}
        if nkb < NKB:
            memset("pool", LOGF[:, :], 0.0, [("LOGF", kb) for kb in range(NKB)])

        def rope_store(ps, rps, n, cs_ap, rcs, dst_dram, rdst, dup_dst=None, rdup=None):
            i = cnt["fm"] % 2
            cnt["fm"] += 1
            copy("act", FMB[i][:, :n], ps[:, :n], [rps], [("FMB", i)])
            tt("dve", RT1[i][:, :n], ps[:, :n], cs_ap[:, 0:n], ALU.mult, [rps, rcs], [("RT1", i)])
            pp, rpp = pf()
            S.op("pe", mm(pp[:, :n], ropeP, FMB[i][:, :n], True, True), ["CB", ("FMB", i)], [rpp])
            tt("dve", RT2[i][:, :n], pp[:, :n], cs_ap[:, 512:512 + n], ALU.mult, [rpp, rcs], [("RT2", i)])
            o = cnt["ob"] % 3
            cnt["ob"] += 1
            if dup_dst is not None:
                tt("pool", dup_dst, RT1[i][:, :n], RT2[i][:, :n], ALU.add, [("RT1", i), ("RT2", i)], [rdup])
            else:
                tt("pool", OUTB[o][:, :n], RT1[i][:, :n], RT2[i][:, :n], ALU.add, [("RT1", i), ("RT2", i)], [("OUTB", o)])
                dma("sp", dst_dram, OUTB[o][:, :n], [("OUTB", o)], [rdst])

        def plain_store(ps, rps, n, dst_dram, rdst):
            o = cnt["ob"] % 3
            cnt["ob"] += 1
            copy("act", OUTB[o][:, :n], ps[:, :n], [rps], [("OUTB", o)])
            dma("sp", dst_dram, OUTB[o][:, :n], [("OUTB", o)], [rdst])

        def load_ln_transpose(src_rows, ti, hti, col0):
            b = ti % 2
            dma("sp", XT[b][:, :], src_rows, (), [("XT", b)])
            layer_norm_tile(XT[b], 0, 1, HB[b][:, :], "emb", ("XT", b), [("HB", b)])
            for half in range(2):
                tp_, rtp = pb()
                def fn(e, tp_=tp_, b=b, half=half):
                    ins = None
                    for kc in range(4):
                        k = half * 4 + kc
                        ins = e.transpose(tp_[:, kc * 128:(kc + 1) * 128], HB[b][:, k * 128:(k + 1) * 128], identB)
                    return ins
                S.op("pe", fn, [("HB", b), "CB"], [rtp])
                dst = HT[hti][:, half * 4:half * 4 + 4, col0:col0 + 128]
                src = tp_[:, 0:512].rearrange("p (k n) -> p k n", k=4)
                copy("act" if half == 0 else "dve", dst, src, [rtp], [("HT", hti, half, col0)])

        def ht_res(hti, ntile):
            return [("HT", hti, half, c * 128) for half in range(2) for c in range(ntile)]

        def proj_fm(W, col0, hti, n, rw):
            ps, rps = pf()
            items = [(ps[:, :n], W[:, kc, col0:col0 + 128], HT[hti][:, kc, :n], kc == 0, kc == 7) for kc in range(8)]
            S.op("pe", mm_chain(items), [rw] + ht_res(hti, (n + 127) // 128), [rps])
            return ps, rps

        nchunks = (nkb + 3) // 4 if stop_after >= 1 else 0
        for ch in range(nchunks):
            t0 = ch * 4
            ntile = min(4, nkb - t0)
            n = ntile * 128
            k0 = t0 * 128
            hti = ch % 2
            for t in range(ntile):
                load_ln_transpose(xk[(t0 + t) * 128:(t0 + t + 1) * 128, :], t0 + t, hti, t * 128)
            ci = ch % 2
            dma("sp", CS[ci][:, 0:n], ck_d[:, k0:k0 + n], (), [("CS", ci, 0)])
            dma("sp", CS[ci][:, 512:512 + n], sk_d[:, k0:k0 + n], (), [("CS", ci, 1)])
            csr = [("CS", ci, 0), ("CS", ci, 1)]
            ps, rps = proj_fm(WK, 256, hti, n, "WK")
            i = cnt["fm"] % 2
            cnt["fm"] += 1
            copy("act", FMB[i][:, :n], ps[:, :n], [rps], [("FMB", i)])
            tt("dve", RT1[i][:, :n], ps[:, :n], CS[ci][:, 0:n], ALU.mult, [rps] + csr, [("RT1", i)])
            pp, rpp = pf()
            S.op("pe", mm(pp[:, :n], ropeP, FMB[i][:, :n], True, True), ["CB", ("FMB", i)], [rpp])
            tt("dve", RT2[i][:, :n], pp[:, :n], CS[ci][:, 512:512 + n], ALU.mult, [rpp] + csr, [("RT2", i)])
            tt("pool", KIT[:, k0:k0 + n], RT1[i][:, :n], RT2[i][:, :n], ALU.add, [("RT1", i), ("RT2", i)], [("KIT", ch)])
            for ot in range(4):
                ps, rps = proj_fm(WK, 384 + ot * 128, hti, n, "WK")
                plain_store(ps, rps, n, kfT_d[ot, :, k0:k0 + n], ("kfT_d", ot, ch))
            for t in range(ntile):
                kb = t0 + t
                hres = [("HT", hti, half, t * 128) for half in range(2)]
                ps, rps = pf()
                items = [(ps[:, 0:256], HT[hti][:, kc, t * 128:(t + 1) * 128], WK[:, kc, 0:256], kc == 0, kc == 7) for kc in range(8)]
                S.op("pe", mm_chain(items), ["WK"] + hres, [rps])
                c = cnt["ck"] % 2
                cnt["ck"] += 1
                act(CKV32[c][:, :], ps[:, 0:256], AF.Square, [rps], [("CKV32", c)], accum=SM[c][:, 0:1])
                ts("dve", SM[c][:, 1:2], SM[c][:, 0:1], 1.0 / 256.0, 1e-6, ALU.mult, ALU.add, [("CKV32", c)], [("SM", c, 1)])
                act(SM[c][:, 3:4], SM[c][:, 1:2], AF.Ln, [("SM", c, 1)], [("SM", c, 3)])
                act(SM[c][:, 2:3], SM[c][:, 3:4], AF.Exp, [("SM", c, 3)], [("SM", c, 2)], scale=-0.5)
                stt("dve", CKVB[c][:, :], ps[:, 0:256], SM[c][:, 2:3], KVG[:, :], ALU.mult, ALU.mult, [rps, ("SM", c, 2), "KVG"], [("CKVB", c)])
                tp_, rtp = pb()
                def fn(e, tp_=tp_, c=c):
                    ins = None
                    for kc in range(2):
                        ins = e.transpose(tp_[:, kc * 128:(kc + 1) * 128], CKVB[c][:, kc * 128:(kc + 1) * 128], identB)
                    return ins
                S.op("pe", fn, [("CKVB", c), "CB"], [rtp])
                copy("act", CKVT[hti][:, :, t * 128:(t + 1) * 128], tp_[:, 0:256].rearrange("p (k n) -> p k n", k=2), [rtp], [("CKVT", hti, t)])
                ps, rps = pf()
                items = [(ps[:, 0:512], CKVT[hti][:, kc, t * 128:(t + 1) * 128], WKV[:, kc, 512:1024], kc == 0, kc == 1) for kc in range(2)]
                S.op("pe", mm_chain(items), ["WKV", ("CKVT", hti, t)], [rps])
                v = cnt["vs"] % 2
                cnt["vs"] += 1
                VV = VST[v].rearrange("p (h c) -> p h c", h=8)
                memset("pool", VST[v][:, :], 1.0, [("VST", v)])
                if kb == 0:
                    ts("dve", VV[:, :, 0:64], ps[:, 0:512].rearrange("p (h c) -> p h c", h=8), padmask, None, ALU.mult, None, [rps, ("VST", v), "CF"], [("VST", v)])
                    ts("dve", VV[:, :, 64:65], VV[:, :, 64:65], padmask, None, ALU.mult, None, [("VST", v), "CF"], [("VST", v)])
                else:
                    copy("act", VV[:, :, 0:64], ps[:, 0:512].rearrange("p (h c) -> p h c", h=8), [rps, ("VST", v)], [("VST", v)])
                dma("sp", va_d[kb], VST[v][:, :], [("VST", v)], [("va_d", kb)])
                ps, rps = pf()
                items = [(ps[:, 0:512], HT[hti][:, kc, t * 128:(t + 1) * 128], WK[:, kc, 896:1408], kc == 0, kc == 7) for kc in range(8)]
                S.op("pe", mm_chain(items), ["WK"] + hres, [rps])
                v = cnt["vs"] % 2
                cnt["vs"] += 1
                VV = VST[v].rearrange("p (h c) -> p h c", h=8)
                memset("pool", VST[v][:, :], 1.0, [("VST", v)])
                if kb == 0:
                    ts("dve", VV[:, :, 0:64], ps[:, 0:512].rearrange("p (h c) -> p h c", h=8), padmask, None, ALU.mult, None, [rps, ("VST", v), "CF"], [("VST", v)])
                    ts("dve", VV[:, :, 64:65], VV[:, :, 64:65], padmask, None, ALU.mult, None, [("VST", v), "CF"], [("VST", v)])
                else:
                    copy("act", VV[:, :, 0:64], ps[:, 0:512].rearrange("p (h c) -> p h c", h=8), [rps, ("VST", v)], [("VST", v)])
                dma("sp", vf_d[kb], VST[v][:, :], [("VST", v)], [("vf_d", kb)])
                ps, rps = pf()
                items = [(ps[:, 0:8], HT[hti][:, kc, t * 128:(t + 1) * 128], WK[:, kc, 1408:1416], kc == 0, kc == 7) for kc in range(8)]
                S.op("pe", mm_chain(items), ["WK"] + hres, [rps])
                c2 = cnt["ck"] % 2
                tt("dve", SM[c2][:, 8:16], ps[:, 0:8], BFG[:, :], ALU.add, [rps, "BFG"], [("SM", c2, 8)])
                act(SM[c2][:, 16:24], SM[c2][:, 8:16], AF.Exp, [("SM", c2, 8)], [("SM", c2, 16)], scale=-1.0)
                act(SM[c2][:, 24:32], SM[c2][:, 16:24], AF.Ln, [("SM", c2, 16)], [("SM", c2, 24)], bias=1.0)
                if kb == 0:
                    stt("dve", LOGF[:, kb * 8:(kb + 1) * 8], SM[c2][:, 24:32], -1.0, padmask.to_broadcast([128, 8]), ALU.mult, ALU.mult, [("SM", c2, 24), "CF"], [("LOGF", kb)])
                else:
                    ts("dve", LOGF[:, kb * 8:(kb + 1) * 8], SM[c2][:, 24:32], -1.0, None, ALU.mult, None, [("SM", c2, 24)], [("LOGF", kb)])
            for ot in range(4):
                ps, rps = pf()
                items = [(ps[:, :n], WKV[:, kc, ot * 128:(ot + 1) * 128], CKVT[hti][:, kc, :n], kc == 0, kc == 1) for kc in range(2)]
                S.op("pe", mm_chain(items), ["WKV"] + [("CKVT", hti, t) for t in range(ntile)], [rps])
                i = cnt["fm"] % 2
                cnt["fm"] += 1
                copy("act", FMB[i][:, :n], ps[:, :n], [rps], [("FMB", i)])
                tt("dve", RT1[i][:, :n], ps[:, :n], CS[ci][:, 0:n], ALU.mult, [rps] + csr, [("RT1", i)])
                pp, rpp = pf()
                S.op("pe", mm(pp[:, :n], ropeP, FMB[i][:, :n], True, True), ["CB", ("FMB", i)], [rpp])
                tt("dve", RT2[i][:, :n], pp[:, :n], CS[ci][:, 512:512 + n], ALU.mult, [rpp] + csr, [("RT2", i)])
                o = cnt["ob"] % 3
                cnt["ob"] += 1
                tt("pool", OUTB[o][:, :n], RT1[i][:, :n], RT2[i][:, :n], ALU.add, [("RT1", i), ("RT2", i)], [("OUTB", o)])
                dma("sp", kaT_d[ot, :, k0:k0 + n], OUTB[o][:, :n], [("OUTB", o)], [("kaT_d", ot, ch)])

        logf_res = [("LOGF", kb) for kb in range(NKB)]
        nb8 = NKB * 8
        for half in range(2):
            c0 = half * 264
            c1 = min(nb8, c0 + 264)
            ps, rps = pf()
            S.op("pe", mm(ps[:, 0:c1 - c0], utri, LOGF[:, c0:c1], True, True), ["CF"] + logf_res, [rps])
            copy("dve", CC[:, c0:c1], ps[:, 0:c1 - c0], [rps], [("CC", half)])
            ps2, rps2 = pf()
            S.op("pe", mm(ps2[:, 0:c1 - c0], onesF, LOGF[:, c0:c1], True, True), ["CF"] + logf_res, [rps2])
            copy("dve", PI[:, c0:c1], ps2[:, 0:c1 - c0], [rps2], [("PI", half)])
        for kb in range(1, NKB):
            tt("dve", PI[:, kb * 8:(kb + 1) * 8], PI[:, kb * 8:(kb + 1) * 8], PI[:, (kb - 1) * 8:kb * 8], ALU.add,
               [("PI", 0), ("PI", 1), ("PIx", kb - 1)], [("PIx", kb)])
        for kb in range(1, NKB):
            tt("pool", CC[:, kb * 8:(kb + 1) * 8], CC[:, kb * 8:(kb + 1) * 8], PI[:, (kb - 1) * 8:kb * 8], ALU.add,
               [("CC", 0), ("CC", 1), ("PIx", kb - 1), ("PIx", max(kb - 2, 0))], [("CCx", kb)])
        cc_res = [("CC", 0), ("CC", 1)] + [("CCx", kb) for kb in range(1, NKB)]
        pi_res = [("PI", 0), ("PI", 1)] + [("PIx", kb) for kb in range(1, NKB)]
        for j in range(nslot):
            ts("dve", CREF[:, j * 8:(j + 1) * 8], PI[:, (4 * j + 1) * 8:(4 * j + 2) * 8], selr[:, 0:1], None, ALU.mult, None, pi_res + ["CF"], [("CREF", j)])
            for t in range(1, 4):
                stt("dve", CREF[:, j * 8:(j + 1) * 8], PI[:, (4 * j + 1 + t) * 8:(4 * j + 2 + t) * 8], selr[:, t:t + 1], CREF[:, j * 8:(j + 1) * 8],
                    ALU.mult, ALU.add, pi_res + ["CF", ("CREF", j)], [("CREF", j)])

        for ch in range(nslot // 4 if nslot >= 4 else 1):
            ntile = min(4, nslot)
            n = ntile * 128
            q0 = ch * 512
            hti = ch % 2
            for t in range(ntile):
                load_ln_transpose(xq[q0 + t * 128:q0 + (t + 1) * 128, :], t, hti, t * 128)
            for kc in range(8):
                dma("sp", hqT_d[kc, :, q0:q0 + n], HT[hti][:, kc, :n], ht_res(hti, ntile), [("hqT_d", ch, kc)])
            ci = ch % 2
            dma("sp", CS[ci][:, 0:n], cq_d[:, q0:q0 + n], (), [("CS", ci, 0)])
            dma("sp", CS[ci][:, 512:512 + n], sq_d[:, q0:q0 + n], (), [("CS", ci, 1)])
            csr = [("CS", ci, 0), ("CS", ci, 1)]
            for grp, dst in ((0, qaT_d), (1, qiT_d)):
                for ot in range(4):
                    ps, rps = proj_fm(WQ, grp * 512 + ot * 128, hti, n, "WQ")
                    i = cnt["fm"] % 2
                    cnt["fm"] += 1
                    copy("act", FMB[i][:, :n], ps[:, :n], [rps], [("FMB", i)])
                    tt("dve", RT1[i][:, :n], ps[:, :n], CS[ci][:, 0:n], ALU.mult, [rps] + csr, [("RT1", i)])
                    pp, rpp = pf()
                    S.op("pe", mm(pp[:, :n], ropeP, FMB[i][:, :n], True, True), ["CB", ("FMB", i)], [rpp])
                    tt("dve", RT2[i][:, :n], pp[:, :n], CS[ci][:, 512:512 + n], ALU.mult, [rpp] + csr, [("RT2", i)])
                    o = cnt["ob"] % 3
                    cnt["ob"] += 1
                    tt("pool", OUTB[o][:, :n], RT1[i][:, :n], RT2[i][:, :n], ALU.add, [("RT1", i), ("RT2", i)], [("OUTB", o)])
                    dma("sp", dst[ot, :, q0:q0 + n], OUTB[o][:, :n], [("OUTB", o)], [(id(dst), ot, ch)])
            for ot in range(4):
                ps, rps = proj_fm(WQ, 1024 + ot * 128, hti, n, "WQ")
                plain_store(ps, rps, n, qfT_d[ot, :, q0:q0 + n], ("qfT_d", ot, ch))
            for t in range(ntile):
                sl = ch * 4 + t
                hres = [("HT", hti, half, t * 128) for half in range(2)]
                ps, rps = pf()
                items = [(ps[:, 0:8], HT[hti][:, kc, t * 128:(t + 1) * 128], WQ[:, kc, 1536:1544], kc == 0, kc == 7) for kc in range(8)]
                S.op("pe", mm_chain(items), ["WQ"] + hres, [rps])
                copy("dve", WIQ[:, sl * 8:(sl + 1) * 8], ps[:, 0:8], [rps], [("WIQ", sl)])

        S.barrier()

        A.off = P1_END
        SIDX = A.f32(TP)
        MNEGS = [A.bf(TP) for _ in range(2)]
        RL = [A.bf(512) for _ in range(8)]
        DW = A.bf(8 * 128)
        QI = A.bf(4 * 128).rearrange("p (t n) -> p t n", t=4)
        QA = A.bf(4 * 128).rearrange("p (t n) -> p t n", t=4)
        QF = A.bf(4 * 128).rearrange("p (t n) -> p t n", t=4)
        KS = [A.bf(4 * 512).rearrange("p (t n) -> p t n", t=4) for _ in range(2)]
        VS = [A.bf(4 * 8 * VW).rearrange("p (b n) -> p b n", b=4) for _ in range(2)]
        PT = [A.bf(1024) for _ in range(2)]
        BIASK = A.f32(NKB * 8)
        EFOX = A.f32(NKB * 8)
        BS = A.f32(64)
        HW = A.f32(NBIS)
        OSB = A.f32(8 * VW)
        RCP = A.f32(8)
        YB = A.bf(512)
        YT = A.bf(4 * 128)
        yaT_d = dscr("yaT_d", [4, 128, nq])
        yfT_d = dscr("yfT_d", [4, 128, nq])
        P2_END = A.off

        O_A = (PF[4], ("PF", 4))
        O_B = (PF[5], ("PF", 5))
        sc = {"k": 0}

        def attention(j, nk, kT_dram, v_dram, Qt, rQ, fox, yT_dram, tagname, hook):
            memset("dve", O_A[0][:, :], 0.0, [O_A[1]])
            memset("dve", O_B[0][:, :], 0.0, [O_B[1]])
            if hook is not None:
                hook()
            MNEG = MNEGS[j % 2]
            cbuf = {}

            def stage_a(kb):
                ch, kbl = divmod(kb, 4)
                if kbl == 0:
                    nb = min(4, nk - ch * 4)
                    w = nb * 128
                    k0 = ch * 512
                    cbuf[ch] = sc["k"] % 2
                    sc["k"] += 1
                    b = cbuf[ch]
                    dma("sp", KS[b][:, :, 0:w], kT_dram[:, :, k0:k0 + w].rearrange("t p k -> p t k"),
                        [(tagname + "kT", ot, ch) for ot in range(4)], [("KS", b)])
                    dma("sp", VS[b][:, 0:nb, :], v_dram[ch * 4:ch * 4 + nb].rearrange("b p c -> p b c"),
                        [(tagname + "v", kk) for kk in range(ch * 4, ch * 4 + nb)], [("VS", b)])
                    if fox:
                        vv = VS[b][:, 0:nb, :].rearrange("p b (h c) -> p b h c", h=8)
                        ee = EFOX[:, ch * 32:ch * 32 + nb * 8].rearrange("p (b h) -> p b h", h=8).unsqueeze(3).to_broadcast([128, nb, 8, VW])
                        tt("pool", vv, vv, ee, ALU.mult, [("VS", b), "EFOX"], [("VS", b)])
                b = cbuf[ch]
                tail = kb - (nk - 4)
                for par in range(2):
                    st_, rst = pf(0, 4)
                    items = []
                    if not fox:
                        items.append((st_[:, :], MNEG[:, kb * 128:(kb + 1) * 128], ident4, True, False))
                    elif tail >= 0:
                        items.append((st_[:, :], cmneg[:, tail * 128:(tail + 1) * 128], ident4, True, False))
                    masked = len(items) > 0
                    p0 = par * 64
                    for hh in range(4):
                        items.append((st_[:, hh * 128:(hh + 1) * 128],
                                      KS[b][p0:p0 + 64, hh, kbl * 128:(kbl + 1) * 128],
                                      Qt[p0:p0 + 64, hh, :], not masked, True))
                    reads = [("KS", b), rQ, "CB"]
                    if not fox:
                        reads.append(("MNEG", j % 2))
                    S.op("pe", mm_chain(items), reads, [rst])
                    act(PTcur(kb)[:, par * 512:(par + 1) * 512], st_[:, :], AF.Exp, [rst], [("PT", kb % 2, par)], scale=0.125)

            def stage_b(kb):
                ch, kbl = divmod(kb, 4)
                b = cbuf[ch]
                for hg, (O_, rO) in enumerate((O_A, O_B)):
                    items = []
                    for hh in range(4):
                        h = hg * 4 + hh
                        sl = (h % 2) * 4 + h // 2
                        items.append((O_[:, hh * VW:hh * VW + 65], PTcur(kb)[:, sl * 128:(sl + 1) * 128],
                                      VS[b][:, kbl, h * VW:h * VW + 65], False, False))
                    S.op("pe", mm_chain(items), [("PT", kb % 2, 0), ("PT", kb % 2, 1), ("VS", b)], [rO])

            stage_a(0)
            for kb in range(nk):
                if kb + 1 < nk:
                    stage_a(kb + 1)
                stage_b(kb)
            for hg, (O_, rO) in enumerate((O_A, O_B)):
                copy("dve", OSB[:, hg * 4 * VW:(hg + 1) * 4 * VW], O_[:, 0:4 * VW], [rO], [("OSB", hg)])
            OV = OSB.rearrange("p (h c) -> p h c", h=8)
            S.op("dve", lambda e: e.reciprocal(RCP[:, :], OV[:, :, 64]), [("OSB", 0), ("OSB", 1)], ["RCP"])
            for h in range(8):
                ts("dve", YB[:, h * 64:(h + 1) * 64], OV[:, h, 0:64], RCP[:, h:h + 1], None, ALU.mult, None,
                   [("OSB", 0), ("OSB", 1), "RCP"], [("YB", h)])
            tp_, rtp = pb()
            def fn(e, tp_=tp_):
                ins = None
                for kc in range(4):
                    ins = e.transpose(tp_[:, kc * 128:(kc + 1) * 128], YB[:, kc * 128:(kc + 1) * 128], identB)
                return ins
            S.op("pe", fn, [("YB", h) for h in range(8)] + ["CB"], [rtp])
            copy("act", YT[:, :], tp_[:, 0:512], [rtp], ["YT"])
            dma("sp", yT_dram[:, :, j * 128:(j + 1) * 128].rearrange("t p n -> p t n"), YT.rearrange("p (t n) -> p t n", t=4),
                ["YT"], [(tagname + "yT", j)])

        def kT_dram_name(x):
            return id(x)

        def PTcur(kb):
            return PT[kb % 2]

        def emit_idx(j):
            nk = 4 * j + 5
            n = nk * 128
            dma("sp", QI, qiT_d[:, :, j * 128:(j + 1) * 128].rearrange("t p n -> p t n"), (), ["QI"])
            for h in range(8):
                ts("dve", DW[:, h * 128:(h + 1) * 128], identB, WIQ[:, j * 8 + h:j * 8 + h + 1], None, ALU.mult, None,
                   ["CB", ("WIQ", j)], [("DW", h)])
            nch = (nk + 3) // 4
            for ch in range(nch):
                w = min(512, n - ch * 512)
                k0 = ch * 512
                for h in range(8):
                    p0 = (h % 2) * 64
                    z, rz = pf(0, 4)
                    S.op("pe", mm(z[:, :w], QI[p0:p0 + 64, h // 2, :], KIT[p0:p0 + 64, k0:k0 + w], True, True), ["QI", "KITall"], [rz])
                    if h % 2 == 0:
                        act(RL[h][:, :w], z[:, :w], AF.Relu, [rz], [("RL", h)])
                    else:
                        ts("dve", RL[h][:, :w], z[:, :w], 0.0, None, ALU.max, None, [rz], [("RL", h)])
                acc, racc = pf(4, 6)
                items = [(acc[:, :w], DW[:, h * 128:(h + 1) * 128], RL[h][:, :w], h == 0, h == 7) for h in range(8)]
                S.op("pe", mm_chain(items), [("DW", h) for h in range(8)] + [("RL", h) for h in range(8)], [racc])
                copy("dve", SIDX[:, k0:k0 + w], acc[:, :w], [racc], [("SIDX", ch)])

        def emit_bis(j):
            nk = 4 * j + 5
            n = nk * 128
            nch = (nk + 3) // 4
            MN = MNEGS[j % 2]
            rm = ("MNEG", j % 2)
            sres = [("SIDX", ch) for ch in range(nch)]
            S.op("dve", lambda e, n=n: e.tensor_reduce(BS[:, 0:1], SIDX[:, 0:n], AX.X, ALU.max, True), sres, [("BS", 0)])
            memset("dve", SIDX[:, 0:PAD], -1e30, [("SIDX", 0)])
            tt("dve", SIDX[:, n - 512:n], SIDX[:, n - 512:n], cmadd, ALU.add, sres + ["CF"], sres)
            ts("dve", BS[:, 2:3], BS[:, 0:1], -1.0, None, ALU.mult, None, [("BS", 0)], [("BS", 2)])
            ts("dve", BS[:, 1:2], BS[:, 0:1], 2.0, None, ALU.mult, None, [("BS", 0)], [("BS", 1)])
            ts("dve", HW[:, :], pow2, BS[:, 1:2], None, ALU.mult, None, ["CF", ("BS", 1)], ["HW"])
            for it in range(NBIS):
                tt("dve", BS[:, 3:4], BS[:, 2:3], HW[:, it:it + 1], ALU.add, [("BS", 2), "HW"], [("BS", 3)])
                ts("dve", MN[:, 0:n], SIDX[:, 0:n], BS[:, 3:4], 0.0, ALU.is_ge, ALU.add, sres + [("BS", 3)], [rm, ("BS", 4)], accum=BS[:, 4:5])
                stt("dve", BS[:, 5:6], BS[:, 4:5], 255.5, HW[:, it:it + 1], ALU.is_ge, ALU.mult, [("BS", 4), "HW"], [("BS", 5)])
                tt("dve", BS[:, 2:3], BS[:, 2:3], BS[:, 5:6], ALU.add, [("BS", 2), ("BS", 5)], [("BS", 2)])
            ts("dve", MN[:, 0:n], SIDX[:, 0:n], BS[:, 2:3], NEGM, ALU.is_lt, ALU.mult, sres + [("BS", 2)], [rm])

        nsl = nslot if stop_after >= 2 else 0
        if nsl:
            emit_idx(0)
            emit_bis(0)
        for j in range(nsl):
            nk = 4 * j + 5
            dma("sp", QA, qaT_d[:, :, j * 128:(j + 1) * 128].rearrange("t p n -> p t n"), (), ["QA"])
            dma("sp", QF, qfT_d[:, :, j * 128:(j + 1) * 128].rearrange("t p n -> p t n"), (), ["QF"])
            hook = None
            if j + 1 < nsl:
                emit_idx(j + 1)
                hook = (lambda jj=j + 1: emit_bis(jj))
            attention(j, nk, kaT_d, va_d, QA, "QA", False, yaT_d, "a", hook)
            BK = BIASK[:, 0:nk * 8].rearrange("p (k h) -> p k h", h=8)
            tt("dve", BK, CREF[:, j * 8:(j + 1) * 8].unsqueeze(1).to_broadcast([128, nk, 8]),
               CC[:, 0:nk * 8].rearrange("p (k h) -> p k h", h=8), ALU.subtract, [("CREF", j)] + cc_res, ["BIASK"])
            act(EFOX[:, 0:nk * 8], BIASK[:, 0:nk * 8], AF.Exp, ["BIASK"], ["EFOX"])
            attention(j, nk, kfT_d, vf_d, QF, "QF", True, yfT_d, "f", None)

        S.barrier()

        A.off = 0
        GATE = A.f32(nslot * 32)
        P3_KEEP = A.off
        WBA = A.bf(4 * 1024).rearrange("p (k n) -> p k n", k=4)
        WBF = A.bf(4 * 1024).rearrange("p (k n) -> p k n", k=4)
        WG = A.bf(8 * 2048).rearrange("p (k n) -> p k n", k=8)
        WO = A.bf(8 * 1024).rearrange("p (k n) -> p k n", k=8)
        WR = A.f32(8 * 36).rearrange("p (k n) -> p k n", k=8)
        H1TS = [A.bf(D).rearrange("p (k n) -> p k n", k=8) for _ in range(2)]
        YAT = [A.bf(512).rearrange("p (t n) -> p t n", t=4) for _ in range(2)]
        YFT = [A.bf(512).rearrange("p (t n) -> p t n", t=4) for _ in range(2)]
        HQ = [A.bf(1024).rearrange("p (k n) -> p k n", k=8) for _ in range(2)]
        XQ = [A.f32(D) for _ in range(2)]
        H32 = [A.f32(D) for _ in range(2)]
        GA = [A.f32(D) for _ in range(2)]
        GF = [A.f32(D) for _ in range(2)]
        MGB = [A.bf(D) for _ in range(2)]
        MGT = [A.bf(D).rearrange("p (k n) -> p k n", k=8) for _ in range(2)]
        H1P = [A.f32(D) for _ in range(2)]
        H1 = [A.f32(D) for _ in range(2)]
        H1B = [A.bf(D) for _ in range(2)]
        H1T32 = [A.f32(D).rearrange("p (k n) -> p k n", k=8) for _ in range(2)]
        RS = [A.f32(256) for _ in range(2)]

        dma("pool", WBA, wba_d.rearrange("(k p) n -> p k n", p=128), (), ["WBA"])
        dma("pool", WBF, wbf_d.rearrange("(k p) n -> p k n", p=128), (), ["WBF"])
        dma("pool", WG, wg_d.rearrange("(k p) n -> p k n", p=128), (), ["WG"])
        dma("pool", WO, wo_d.rearrange("(k p) n -> p k n", p=128), (), ["WO"])
        dma("sp", WR, wr_d.rearrange("(k p) n -> p k n", p=128), (), ["WR"])

        for i in range(nslot if stop_after >= 3 else 0):
            b = i % 2
            dma("sp", YAT[b], yaT_d[:, :, i * 128:(i + 1) * 128].rearrange("t p n -> p t n"), [("ayT", i)], [("YAT", b)])
            dma("sp", YFT[b], yfT_d[:, :, i * 128:(i + 1) * 128].rearrange("t p n -> p t n"), [("fyT", i)], [("YFT", b)])
            dma("sp", HQ[b], hqT_d[:, :, i * 128:(i + 1) * 128].rearrange("k p n -> p k n"),
                [("hqT_d", i // 4, kc) for kc in range(8)], [("HQ", b)])
            dma("sp", XQ[b][:, :], xq[i * 128:(i + 1) * 128, :], (), [("XQ", b)])
            layer_norm_tile(XQ[b], 0, 1, H32[b][:, :], "emb", ("XQ", b), [("H32", b)])
            for half in range(2):
                c0 = half * 512
                for (Wcol, GT, nm) in ((0, GA, "GA"), (1024, GF, "GF")):
                    ps, rps = pf()
                    items = [(ps[:, :], HQ[b][:, kc, :], WG[:, kc, Wcol + c0:Wcol + c0 + 512], kc == 0, kc == 7) for kc in range(8)]
                    S.op("pe", mm_chain(items), ["WG", ("HQ", b)], [rps])
                    act(GT[b][:, c0:c0 + 512], ps[:, :], AF.Sigmoid, [rps], [(nm, b, half)])
                ps, rps = pf()
                items = [(ps[:, :], YAT[b][:, kc, :], WBA[:, kc, c0:c0 + 512], kc == 0, kc == 3) for kc in range(4)]
                S.op("pe", mm_chain(items), ["WBA", ("YAT", b)], [rps])
                tt("dve", GA[b][:, c0:c0 + 512], ps[:, :], GA[b][:, c0:c0 + 512], ALU.mult, [rps, ("GA", b, half)], [("GA", b, half)])
                ps, rps = pf()
                items = [(ps[:, :], YFT[b][:, kc, :], WBF[:, kc, c0:c0 + 512], kc == 0, kc == 3) for kc in range(4)]
                S.op("pe", mm_chain(items), ["WBF", ("YFT", b)], [rps])
                tt("dve", GF[b][:, c0:c0 + 512], ps[:, :], GF[b][:, c0:c0 + 512], ALU.mult, [rps, ("GF", b, half)], [("GF", b, half)])
                tt("pool", MGB[b][:, c0:c0 + 512], GA[b][:, c0:c0 + 512], GF[b][:, c0:c0 + 512], ALU.add,
                   [("GA", b, half), ("GF", b, half)], [("MGB", b, half)])
            for half in range(2):
                tp_, rtp = pb()
                def fn(e, tp_=tp_, b=b, half=half):
                    ins = None
                    for kc in range(4):
                        k = half * 4 + kc
                        ins = e.transpose(tp_[:, kc * 128:(kc + 1) * 128], MGB[b][:, k * 128:(k + 1) * 128], identB)
                    return ins
                S.op("pe", fn, [("MGB", b, 0), ("MGB", b, 1), "CB"], [rtp])
                copy("act", MGT[b][:, half * 4:half * 4 + 4, :], tp_[:, 0:512].rearrange("p (k n) -> p k n", k=4), [rtp], [("MGT", b, half)])
            for half in range(2):
                c0 = half * 512
                ps, rps = pf()
                items = [(ps[:, :], MGT[b][:, kc, :], WO[:, kc, c0:c0 + 512], kc == 0, kc == 7) for kc in range(8)]
                S.op("pe", mm_chain(items), ["WO", ("MGT", b, 0), ("MGT", b, 1)], [rps])
                stt("dve", H1P[b][:, c0:c0 + 512], H32[b][:, c0:c0 + 512], ALPHA, ps[:, :], ALU.mult, ALU.add, [("H32", b), rps], [("H1P", b, half)])
            layer_norm_tile(H1P[b], 2, 3, H1[b][:, :], "ln1", [("H1P", b, 0), ("H1P", b, 1)], [("H1", b)])
            dma("sp", h1_d[i * 128:(i + 1) * 128, :], H1[b][:, :], [("H1", b)], [("h1_d", i)])
            copy("act", H1B[b][:, :], H1[b][:, :], [("H1", b)], [("H1B", b)])
            for half in range(2):
                tp_, rtp = pb()
                def fn(e, tp_=tp_, b=b, half=half):
                    ins = None
                    for kc in range(4):
                        k = half * 4 + kc
                        ins = e.transpose(tp_[:, kc * 128:(kc + 1) * 128], H1B[b][:, k * 128:(k + 1) * 128], identB)
                    return ins
                S.op("pe", fn, [("H1B", b), "CB"], [rtp])
                copy("act", H1TS[b][:, half * 4:half * 4 + 4, :], tp_[:, 0:512].rearrange("p (k n) -> p k n", k=4), [rtp], [("H1TS", b, half)])
            dma("sp", h1T_d[:, :, i * 128:(i + 1) * 128].rearrange("k p n -> p k n"), H1TS[b], [("H1TS", b, 0), ("H1TS", b, 1)], [("h1T_d", i)])
            for half in range(2):
                ps, rps = pf()
                def fn(e, ps=ps, b=b, half=half):
                    ins = None
                    for kc in range(4):
                        k = half * 4 + kc
                        ins = e.transpose(ps[:, kc * 128:(kc + 1) * 128], H1[b][:, k * 128:(k + 1) * 128], identF)
                    return ins
                S.op("pe", fn, [("H1", b), "CF"], [rps])
                copy("dve", H1T32[b][:, half * 4:half * 4 + 4, :], ps[:, :].rearrange("p (k n) -> p k n", k=4), [rps], [("H1T32", b, half)])
            ps, rps = pf()
            items = [(ps[:, 0:36], H1T32[b][:, kc, :], WR[:, kc, :], kc == 0, kc == 7) for kc in range(8)]
            S.op("pe", mm_chain(items), ["WR", ("H1T32", b, 0), ("H1T32", b, 1)], [rps])
            R = RS[b]
            rR = ("RS", b)
            k_ = [0]

            def rr_(nm):
                return ("RS", b, nm)
            tt("dve", R[:, 0:36], ps[:, 0:36], BRT[:, :], ALU.add, [rps, "BRT"], [rr_("lg")])
            S.op("dve", lambda e, R=R: e.tensor_reduce(R[:, 40:41], R[:, 0:4], AX.X, ALU.max), [rr_("lg")], [rr_("gmax")])
            ts("dve", R[:, 44:48], R[:, 0:4], R[:, 40:41], None, ALU.is_ge, None, [rr_("lg"), rr_("gmax")], [rr_("goh")])
            ts("dve", R[:, 41:42], R[:, 40:41], -1.0, None, ALU.mult, None, [rr_("gmax")], [rr_("ngmax")])
            act(R[:, 48:52], R[:, 0:4], AF.Exp, [rr_("lg"), rr_("ngmax")], [rr_("gexp"), rr_("gsum")], bias=R[:, 41:42], scale=1.0, accum=R[:, 42:43])
            S.op("dve", lambda e, R=R: e.reciprocal(R[:, 43:44], R[:, 42:43]), [rr_("gsum")], [rr_("pg")])
            ts("dve", R[:, 52:56], R[:, 44:48], -1.0, 1e30, ALU.add, ALU.mult, [rr_("goh")], [rr_("gpen")])
            tt("dve", R[:, 64:96].rearrange("p (g e) -> p g e", g=4), R[:, 4:36].rearrange("p (g e) -> p g e", g=4),
               R[:, 52:56].unsqueeze(2).to_broadcast([128, 4, 8]), ALU.add, [rr_("lg"), rr_("gpen")], [rr_("em")])
            S.op("dve", lambda e, R=R: e.tensor_reduce(R[:, 56:57], R[:, 64:96], AX.X, ALU.max), [rr_("em")], [rr_("m1")])
            ts("dve", R[:, 96:128], R[:, 64:96], R[:, 56:57], None, ALU.is_ge, None, [rr_("em"), rr_("m1")], [rr_("oh1")])
            stt("dve", R[:, 128:160], R[:, 96:128], -1e30, R[:, 64:96], ALU.mult, ALU.add, [rr_("oh1"), rr_("em")], [rr_("em2")])
            S.op("dve", lambda e, R=R: e.tensor_reduce(R[:, 57:58], R[:, 128:160], AX.X, ALU.max), [rr_("em2")], [rr_("m2")])
            ts("dve", R[:, 160:192], R[:, 128:160], R[:, 57:58], None, ALU.is_ge, None, [rr_("em2"), rr_("m2")], [rr_("oh2")])
            tt("dve", R[:, 58:59], R[:, 57:58], R[:, 56:57], ALU.subtract, [rr_("m1"), rr_("m2")], [rr_("dm")])
            act(R[:, 59:60], R[:, 58:59], AF.Exp, [rr_("dm")], [rr_("edm")])
            ts("dve", R[:, 60:61], R[:, 59:60], 1.0, None, ALU.add, None, [rr_("edm")], [rr_("den")])
            S.op("dve", lambda e, R=R: e.reciprocal(R[:, 61:62], R[:, 60:61]), [rr_("den")], [rr_("w1")])
            tt("dve", R[:, 62:63], R[:, 61:62], R[:, 43:44], ALU.mult, [rr_("w1"), rr_("pg")], [rr_("g1")])
            tt("dve", R[:, 63:64], R[:, 43:44], R[:, 62:63], ALU.subtract, [rr_("pg"), rr_("g1")], [rr_("g2")])
            ts("dve", R[:, 192:224], R[:, 96:128], R[:, 62:63], None, ALU.mult, None, [rr_("oh1"), rr_("g1")], [rr_("t1")])
            stt("dve", GATE[:, i * 32:(i + 1) * 32], R[:, 160:192], R[:, 63:64], R[:, 192:224], ALU.mult, ALU.add,
                [rr_("oh2"), rr_("g2"), rr_("t1")], [("GATE", i)])

        S.barrier()

        A.off = P3_KEEP
        H1T = A.bf(8 * nq).rearrange("p (k n) -> p k n", k=8)
        ACC = A.f32(nslot * D)
        dma("sp", H1T, h1T_d.rearrange("k p n -> p k n"), [("h1T_d", i) for i in range(nslot)], ["H1Tall"])
        WGT = [A.bf(8 * 256).rearrange("p (k n) -> p k n", k=8) for _ in range(2)]
        WUP = [A.bf(8 * 256).rearrange("p (k n) -> p k n", k=8) for _ in range(2)]
        WDN = [A.bf(2 * 1024).rearrange("p (k n) -> p k n", k=2) for _ in range(2)]
        SG = [A.f32(512) for _ in range(2)]
        AT = [A.bf(2 * 512).rearrange("p (f n) -> p f n", f=2) for _ in range(2)]
        FX = [A.f32(D) for _ in range(2)]
        FO = [A.f32(D) for _ in range(2)]

        memset("pool", ACC[:, :], 0.0, ["ACCall"])
        nchq = max(1, nq // 512)
        ac = {"n": 0}
        for ex in range(nexp if stop_after >= 4 else 0):
            b = ex % 2
            dma("pool", WGT[b], wgate_d[ex].rearrange("(k p) n -> p k n", p=128), (), [("WGT", b)])
            dma("pool", WUP[b], wup_d[ex].rearrange("(k p) n -> p k n", p=128), (), [("WUP", b)])
            dma("pool", WDN[b], wdn_d[ex].rearrange("(k p) n -> p k n", p=128), (), [("WDN", b)])
            for ch in range(nchq):
                n = min(512, nq)
                q0 = ch * 512
                a = ac["n"] % 2
                ac["n"] += 1
                hres = ["H1Tall"]
                for ft in range(2):
                    pg_, rpg = pf()
                    items = [(pg_[:, :n], WGT[b][:, kc, ft * 128:(ft + 1) * 128], H1T[:, kc, q0:q0 + n], kc == 0, kc == 7) for kc in range(8)]
                    S.op("pe", mm_chain(items), [("WGT", b)] + hres, [rpg])
                    pu_, rpu = pf()
                    items = [(pu_[:, :n], WUP[b][:, kc, ft * 128:(ft + 1) * 128], H1T[:, kc, q0:q0 + n], kc == 0, kc == 7) for kc in range(8)]
                    S.op("pe", mm_chain(items), [("WUP", b)] + hres, [rpu])
                    s = (ac["n"] + ft) % 2
                    act(SG[s][:, :n], pg_[:, :n], AF.Silu, [rpg], [("SG", s)])
                    tt("dve", AT[a][:, ft, :n], SG[s][:, :n], pu_[:, :n], ALU.mult, [("SG", s), rpu], [("AT", a, ft)])
                for t in range(n // 128):
                    i = ch * 4 + t
                    for half in range(2):
                        c0 = half * 512
                        py, rpy = pf()
                        items = [(py[:, :], AT[a][:, ft, t * 128:(t + 1) * 128], WDN[b][:, ft, c0:c0 + 512], ft == 0, ft == 1) for ft in range(2)]
                        S.op("pe", mm_chain(items), [("WDN", b), ("AT", a, 0), ("AT", a, 1)], [rpy])
                        stt("dve", ACC[:, i * D + c0:i * D + c0 + 512], py[:, :], GATE[:, i * 32 + ex:i * 32 + ex + 1],
                            ACC[:, i * D + c0:i * D + c0 + 512], ALU.mult, ALU.add,
                            [rpy, ("GATE", i), "ACCall", ("ACC", i, half)], [("ACC", i, half)])
        for i in range(nslot):
            b = i % 2
            dma("sp", FX[b][:, :], h1_d[i * 128:(i + 1) * 128, :], [("h1_d", i)], [("FX", b)])
            stt("dve", FX[b][:, :], FX[b][:, :], ALPHA, ACC[:, i * D:(i + 1) * D], ALU.mult, ALU.add,
                [("FX", b), ("ACC", i, 0), ("ACC", i, 1), "ACCall"], [("FX", b)])
            layer_norm_tile(FX[b], 4, 5, FO[b][:, :], "ln2", ("FX", b), [("FO", b)])
            dma("sp", out_d[i * 128:(i + 1) * 128, :], FO[b][:, :], [("FO", b)], [("out", i)])
        S.finish()
        S.prepare()

        with nc.Block() as block:
            @block.tensor
            def _(e):
                S.replay("pe", e, sems)

            @block.scalar
            def _(e):
                S.replay("act", e, sems)

            @block.vector
            def _(e):
                S.replay("dve", e, sems)

            @block.gpsimd
            def _(e):
                S.replay("pool", e, sems)

            @block.sync
            def _(e):
                S.replay("sp", e, sems)
    return nc


def _consts(r):
    bf = ml_dtypes.bfloat16
    ident = np.eye(128, dtype=np.float32)
    k = np.arange(128)
    utri = (k[:, None] <= k[None, :]).astype(np.float32)
    ones = np.ones((128, 128), np.float32)
    q = np.arange(128)[:, None]
    kk = np.arange(128)[None, :]
    tri_ok = kk <= q
    cm = np.zeros((128, 4, 128), bool)
    for t in range(4):
        if t < r:
            cm[:, t, :] = True
        elif t == r:
            cm[:, t, :] = tri_ok
    cmadd = np.where(cm, 0.0, -1e30).astype(np.float32).reshape(128, 512)
    cmneg = np.where(cm, 0.0, NEGM).astype(np.float32).reshape(128, 512)
    selr = np.zeros((128, 4), np.float32)
    selr[:, r] = 1.0
    pow2 = np.tile((2.0 ** -(np.arange(NBIS) + 1.0)).astype(np.float32)[None], (128, 1))
    padmask = (np.arange(128) >= PAD).astype(np.float32)[:, None]
    cf = np.concatenate([ident, utri, ones, cmadd, selr, pow2, padmask], axis=1).astype(np.float32)
    P = np.zeros((128, 128), np.float32)
    for hb in (0, 64):
        for d in range(8):
            P[hb + d + 8, hb + d] = -1.0
            P[hb + d, hb + d + 8] = 1.0
    cb = np.concatenate([ident, np.tile(ident, (1, 4)), P, cmneg], axis=1).astype(bf)
    return cf, cb


def _rope_tables(pos):
    half = 8
    inv = (500000.0 ** (-np.arange(half, dtype=np.float32) / half)).astype(np.float32)
    ang = pos.astype(np.float32)[None, :] * inv[:, None]
    c8, s8 = np.cos(ang).astype(np.float32), np.sin(ang).astype(np.float32)
    n = pos.shape[0]
    C = np.ones((128, n), np.float32)
    Sn = np.zeros((128, n), np.float32)
    for hb in (0, 64):
        C[hb:hb + 8] = c8
        C[hb + 8:hb + 16] = c8
        Sn[hb:hb + 8] = s8
        Sn[hb + 8:hb + 16] = s8
    return C, Sn


_NC_CACHE = {}


def kernel(x, meta_tokens, emb_ln_g, emb_ln_b, w_in, b_forget, kv_norm_g, w_kv_up, w_branch_dsa,
           w_branch_fox, w_out, ln1_g, ln1_b, w_router_group, b_router_group, w_router_expert,
           b_router_expert, w_gate, w_up, w_down, ln2_g, ln2_b):
    f = lambda a: np.ascontiguousarray(np.asarray(a, dtype=np.float32))
    x = f(x)
    w_in0 = f(w_in)[0]
    offs = np.cumsum([0, 512, 256, 512, 64, 8, 512, 512, 512, 8, 1024, 1024])
    seg = lambda i: w_in0[:, offs[i]:offs[i + 1]]
    q_a, c_kv, q_i, k_i, w_i, q_f, k_f, v_f, f_lg, g_a, g_f = [seg(i) for i in range(11)]
    wk = f(np.concatenate([c_kv, k_i, k_i, k_f, v_f, f_lg], axis=1))
    wq = f(np.concatenate([q_a, q_i, q_f, w_i], axis=1))
    wg = f(np.concatenate([g_a, g_f], axis=1))
    rep = lambda v: f(np.tile(np.asarray(v, np.float32).reshape(1, -1), (128, 1)))
    lnp = f(np.concatenate([rep(emb_ln_g), rep(emb_ln_b), rep(ln1_g[0]), rep(ln1_b[0]), rep(ln2_g[0]), rep(ln2_b[0])], axis=1))
    wr = f(np.concatenate([f(w_router_group)[0], f(w_router_expert)[0]], axis=1))
    brt = rep(np.concatenate([f(b_router_group)[0], f(b_router_expert)[0]]))
    common = dict(
        wk=wk, wq=wq, wg=wg, wkv=f(w_kv_up)[0], wba=f(w_branch_dsa)[0], wbf=f(w_branch_fox)[0], wo=f(w_out)[0],
        wr=wr, wgate=f(w_gate)[0][:NEXP_RUN], wup=f(w_up)[0][:NEXP_RUN], wdn=f(w_down)[0][:NEXP_RUN], lnp=lnp, kvg=rep(kv_norm_g[0]),
        bfg=rep(b_forget[0]), brt=brt,
    )
    kpos = np.arange(TP) - PAD
    ck, sk = _rope_tables(kpos)
    in_maps = []
    own = []
    for c in range(8):
        b, r = c // 4, c % 4
        xkc = np.zeros((TP, D), np.float32)
        xkc[PAD:PAD + NMETA] = f(meta_tokens)
        xkc[PAD + NMETA:] = x[b]
        blocks = [4 * j + r for j in range(NSLOT)]
        rows = np.concatenate([np.arange(bl * 128, (bl + 1) * 128) for bl in blocks])
        own.append((b, rows))
        cq, sq = _rope_tables(rows + NMETA)
        cf, cb = _consts(r)
        m = dict(common)
        m.update(xk=xkc, xq=f(x[b][rows]), ck=ck, sk=sk, cq=cq, sq=sq, cf32=cf, cbf=cb)
        in_maps.append(m)
    if "nc" not in _NC_CACHE:
        _NC_CACHE["nc"] = build()
    nc = _NC_CACHE["nc"]
    res = run_bass_kernel_spmd(nc, in_maps, core_ids=list(range(8)))
    out = np.zeros((2, SEQ, D), np.float32)
    for c in range(8):
        b, rows = own[c]
        out[b, rows] = np.asarray(res.results[c]["out"], dtype=np.float32)
    return out
```

```python
import os
import numpy as np
import ml_dtypes
from contextlib import ExitStack
import concourse.bass as bass
import concourse.mybir as mybir
from concourse.bass_utils import run_bass_kernel_spmd

F32 = mybir.dt.float32
BF16 = mybir.dt.bfloat16
AF = mybir.ActivationFunctionType
ALU = mybir.AluOpType
AX = mybir.AxisListType

D = 1024
SEQ = 8192
NMETA = 16
PAD = 112
TP = SEQ + NMETA + PAD
NKB = TP // 128
NSLOT = 16
NQ = NSLOT * 128
NBIS = 20
ALPHA = 2.0 ** 0.25
NEGM = -30000.0
NEXP = 32
VW = 68
NEXP_RUN = 32

WK_COLS = 256 + 128 + 512 + 512 + 8
WQ_COLS = 512 * 3 + 8


class Sched:
    ENG = ("pe", "act", "dve", "pool", "sp")
    NDMA = 8

    def __init__(self):
        self.ops = {e: [] for e in self.ENG}
        self.count = {e: 0 for e in self.ENG}
        self.seen = {e: {} for e in self.ENG}
        self.res = {}
        self.dma_rr = {e: 0 for e in self.ENG}
        self.dma_val = {}

    def op(self, eng, fn, reads=(), writes=(), dma=False):
        self.nops = getattr(self, "nops", 0) + 1
        if self.nops > getattr(self, "maxops", 10 ** 9):
            return None
        need = {}

        def add(tok):
            if tok is None:
                return
            k, v = tok
            if need.get(k, 0) < v:
                need[k] = v

        reads = list(reads)
        writes = list(writes)
        for r in list(reads):
            if isinstance(r, tuple) and r and r[0] in ("PF", "PB") and r not in writes:
                writes.append(r)
        for r in reads:
            st = self.res.get(r)
            if st:
                add(st["w"])
        for w in writes:
            st = self.res.get(w)
            if st:
                add(st["w"])
                for k, v in st["r"].items():
                    add((k, v))
        waits = []
        for k, v in need.items():
            if eng == "pe" and k == "pe":
                continue
            if self.seen[eng].get(k, 0) >= v:
                continue
            waits.append((k, v))
            self.seen[eng][k] = v
        if dma:
            idx = self.dma_rr[eng]
            self.dma_rr[eng] = (idx + 1) % self.NDMA
            key = ("dma", eng, idx)
            prev = self.dma_val.get(key, 0)
            if prev > 0 and self.seen[eng].get(key, 0) < prev:
                waits.append((key, prev))
                self.seen[eng][key] = prev
            val = prev + 16
            self.dma_val[key] = val
            tok = (key, val)
        else:
            self.count[eng] += 1
            tok = (eng, self.count[eng])
        self.ops[eng].append((waits, fn, tok))
        for r in reads:
            st = self.res.get(r)
            if not st:
                st = {"w": None, "r": {}}
                self.res[r] = st
            if st["r"].get(tok[0], 0) < tok[1]:
                st["r"][tok[0]] = tok[1]
        for w in writes:
            self.res[w] = {"w": tok, "r": {}}
        return tok

    def barrier(self):
        toks = [(e, self.count[e]) for e in self.ENG if self.count[e] > 0]
        toks += [(k, v) for k, v in self.dma_val.items()]
        for e in self.ENG:
            waits = []
            for k, v in toks:
                if k == e and e == "pe":
                    continue
                if self.seen[e].get(k, 0) >= v:
                    continue
                waits.append((k, v))
                self.seen[e][k] = v
            if waits:
                self.ops[e].append((waits, None, None))

    def finish(self):
        waits = [(k, v) for k, v in self.dma_val.items() if self.seen["sp"].get(k, 0) < v]
        self.ops["sp"].append((waits, None, None))

    def prepare(self):
        import bisect as _b
        sig = {e: set() for e in self.ENG}
        for e in self.ENG:
            for waits, fn, tok in self.ops[e]:
                for k, v in waits:
                    if k in sig and k != e:
                        sig[k].add(v)
        self.sig = {e: sorted(v) for e, v in sig.items()}
        self._b = _b

    def rank(self, k, v):
        lst = self.sig[k]
        i = self._b.bisect_right(lst, v)
        assert i > 0 and lst[i - 1] == v, (k, v)
        return i

    def replay(self, name, e, sems):
        sigset = set(self.sig[name])
        for waits, fn, tok in self.ops[name]:
            for k, v in waits:
                if k == name:
                    e.drain()
                elif k in self.sig:
                    e.wait_ge(sems[k], self.rank(k, v))
                else:
                    e.wait_ge(sems[k], v)
            if fn is None:
                continue
            ins = fn(e)
            if tok[0][0] == "dma":
                ins.then_inc(sems[tok[0]], 16)
            elif tok[1] in sigset:
                ins.then_inc(sems[tok[0]], 1)


def build(nslot=NSLOT, nexp=NEXP, debug=False, nkb=NKB, stop_after=9):
    nc = bass.Bass("TRN2", target_bir_lowering=False)
    S = Sched()
    import os
    S.maxops = int(os.environ.get("MAXOPS", 10 ** 9))
    nq = nslot * 128

    def din(name, shape, dt=F32):
        return nc.dram_tensor(name, list(shape), dt, kind="ExternalInput").ap()

    def dscr(name, shape, dt=BF16):
        return nc.dram_tensor(name, list(shape), dt, kind="Internal").ap()

    xk = din("xk", [TP, D])
    xq = din("xq", [nq, D])
    wk_d = din("wk", [D, WK_COLS])
    wq_d = din("wq", [D, WQ_COLS])
    wg_d = din("wg", [D, 2048])
    wkv_d = din("wkv", [256, 1024])
    wba_d = din("wba", [512, 1024])
    wbf_d = din("wbf", [512, 1024])
    wo_d = din("wo", [D, D])
    wr_d = din("wr", [D, 36])
    wgate_d = din("wgate", [nexp, D, 256])
    wup_d = din("wup", [nexp, D, 256])
    wdn_d = din("wdn", [nexp, 256, D])
    lnp_d = din("lnp", [128, 6 * D])
    kvg_d = din("kvg", [128, 256])
    bfg_d = din("bfg", [128, 8])
    brt_d = din("brt", [128, 36])
    ck_d = din("ck", [128, TP])
    sk_d = din("sk", [128, TP])
    cq_d = din("cq", [128, nq])
    sq_d = din("sq", [128, nq])
    cf32_d = din("cf32", [128, 128 * 3 + 512 + 4 + NBIS + 1])
    cbf_d = din("cbf", [128, 128 + 512 + 128 + 512], BF16)
    out_d = nc.dram_tensor("out", [nq, D], F32, kind="ExternalOutput").ap()

    kaT_d = dscr("kaT_d", [4, 128, TP])
    kfT_d = dscr("kfT_d", [4, 128, TP])
    va_d = dscr("va_d", [NKB, 128, 8 * VW])
    vf_d = dscr("vf_d", [NKB, 128, 8 * VW])
    qaT_d = dscr("qaT_d", [4, 128, nq])
    qiT_d = dscr("qiT_d", [4, 128, nq])
    qfT_d = dscr("qfT_d", [4, 128, nq])
    hqT_d = dscr("hqT_d", [8, 128, nq])
    h1_d = dscr("h1_d", [nq, D], F32)
    h1T_d = dscr("h1T_d", [8, 128, nq])

    es = ExitStack()
    with es:
        def sb(name, cols, dt=F32, parts=128):
            return es.enter_context(nc.sbuf_tensor(name, [parts, cols], dt))

        CF = sb("CF", 128 * 3 + 512 + 4 + NBIS + 1)
        CB = sb("CB", 128 + 512 + 128 + 512, BF16)
        LNP = sb("LNP", 6 * D)
        KVG = sb("KVG", 256)
        BFG = sb("BFG", 8)
        BRT = sb("BRT", 36)
        identF = CF[:, 0:128]
        utri = CF[:, 128:256]
        onesF = CF[:, 256:384]
        cmadd = CF[:, 384:896]
        selr = CF[:, 896:900]
        pow2 = CF[:, 900:900 + NBIS]
        padmask = CF[:, 900 + NBIS:901 + NBIS]
        identB = CB[:, 0:128]
        ident4 = CB[:, 128:640]
        ropeP = CB[:, 640:768]
        cmneg = CB[:, 768:1280]

        ARENA_COLS = 38 * 1024
        ARENA = sb("ARENA", ARENA_COLS)
        ARENA_B = ARENA[:, :].bitcast(BF16)

        class Bump:
            def __init__(self):
                self.off = 0

            def f32(self, cols):
                o = self.off // 4
                self.off += cols * 4
                assert self.off <= ARENA_COLS * 4, self.off
                return ARENA[:, o:o + cols]

            def bf(self, cols):
                cols2 = (cols + 1) // 2 * 2
                o = self.off // 2
                self.off += cols2 * 2
                assert self.off <= ARENA_COLS * 4, self.off
                return ARENA_B[:, o:o + cols]

        PF = [es.enter_context(nc.psum_tensor(f"PF{i}", [128, 512], F32)) for i in range(6)]
        PB = [es.enter_context(nc.psum_tensor(f"PB{i}", [128, 1024], BF16)) for i in range(2)]
        rr = {"n": 0, "b": 0}

        def pf(lo=0, hi=6):
            i = lo + rr["n"] % (hi - lo)
            rr["n"] += 1
            return PF[i], ("PF", i)

        def pb():
            i = rr["b"] % 2
            rr["b"] += 1
            return PB[i], ("PB", i)

        sems = {}
        for e in Sched.ENG:
            sems[e] = es.enter_context(nc.semaphore(f"s_{e}"))
        for e in ("sp", "pool", "act"):
            for i in range(Sched.NDMA):
                sems[("dma", e, i)] = es.enter_context(nc.semaphore(f"d_{e}{i}"))

        def dma(q, out, in_, reads, writes):
            S.op(q, lambda e, o=out, i=in_: e.dma_start(out=o, in_=i), reads, writes, dma=True)

        def mm(out, lhsT, rhs, start, stop, skip=True):
            return lambda e: e.matmul(out, lhsT, rhs, start=start, stop=stop, skip_group_check=skip)

        def mm_chain(items):
            def fn(e):
                ins = None
                for (o, l, r, st, sp_) in items:
                    ins = e.matmul(o, l, r, start=st, stop=sp_, skip_group_check=True)
                return ins
            return fn

        def act(out, in_, func, reads, writes, bias=0.0, scale=1.0, accum=None):
            if accum is None:
                S.op("act", lambda e: e.activation(out=out, in_=in_, func=func, bias=bias, scale=scale), reads, writes)
            else:
                S.op("act", lambda e: e.activation(out=out, in_=in_, func=func, bias=bias, scale=scale, accum_out=accum), reads, writes)

        def tt(eng, out, in0, in1, op, reads, writes):
            S.op(eng, lambda e: e.tensor_tensor(out=out, in0=in0, in1=in1, op=op), reads, writes)

        def ts(eng, out, in0, s1, s2, op0, op1, reads, writes, accum=None):
            if accum is None:
                if op1 is None:
                    S.op(eng, lambda e: e.tensor_scalar(out, in0, s1, None, op0), reads, writes)
                else:
                    S.op(eng, lambda e: e.tensor_scalar(out, in0, s1, s2, op0, op1), reads, writes)
            else:
                S.op(eng, lambda e: e.tensor_scalar(out, in0, s1, s2, op0, op1, accum), reads, writes)

        def stt(eng, out, in0, scalar, in1, op0, op1, reads, writes):
            S.op(eng, lambda e: e.scalar_tensor_tensor(out=out, in0=in0, scalar=scalar, in1=in1, op0=op0, op1=op1), reads, writes)

        def copy(eng, out, in_, reads, writes):
            if eng == "act":
                S.op("act", lambda e: e.copy(out, in_), reads, writes)
            else:
                S.op(eng, lambda e: e.tensor_copy(out, in_), reads, writes)

        def memset(eng, ap, val, writes):
            S.op(eng, lambda e: e.memset(ap, val), (), writes)

        def transpose_to(out_ps, in_sb, ident):
            return lambda e: e.transpose(out_ps, in_sb, ident)

        dma("sp", CF[:, :], cf32_d, (), ["CF"])
        dma("sp", CB[:, :], cbf_d, (), ["CB"])
        dma("sp", LNP[:, :], lnp_d, (), ["LNP"])
        dma("sp", KVG[:, :], kvg_d, (), ["KVG"])
        dma("sp", BFG[:, :], bfg_d, (), ["BFG"])
        dma("sp", BRT[:, :], brt_d, (), ["BRT"])
        CONST = ["CF", "CB", "LNP", "KVG", "BFG", "BRT"]

        uid = {"n": 0}

        def layer_norm_tile(x32, gcol, bcol, out_ap, tag, rx, wout, eps=1e-5, prescale=None):
            uid["n"] += 1
            u = uid["n"] % 2
            st = LNS[u]
            rs = ("LNS", u)
            rx = rx if isinstance(rx, list) else [rx]
            S.op("dve", lambda e: e.bn_stats(st[:, 0:6], x32[:, 0:512]), rx, [rs])
            S.op("dve", lambda e: e.bn_stats(st[:, 6:12], x32[:, 512:1024]), rx, [(rs, 1)])
            S.op("dve", lambda e: e.bn_aggr(st[:, 12:14], st[:, 0:12]), [rs, (rs, 1)], [(rs, 2)])
            ts("dve", st[:, 11:12], st[:, 13:14], eps, None, ALU.add, None, [(rs, 2), (rs, 1)], [(rs, 5)])
            act(st[:, 10:11], st[:, 11:12], AF.Ln, [(rs, 5)], [(rs, 6)])
            act(st[:, 14:15], st[:, 10:11], AF.Exp, [(rs, 6)], [(rs, 3)], scale=-0.5)
            stt("dve", st[:, 15:16], st[:, 12:13], -1.0, st[:, 14:15], ALU.mult, ALU.mult, [(rs, 2), (rs, 3)], [(rs, 4)])
            xn = LNX[u]
            rxn = ("LNX", u)
            act(xn[:, :], x32, AF.Identity, rx + [(rs, 3), (rs, 4)], [rxn], bias=st[:, 15:16], scale=st[:, 14:15])
            tt("dve", xn[:, :], xn[:, :], LNP[:, gcol * D:(gcol + 1) * D], ALU.mult, [rxn, "LNP"], [rxn])
            tt("pool", out_ap, xn[:, :], LNP[:, bcol * D:(bcol + 1) * D], ALU.add, [rxn, "LNP"], wout)

        LNS = [sb(f"LNS{i}", 16) for i in range(2)]
        LNX = [sb(f"LNX{i}", D) for i in range(2)]

        A = Bump()
        KIT = A.bf(TP)
        LOGF = A.f32(NKB * 8)
        WIQ = A.f32(nslot * 8)
        CC = A.f32(NKB * 8)
        PI = A.f32(NKB * 8)
        CREF = A.f32(nslot * 8)
        P1_END = A.off
        WKs = A.bf(8 * WK_COLS)
        WK = WKs.rearrange("p (k n) -> p k n", k=8)
        WQs = A.bf(8 * WQ_COLS)
        WQ = WQs.rearrange("p (k n) -> p k n", k=8)
        WKVs = A.bf(2 * 1024)
        WKV = WKVs.rearrange("p (k n) -> p k n", k=2)
        XT = [A.f32(D) for _ in range(2)]
        HB = [A.bf(D) for _ in range(2)]
        HT = [A.bf(8 * 512).rearrange("p (k n) -> p k n", k=8) for _ in range(2)]
        FMB = [A.bf(512) for _ in range(2)]
        RT1 = [A.f32(512) for _ in range(2)]
        RT2 = [A.f32(512) for _ in range(2)]
        CS = [A.f32(1024) for _ in range(2)]
        OUTB = [A.bf(512) for _ in range(3)]
        VST = [A.bf(8 * VW) for _ in range(2)]
        CKV32 = [A.f32(256) for _ in range(4)]
        CKVB = [A.bf(256) for _ in range(4)]
        CKVT = [A.bf(2 * 512).rearrange("p (k n) -> p k n", k=2) for _ in range(2)]
        SM = [A.f32(32) for _ in range(4)]

        dma("pool", WK, wk_d.rearrange("(k p) n -> p k n", p=128), (), ["WK"])
        dma("pool", WQ, wq_d.rearrange("(k p) n -> p k n", p=128), (), ["WQ"])
        dma("pool", WKV, wkv_d.rearrange("(k p) n -> p k n", p=128), (), ["WKV"])

        cnt = {"fm": 0, "ob": 0, "vs": 0, "ck": 0}
        if nkb < NKB:
            memset("pool", LOGF[:, :], 0.0, [("LOGF", kb) for kb in range(NKB)])

        def rope_store(ps, rps, n, cs_ap, rcs, dst_dram, rdst, dup_dst=None, rdup=None):
            i = cnt["fm"] % 2
            cnt["fm"] += 1
            copy("act", FMB[i][:, :n], ps[:, :n], [rps], [("FMB", i)])
            tt("dve", RT1[i][:, :n], ps[:, :n], cs_ap[:, 0:n], ALU.mult, [rps, rcs], [("RT1", i)])
            pp, rpp = pf()
            S.op("pe", mm(pp[:, :n], ropeP, FMB[i][:, :n], True, True), ["CB", ("FMB", i)], [rpp])
            tt("dve", RT2[i][:, :n], pp[:, :n], cs_ap[:, 512:512 + n], ALU.mult, [rpp, rcs], [("RT2", i)])
            o = cnt["ob"] % 3
            cnt["ob"] += 1
            if dup_dst is not None:
                tt("pool", dup_dst, RT1[i][:, :n], RT2[i][:, :n], ALU.add, [("RT1", i), ("RT2", i)], [rdup])
            else:
                tt("pool", OUTB[o][:, :n], RT1[i][:, :n], RT2[i][:, :n], ALU.add, [("RT1", i), ("RT2", i)], [("OUTB", o)])
                dma("sp", dst_dram, OUTB[o][:, :n], [("OUTB", o)], [rdst])

        def plain_store(ps, rps, n, dst_dram, rdst):
            o = cnt["ob"] % 3
            cnt["ob"] += 1
            copy("act", OUTB[o][:, :n], ps[:, :n], [rps], [("OUTB", o)])
            dma("sp", dst_dram, OUTB[o][:, :n], [("OUTB", o)], [rdst])

        def load_ln_transpose(src_rows, ti, hti, col0):
            b = ti % 2
            dma("sp", XT[b][:, :], src_rows, (), [("XT", b)])
            layer_norm_tile(XT[b], 0, 1, HB[b][:, :], "emb", ("XT", b), [("HB", b)])
            for half in range(2):
                tp_, rtp = pb()
                def fn(e, tp_=tp_, b=b, half=half):
                    ins = None
                    for kc in range(4):
                        k = half * 4 + kc
                        ins = e.transpose(tp_[:, kc * 128:(kc + 1) * 128], HB[b][:, k * 128:(k + 1) * 128], identB)
                    return ins
                S.op("pe", fn, [("HB", b), "CB"], [rtp])
                dst = HT[hti][:, half * 4:half * 4 + 4, col0:col0 + 128]
                src = tp_[:, 0:512].rearrange("p (k n) -> p k n", k=4)
                copy("act" if half == 0 else "dve", dst, src, [rtp], [("HT", hti, half, col0)])

        def ht_res(hti, ntile):
            return [("HT", hti, half, c * 128) for half in range(2) for c in range(ntile)]

        def proj_fm(W, col0, hti, n, rw):
            ps, rps = pf()
            items = [(ps[:, :n], W[:, kc, col0:col0 + 128], HT[hti][:, kc, :n], kc == 0, kc == 7) for kc in range(8)]
            S.op("pe", mm_chain(items), [rw] + ht_res(hti, (n + 127) // 128), [rps])
            return ps, rps

        nchunks = (nkb + 3) // 4 if stop_after >= 1 else 0
        for ch in range(nchunks):
            t0 = ch * 4
            ntile = min(4, nkb - t0)
            n = ntile * 128
            k0 = t0 * 128
            hti = ch % 2
            for t in range(ntile):
                load_ln_transpose(xk[(t0 + t) * 128:(t0 + t + 1) * 128, :], t0 + t, hti, t * 128)
            ci = ch % 2
            dma("sp", CS[ci][:, 0:n], ck_d[:, k0:k0 + n], (), [("CS", ci, 0)])
            dma("sp", CS[ci][:, 512:512 + n], sk_d[:, k0:k0 + n], (), [("CS", ci, 1)])
            csr = [("CS", ci, 0), ("CS", ci, 1)]

            def rope_head(ps, rps):
                i = cnt["fm"] % 2
                cnt["fm"] += 1
                copy("act", FMB[i][:, :n], ps[:, :n], [rps], [("FMB", i)])
                tt("dve", RT1[i][:, :n], ps[:, :n], CS[ci][:, 0:n], ALU.mult, [rps] + csr, [("RT1", i)])
                return i

            def rope_tail(i, out_ap, rout):
                pp, rpp = pf()
                S.op("pe", mm(pp[:, :n], ropeP, FMB[i][:, :n], True, True), ["CB", ("FMB", i)], [rpp])
                tt("dve", RT2[i][:, :n], pp[:, :n], CS[ci][:, 512:512 + n], ALU.mult, [rpp] + csr, [("RT2", i)])
                tt("pool", out_ap, RT1[i][:, :n], RT2[i][:, :n], ALU.add, [("RT1", i), ("RT2", i)], [rout])

            def hres_t(t):
                return [("HT", hti, half, t * 128) for half in range(2)]

            for t in range(ntile):
                ps, rps = pf()
                items = [(ps[:, 0:256], HT[hti][:, kc, t * 128:(t + 1) * 128], WK[:, kc, 0:256], kc == 0, kc == 7) for kc in range(8)]
                S.op("pe", mm_chain(items), ["WK"] + hres_t(t), [rps])
                c = t
                act(CKV32[c][:, :], ps[:, 0:256], AF.Square, [rps], [("CKV32", c)], accum=SM[c][:, 0:1])
                ts("dve", SM[c][:, 1:2], SM[c][:, 0:1], 1.0 / 256.0, 1e-6, ALU.mult, ALU.add, [("CKV32", c)], [("SM", c, 1)])
                act(SM[c][:, 3:4], SM[c][:, 1:2], AF.Ln, [("SM", c, 1)], [("SM", c, 3)])
                act(SM[c][:, 2:3], SM[c][:, 3:4], AF.Exp, [("SM", c, 3)], [("SM", c, 2)], scale=-0.5)
                stt("dve", CKVB[c][:, :], ps[:, 0:256], SM[c][:, 2:3], KVG[:, :], ALU.mult, ALU.mult, [rps, ("SM", c, 2), "KVG"], [("CKVB", c)])
            ps, rps = proj_fm(WK, 256, hti, n, "WK")
            ki_i = rope_head(ps, rps)
            for ot in range(4):
                ps, rps = proj_fm(WK, 384 + ot * 128, hti, n, "WK")
                plain_store(ps, rps, n, kfT_d[ot, :, k0:k0 + n], ("kfT_d", ot, ch))
            for t in range(ntile):
                kb = t0 + t
                ps, rps = pf()
                items = [(ps[:, 0:512], HT[hti][:, kc, t * 128:(t + 1) * 128], WK[:, kc, 896:1408], kc == 0, kc == 7) for kc in range(8)]
                S.op("pe", mm_chain(items), ["WK"] + hres_t(t), [rps])
                v = cnt["vs"] % 2
                cnt["vs"] += 1
                VV = VST[v].rearrange("p (h c) -> p h c", h=8)
                memset("pool", VST[v][:, :], 1.0, [("VST", v)])
                if kb == 0:
                    ts("dve", VV[:, :, 0:64], ps[:, 0:512].rearrange("p (h c) -> p h c", h=8), padmask, None, ALU.mult, None, [rps, ("VST", v), "CF"], [("VST", v)])
                    ts("dve", VV[:, :, 64:65], VV[:, :, 64:65], padmask, None, ALU.mult, None, [("VST", v), "CF"], [("VST", v)])
                else:
                    copy("act", VV[:, :, 0:64], ps[:, 0:512].rearrange("p (h c) -> p h c", h=8), [rps, ("VST", v)], [("VST", v)])
                dma("sp", vf_d[kb], VST[v][:, :], [("VST", v)], [("vf_d", kb)])
                ps, rps = pf()
                items = [(ps[:, 0:8], HT[hti][:, kc, t * 128:(t + 1) * 128], WK[:, kc, 1408:1416], kc == 0, kc == 7) for kc in range(8)]
                S.op("pe", mm_chain(items), ["WK"] + hres_t(t), [rps])
                c2 = t
                tt("dve", SM[c2][:, 8:16], ps[:, 0:8], BFG[:, :], ALU.add, [rps, "BFG"], [("SM", c2, 8)])
                act(SM[c2][:, 16:24], SM[c2][:, 8:16], AF.Exp, [("SM", c2, 8)], [("SM", c2, 16)], scale=-1.0)
                act(SM[c2][:, 24:32], SM[c2][:, 16:24], AF.Ln, [("SM", c2, 16)], [("SM", c2, 24)], bias=1.0)
                if kb == 0:
                    stt("dve", LOGF[:, kb * 8:(kb + 1) * 8], SM[c2][:, 24:32], -1.0, padmask.to_broadcast([128, 8]), ALU.mult, ALU.mult, [("SM", c2, 24), "CF"], [("LOGF", kb)])
                else:
                    ts("dve", LOGF[:, kb * 8:(kb + 1) * 8], SM[c2][:, 24:32], -1.0, None, ALU.mult, None, [("SM", c2, 24)], [("LOGF", kb)])
            rope_tail(ki_i, KIT[:, k0:k0 + n], ("KIT", ch))
            for t in range(ntile):
                c = t
                tp_, rtp = pb()
                def fn(e, tp_=tp_, c=c):
                    ins = None
                    for kc in range(2):
                        ins = e.transpose(tp_[:, kc * 128:(kc + 1) * 128], CKVB[c][:, kc * 128:(kc + 1) * 128], identB)
                    return ins
                S.op("pe", fn, [("CKVB", c), "CB"], [rtp])
                copy("act", CKVT[hti][:, :, t * 128:(t + 1) * 128], tp_[:, 0:256].rearrange("p (k n) -> p k n", k=2), [rtp], [("CKVT", hti, t)])
            for t in range(ntile):
                kb = t0 + t
                ps, rps = pf()
                items = [(ps[:, 0:512], CKVT[hti][:, kc, t * 128:(t + 1) * 128], WKV[:, kc, 512:1024], kc == 0, kc == 1) for kc in range(2)]
                S.op("pe", mm_chain(items), ["WKV", ("CKVT", hti, t)], [rps])
                v = cnt["vs"] % 2
                cnt["vs"] += 1
                VV = VST[v].rearrange("p (h c) -> p h c", h=8)
                memset("pool", VST[v][:, :], 1.0, [("VST", v)])
                if kb == 0:
                    ts("dve", VV[:, :, 0:64], ps[:, 0:512].rearrange("p (h c) -> p h c", h=8), padmask, None, ALU.mult, None, [rps, ("VST", v), "CF"], [("VST", v)])
                    ts("dve", VV[:, :, 64:65], VV[:, :, 64:65], padmask, None, ALU.mult, None, [("VST", v), "CF"], [("VST", v)])
                else:
                    copy("act", VV[:, :, 0:64], ps[:, 0:512].rearrange("p (h c) -> p h c", h=8), [rps, ("VST", v)], [("VST", v)])
                dma("sp", va_d[kb], VST[v][:, :], [("VST", v)], [("va_d", kb)])
            for pair in range(2):
                heads_ = []
                for ot in (2 * pair, 2 * pair + 1):
                    ps, rps = pf()
                    items = [(ps[:, :n], WKV[:, kc, ot * 128:(ot + 1) * 128], CKVT[hti][:, kc, :n], kc == 0, kc == 1) for kc in range(2)]
                    S.op("pe", mm_chain(items), ["WKV"] + [("CKVT", hti, t) for t in range(ntile)], [rps])
                    heads_.append((ot, rope_head(ps, rps)))
                for ot, i in heads_:
                    o = cnt["ob"] % 3
                    cnt["ob"] += 1
                    rope_tail(i, OUTB[o][:, :n], ("OUTB", o))
                    dma("sp", kaT_d[ot, :, k0:k0 + n], OUTB[o][:, :n], [("OUTB", o)], [("kaT_d", ot, ch)])

        logf_res = [("LOGF", kb) for kb in range(NKB)]
        nb8 = NKB * 8
        for half in range(2):
            c0 = half * 264
            c1 = min(nb8, c0 + 264)
            ps, rps = pf()
            S.op("pe", mm(ps[:, 0:c1 - c0], utri, LOGF[:, c0:c1], True, True), ["CF"] + logf_res, [rps])
            copy("dve", CC[:, c0:c1], ps[:, 0:c1 - c0], [rps], [("CC", half)])
            ps2, rps2 = pf()
            S.op("pe", mm(ps2[:, 0:c1 - c0], onesF, LOGF[:, c0:c1], True, True), ["CF"] + logf_res, [rps2])
            copy("dve", PI[:, c0:c1], ps2[:, 0:c1 - c0], [rps2], [("PI", half)])
        for kb in range(1, NKB):
            tt("dve", PI[:, kb * 8:(kb + 1) * 8], PI[:, kb * 8:(kb + 1) * 8], PI[:, (kb - 1) * 8:kb * 8], ALU.add,
               [("PI", 0), ("PI", 1), ("PIx", kb - 1)], [("PIx", kb)])
        for kb in range(1, NKB):
            tt("pool", CC[:, kb * 8:(kb + 1) * 8], CC[:, kb * 8:(kb + 1) * 8], PI[:, (kb - 1) * 8:kb * 8], ALU.add,
               [("CC", 0), ("CC", 1), ("PIx", kb - 1), ("PIx", max(kb - 2, 0))], [("CCx", kb)])
        cc_res = [("CC", 0), ("CC", 1)] + [("CCx", kb) for kb in range(1, NKB)]
        pi_res = [("PI", 0), ("PI", 1)] + [("PIx", kb) for kb in range(1, NKB)]
        for j in range(nslot):
            ts("dve", CREF[:, j * 8:(j + 1) * 8], PI[:, (4 * j + 1) * 8:(4 * j + 2) * 8], selr[:, 0:1], None, ALU.mult, None, pi_res + ["CF"], [("CREF", j)])
            for t in range(1, 4):
                stt("dve", CREF[:, j * 8:(j + 1) * 8], PI[:, (4 * j + 1 + t) * 8:(4 * j + 2 + t) * 8], selr[:, t:t + 1], CREF[:, j * 8:(j + 1) * 8],
                    ALU.mult, ALU.add, pi_res + ["CF", ("CREF", j)], [("CREF", j)])

        for ch in range(nslot // 4 if nslot >= 4 else 1):
            ntile = min(4, nslot)
            n = ntile * 128
            q0 = ch * 512
            hti = ch % 2
            for t in range(ntile):
                load_ln_transpose(xq[q0 + t * 128:q0 + (t + 1) * 128, :], t, hti, t * 128)
            for kc in range(8):
                dma("sp", hqT_d[kc, :, q0:q0 + n], HT[hti][:, kc, :n], ht_res(hti, ntile), [("hqT_d", ch, kc)])
            ci = ch % 2
            dma("sp", CS[ci][:, 0:n], cq_d[:, q0:q0 + n], (), [("CS", ci, 0)])
            dma("sp", CS[ci][:, 512:512 + n], sq_d[:, q0:q0 + n], (), [("CS", ci, 1)])
            csr = [("CS", ci, 0), ("CS", ci, 1)]
            for grp, dst in ((0, qaT_d), (1, qiT_d)):
                for ot in range(4):
                    ps, rps = proj_fm(WQ, grp * 512 + ot * 128, hti, n, "WQ")
                    i = cnt["fm"] % 2
                    cnt["fm"] += 1
                    copy("act", FMB[i][:, :n], ps[:, :n], [rps], [("FMB", i)])
                    tt("dve", RT1[i][:, :n], ps[:, :n], CS[ci][:, 0:n], ALU.mult, [rps] + csr, [("RT1", i)])
                    pp, rpp = pf()
                    S.op("pe", mm(pp[:, :n], ropeP, FMB[i][:, :n], True, True), ["CB", ("FMB", i)], [rpp])
                    tt("dve", RT2[i][:, :n], pp[:, :n], CS[ci][:, 512:512 + n], ALU.mult, [rpp] + csr, [("RT2", i)])
                    o = cnt["ob"] % 3
                    cnt["ob"] += 1
                    tt("pool", OUTB[o][:, :n], RT1[i][:, :n], RT2[i][:, :n], ALU.add, [("RT1", i), ("RT2", i)], [("OUTB", o)])
                    dma("sp", dst[ot, :, q0:q0 + n], OUTB[o][:, :n], [("OUTB", o)], [(id(dst), ot, ch)])
            for ot in range(4):
                ps, rps = proj_fm(WQ, 1024 + ot * 128, hti, n, "WQ")
                plain_store(ps, rps, n, qfT_d[ot, :, q0:q0 + n], ("qfT_d", ot, ch))
            for t in range(ntile):
                sl = ch * 4 + t
                hres = [("HT", hti, half, t * 128) for half in range(2)]
                ps, rps = pf()
                items = [(ps[:, 0:8], HT[hti][:, kc, t * 128:(t + 1) * 128], WQ[:, kc, 1536:1544], kc == 0, kc == 7) for kc in range(8)]
                S.op("pe", mm_chain(items), ["WQ"] + hres, [rps])
                copy("dve", WIQ[:, sl * 8:(sl + 1) * 8], ps[:, 0:8], [rps], [("WIQ", sl)])

        S.barrier()

        A.off = P1_END
        SIDX = A.f32(TP)
        MNEGS = [A.bf(TP) for _ in range(2)]
        RL = [A.bf(512) for _ in range(8)]
        DW = A.bf(8 * 128)
        QI = A.bf(4 * 128).rearrange("p (t n) -> p t n", t=4)
        QA = A.bf(4 * 128).rearrange("p (t n) -> p t n", t=4)
        QF = A.bf(4 * 128).rearrange("p (t n) -> p t n", t=4)
        KS = [A.bf(4 * 512).rearrange("p (t n) -> p t n", t=4) for _ in range(2)]
        VS = [A.bf(4 * 8 * VW).rearrange("p (b n) -> p b n", b=4) for _ in range(2)]
        PT = [A.bf(1024) for _ in range(2)]
        BIASK = A.f32(NKB * 8)
        EFOX = A.f32(NKB * 8)
        BS = A.f32(64)
        HW = A.f32(NBIS)
        OSB = A.f32(8 * VW)
        RCP = A.f32(8)
        YB = A.bf(512)
        YT = A.bf(4 * 128)
        yaT_d = dscr("yaT_d", [4, 128, nq])
        yfT_d = dscr("yfT_d", [4, 128, nq])
        P2_END = A.off

        O_A = (PF[4], ("PF", 4))
        O_B = (PF[5], ("PF", 5))
        sc = {"k": 0}

        def attention(j, nk, kT_dram, v_dram, Qt, rQ, fox, yT_dram, tagname, hook):
            memset("dve", O_A[0][:, :], 0.0, [O_A[1]])
            memset("dve", O_B[0][:, :], 0.0, [O_B[1]])
            if hook is not None:
                hook()
            MNEG = MNEGS[j % 2]
            cbuf = {}

            def stage_a(kb):
                ch, kbl = divmod(kb, 4)
                if kbl == 0:
                    nb = min(4, nk - ch * 4)
                    w = nb * 128
                    k0 = ch * 512
                    cbuf[ch] = sc["k"] % 2
                    sc["k"] += 1
                    b = cbuf[ch]
                    dma("sp", KS[b][:, :, 0:w], kT_dram[:, :, k0:k0 + w].rearrange("t p k -> p t k"),
                        [(tagname + "kT", ot, ch) for ot in range(4)], [("KS", b)])
                    dma("sp", VS[b][:, 0:nb, :], v_dram[ch * 4:ch * 4 + nb].rearrange("b p c -> p b c"),
                        [(tagname + "v", kk) for kk in range(ch * 4, ch * 4 + nb)], [("VS", b)])
                    if fox:
                        vv = VS[b][:, 0:nb, :].rearrange("p b (h c) -> p b h c", h=8)
                        ee = EFOX[:, ch * 32:ch * 32 + nb * 8].rearrange("p (b h) -> p b h", h=8).unsqueeze(3).to_broadcast([128, nb, 8, VW])
                        tt("pool", vv, vv, ee, ALU.mult, [("VS", b), "EFOX"], [("VS", b)])
                b = cbuf[ch]
                tail = kb - (nk - 4)
                for par in range(2):
                    st_, rst = pf(0, 4)
                    items = []
                    if not fox:
                        items.append((st_[:, :], MNEG[:, kb * 128:(kb + 1) * 128], ident4, True, False))
                    elif tail >= 0:
                        items.append((st_[:, :], cmneg[:, tail * 128:(tail + 1) * 128], ident4, True, False))
                    masked = len(items) > 0
                    p0 = par * 64
                    for hh in range(4):
                        items.append((st_[:, hh * 128:(hh + 1) * 128],
                                      KS[b][p0:p0 + 64, hh, kbl * 128:(kbl + 1) * 128],
                                      Qt[p0:p0 + 64, hh, :], not masked, True))
                    reads = [("KS", b), rQ, "CB"]
                    if not fox:
                        reads.append(("MNEG", j % 2))
                    S.op("pe", mm_chain(items), reads, [rst])
                    act(PTcur(kb)[:, par * 512:(par + 1) * 512], st_[:, :], AF.Exp, [rst], [("PT", kb % 2, par)], scale=0.125)

            def stage_b(kb):
                ch, kbl = divmod(kb, 4)
                b = cbuf[ch]
                for hg, (O_, rO) in enumerate((O_A, O_B)):
                    items = []
                    for hh in range(4):
                        h = hg * 4 + hh
                        sl = (h % 2) * 4 + h // 2
                        items.append((O_[:, hh * VW:hh * VW + 65], PTcur(kb)[:, sl * 128:(sl + 1) * 128],
                                      VS[b][:, kbl, h * VW:h * VW + 65], False, False))
                    S.op("pe", mm_chain(items), [("PT", kb % 2, 0), ("PT", kb % 2, 1), ("VS", b)], [rO])

            stage_a(0)
            for kb in range(nk):
                if kb + 1 < nk:
                    stage_a(kb + 1)
                stage_b(kb)
            for hg, (O_, rO) in enumerate((O_A, O_B)):
                copy("dve", OSB[:, hg * 4 * VW:(hg + 1) * 4 * VW], O_[:, 0:4 * VW], [rO], [("OSB", hg)])
            OV = OSB.rearrange("p (h c) -> p h c", h=8)
            S.op("dve", lambda e: e.reciprocal(RCP[:, :], OV[:, :, 64]), [("OSB", 0), ("OSB", 1)], ["RCP"])
            for h in range(8):
                ts("dve", YB[:, h * 64:(h + 1) * 64], OV[:, h, 0:64], RCP[:, h:h + 1], None, ALU.mult, None,
                   [("OSB", 0), ("OSB", 1), "RCP"], [("YB", h)])
            tp_, rtp = pb()
            def fn(e, tp_=tp_):
                ins = None
                for kc in range(4):
                    ins = e.transpose(tp_[:, kc * 128:(kc + 1) * 128], YB[:, kc * 128:(kc + 1) * 128], identB)
                return ins
            S.op("pe", fn, [("YB", h) for h in range(8)] + ["CB"], [rtp])
            copy("act", YT[:, :], tp_[:, 0:512], [rtp], ["YT"])
            dma("sp", yT_dram[:, :, j * 128:(j + 1) * 128].rearrange("t p n -> p t n"), YT.rearrange("p (t n) -> p t n", t=4),
                ["YT"], [(tagname + "yT", j)])

        def kT_dram_name(x):
            return id(x)

        def PTcur(kb):
            return PT[kb % 2]

        def emit_idx(j):
            nk = 4 * j + 5
            n = nk * 128
            dma("sp", QI, qiT_d[:, :, j * 128:(j + 1) * 128].rearrange("t p n -> p t n"), (), ["QI"])
            for h in range(8):
                ts("dve", DW[:, h * 128:(h + 1) * 128], identB, WIQ[:, j * 8 + h:j * 8 + h + 1], None, ALU.mult, None,
                   ["CB", ("WIQ", j)], [("DW", h)])
            nch = (nk + 3) // 4
            for ch in range(nch):
                w = min(512, n - ch * 512)
                k0 = ch * 512
                for h in range(8):
                    p0 = (h % 2) * 64
                    z, rz = pf(0, 4)
                    S.op("pe", mm(z[:, :w], QI[p0:p0 + 64, h // 2, :], KIT[p0:p0 + 64, k0:k0 + w], True, True), ["QI", "KITall"], [rz])
                    if h % 2 == 0:
                        act(RL[h][:, :w], z[:, :w], AF.Relu, [rz], [("RL", h)])
                    else:
                        ts("dve", RL[h][:, :w], z[:, :w], 0.0, None, ALU.max, None, [rz], [("RL", h)])
                acc, racc = pf(4, 6)
                items = [(acc[:, :w], DW[:, h * 128:(h + 1) * 128], RL[h][:, :w], h == 0, h == 7) for h in range(8)]
                S.op("pe", mm_chain(items), [("DW", h) for h in range(8)] + [("RL", h) for h in range(8)], [racc])
                copy("dve", SIDX[:, k0:k0 + w], acc[:, :w], [racc], [("SIDX", ch)])

        def emit_bis(j):
            nk = 4 * j + 5
            n = nk * 128
            nch = (nk + 3) // 4
            MN = MNEGS[j % 2]
            rm = ("MNEG", j % 2)
            sres = [("SIDX", ch) for ch in range(nch)]
            S.op("dve", lambda e, n=n: e.tensor_reduce(BS[:, 0:1], SIDX[:, 0:n], AX.X, ALU.max, True), sres, [("BS", 0)])
            memset("dve", SIDX[:, 0:PAD], -1e30, [("SIDX", 0)])
            tt("dve", SIDX[:, n - 512:n], SIDX[:, n - 512:n], cmadd, ALU.add, sres + ["CF"], sres)
            ts("dve", BS[:, 2:3], BS[:, 0:1], -1.0, None, ALU.mult, None, [("BS", 0)], [("BS", 2)])
            ts("dve", BS[:, 1:2], BS[:, 0:1], 2.0, None, ALU.mult, None, [("BS", 0)], [("BS", 1)])
            ts("dve", HW[:, :], pow2, BS[:, 1:2], None, ALU.mult, None, ["CF", ("BS", 1)], ["HW"])
            for it in range(NBIS):
                tt("dve", BS[:, 3:4], BS[:, 2:3], HW[:, it:it + 1], ALU.add, [("BS", 2), "HW"], [("BS", 3)])
                ts("dve", MN[:, 0:n], SIDX[:, 0:n], BS[:, 3:4], 0.0, ALU.is_ge, ALU.add, sres + [("BS", 3)], [rm, ("BS", 4)], accum=BS[:, 4:5])
                stt("dve", BS[:, 5:6], BS[:, 4:5], 255.5, HW[:, it:it + 1], ALU.is_ge, ALU.mult, [("BS", 4), "HW"], [("BS", 5)])
                tt("dve", BS[:, 2:3], BS[:, 2:3], BS[:, 5:6], ALU.add, [("BS", 2), ("BS", 5)], [("BS", 2)])
            ts("dve", MN[:, 0:n], SIDX[:, 0:n], BS[:, 2:3], NEGM, ALU.is_lt, ALU.mult, sres + [("BS", 2)], [rm])

        nsl = nslot if stop_after >= 2 else 0
        if nsl:
            emit_idx(0)
            emit_bis(0)
        for j in range(nsl):
            nk = 4 * j + 5
            dma("sp", QA, qaT_d[:, :, j * 128:(j + 1) * 128].rearrange("t p n -> p t n"), (), ["QA"])
            dma("sp", QF, qfT_d[:, :, j * 128:(j + 1) * 128].rearrange("t p n -> p t n"), (), ["QF"])
            hook = None
            if j + 1 < nsl:
                emit_idx(j + 1)
                hook = (lambda jj=j + 1: emit_bis(jj))
            attention(j, nk, kaT_d, va_d, QA, "QA", False, yaT_d, "a", hook)
            BK = BIASK[:, 0:nk * 8].rearrange("p (k h) -> p k h", h=8)
            tt("dve", BK, CREF[:, j * 8:(j + 1) * 8].unsqueeze(1).to_broadcast([128, nk, 8]),
               CC[:, 0:nk * 8].rearrange("p (k h) -> p k h", h=8), ALU.subtract, [("CREF", j)] + cc_res, ["BIASK"])
            act(EFOX[:, 0:nk * 8], BIASK[:, 0:nk * 8], AF.Exp, ["BIASK"], ["EFOX"])
            attention(j, nk, kfT_d, vf_d, QF, "QF", True, yfT_d, "f", None)

        S.barrier()

        A.off = 0
        GATE = A.f32(nslot * 32)
        P3_KEEP = A.off
        WBA = A.bf(4 * 1024).rearrange("p (k n) -> p k n", k=4)
        WBF = A.bf(4 * 1024).rearrange("p (k n) -> p k n", k=4)
        WG = A.bf(8 * 2048).rearrange("p (k n) -> p k n", k=8)
        WO = A.bf(8 * 1024).rearrange("p (k n) -> p k n", k=8)
        WR = A.f32(8 * 36).rearrange("p (k n) -> p k n", k=8)
        H1TS = [A.bf(D).rearrange("p (k n) -> p k n", k=8) for _ in range(2)]
        YAT = [A.bf(512).rearrange("p (t n) -> p t n", t=4) for _ in range(2)]
        YFT = [A.bf(512).rearrange("p (t n) -> p t n", t=4) for _ in range(2)]
        HQ = [A.bf(1024).rearrange("p (k n) -> p k n", k=8) for _ in range(2)]
        XQ = [A.f32(D) for _ in range(2)]
        H32 = [A.f32(D) for _ in range(2)]
        GA = [A.f32(D) for _ in range(2)]
        GF = [A.f32(D) for _ in range(2)]
        MGB = [A.bf(D) for _ in range(2)]
        MGT = [A.bf(D).rearrange("p (k n) -> p k n", k=8) for _ in range(2)]
        H1P = [A.f32(D) for _ in range(2)]
        H1 = [A.f32(D) for _ in range(2)]
        H1B = [A.bf(D) for _ in range(2)]
        H1T32 = [A.f32(D).rearrange("p (k n) -> p k n", k=8) for _ in range(2)]
        RS = [A.f32(256) for _ in range(2)]

        dma("pool", WBA, wba_d.rearrange("(k p) n -> p k n", p=128), (), ["WBA"])
        dma("pool", WBF, wbf_d.rearrange("(k p) n -> p k n", p=128), (), ["WBF"])
        dma("pool", WG, wg_d.rearrange("(k p) n -> p k n", p=128), (), ["WG"])
        dma("pool", WO, wo_d.rearrange("(k p) n -> p k n", p=128), (), ["WO"])
        dma("sp", WR, wr_d.rearrange("(k p) n -> p k n", p=128), (), ["WR"])

        for i in range(nslot if stop_after >= 3 else 0):
            b = i % 2
            dma("sp", YAT[b], yaT_d[:, :, i * 128:(i + 1) * 128].rearrange("t p n -> p t n"), [("ayT", i)], [("YAT", b)])
            dma("sp", YFT[b], yfT_d[:, :, i * 128:(i + 1) * 128].rearrange("t p n -> p t n"), [("fyT", i)], [("YFT", b)])
            dma("sp", HQ[b], hqT_d[:, :, i * 128:(i + 1) * 128].rearrange("k p n -> p k n"),
                [("hqT_d", i // 4, kc) for kc in range(8)], [("HQ", b)])
            dma("sp", XQ[b][:, :], xq[i * 128:(i + 1) * 128, :], (), [("XQ", b)])
            layer_norm_tile(XQ[b], 0, 1, H32[b][:, :], "emb", ("XQ", b), [("H32", b)])
            for half in range(2):
                c0 = half * 512
                for (Wcol, GT, nm) in ((0, GA, "GA"), (1024, GF, "GF")):
                    ps, rps = pf()
                    items = [(ps[:, :], HQ[b][:, kc, :], WG[:, kc, Wcol + c0:Wcol + c0 + 512], kc == 0, kc == 7) for kc in range(8)]
                    S.op("pe", mm_chain(items), ["WG", ("HQ", b)], [rps])
                    act(GT[b][:, c0:c0 + 512], ps[:, :], AF.Sigmoid, [rps], [(nm, b, half)])
                ps, rps = pf()
                items = [(ps[:, :], YAT[b][:, kc, :], WBA[:, kc, c0:c0 + 512], kc == 0, kc == 3) for kc in range(4)]
                S.op("pe", mm_chain(items), ["WBA", ("YAT", b)], [rps])
                tt("dve", GA[b][:, c0:c0 + 512], ps[:, :], GA[b][:, c0:c0 + 512], ALU.mult, [rps, ("GA", b, half)], [("GA", b, half)])
                ps, rps = pf()
                items = [(ps[:, :], YFT[b][:, kc, :], WBF[:, kc, c0:c0 + 512], kc == 0, kc == 3) for kc in range(4)]
                S.op("pe", mm_chain(items), ["WBF", ("YFT", b)], [rps])
                tt("dve", GF[b][:, c0:c0 + 512], ps[:, :], GF[b][:, c0:c0 + 512], ALU.mult, [rps, ("GF", b, half)], [("GF", b, half)])
                tt("pool", MGB[b][:, c0:c0 + 512], GA[b][:, c0:c0 + 512], GF[b][:, c0:c0 + 512], ALU.add,
                   [("GA", b, half), ("GF", b, half)], [("MGB", b, half)])
            for half in range(2):
                tp_, rtp = pb()
                def fn(e, tp_=tp_, b=b, half=half):
                    ins = None
                    for kc in range(4):
                        k = half * 4 + kc
                        ins = e.transpose(tp_[:, kc * 128:(kc + 1) * 128], MGB[b][:, k * 128:(k + 1) * 128], identB)
                    return ins
                S.op("pe", fn, [("MGB", b, 0), ("MGB", b, 1), "CB"], [rtp])
                copy("act", MGT[b][:, half * 4:half * 4 + 4, :], tp_[:, 0:512].rearrange("p (k n) -> p k n", k=4), [rtp], [("MGT", b, half)])
            for half in range(2):
                c0 = half * 512
                ps, rps = pf()
                items = [(ps[:, :], MGT[b][:, kc, :], WO[:, kc, c0:c0 + 512], kc == 0, kc == 7) for kc in range(8)]
                S.op("pe", mm_chain(items), ["WO", ("MGT", b, 0), ("MGT", b, 1)], [rps])
                stt("dve", H1P[b][:, c0:c0 + 512], H32[b][:, c0:c0 + 512], ALPHA, ps[:, :], ALU.mult, ALU.add, [("H32", b), rps], [("H1P", b, half)])
            layer_norm_tile(H1P[b], 2, 3, H1[b][:, :], "ln1", [("H1P", b, 0), ("H1P", b, 1)], [("H1", b)])
            dma("sp", h1_d[i * 128:(i + 1) * 128, :], H1[b][:, :], [("H1", b)], [("h1_d", i)])
            copy("act", H1B[b][:, :], H1[b][:, :], [("H1", b)], [("H1B", b)])
            for half in range(2):
                tp_, rtp = pb()
                def fn(e, tp_=tp_, b=b, half=half):
                    ins = None
                    for kc in range(4):
                        k = half * 4 + kc
                        ins = e.transpose(tp_[:, kc * 128:(kc + 1) * 128], H1B[b][:, k * 128:(k + 1) * 128], identB)
                    return ins
                S.op("pe", fn, [("H1B", b), "CB"], [rtp])
                copy("act", H1TS[b][:, half * 4:half * 4 + 4, :], tp_[:, 0:512].rearrange("p (k n) -> p k n", k=4), [rtp], [("H1TS", b, half)])
            dma("sp", h1T_d[:, :, i * 128:(i + 1) * 128].rearrange("k p n -> p k n"), H1TS[b], [("H1TS", b, 0), ("H1TS", b, 1)], [("h1T_d", i)])
            for half in range(2):
                ps, rps = pf()
                def fn(e, ps=ps, b=b, half=half):
                    ins = None
                    for kc in range(4):
                        k = half * 4 + kc
                        ins = e.transpose(ps[:, kc * 128:(kc + 1) * 128], H1[b][:, k * 128:(k + 1) * 128], identF)
                    return ins
                S.op("pe", fn, [("H1", b), "CF"], [rps])
                copy("dve", H1T32[b][:, half * 4:half * 4 + 4, :], ps[:, :].rearrange("p (k n) -> p k n", k=4), [rps], [("H1T32", b, half)])
            ps, rps = pf()
            items = [(ps[:, 0:36], H1T32[b][:, kc, :], WR[:, kc, :], kc == 0, kc == 7) for kc in range(8)]
            S.op("pe", mm_chain(items), ["WR", ("H1T32", b, 0), ("H1T32", b, 1)], [rps])
            R = RS[b]
            rR = ("RS", b)
            k_ = [0]

            def rr_(nm):
                return ("RS", b, nm)
            tt("dve", R[:, 0:36], ps[:, 0:36], BRT[:, :], ALU.add, [rps, "BRT"], [rr_("lg")])
            S.op("dve", lambda e, R=R: e.tensor_reduce(R[:, 40:41], R[:, 0:4], AX.X, ALU.max), [rr_("lg")], [rr_("gmax")])
            ts("dve", R[:, 44:48], R[:, 0:4], R[:, 40:41], None, ALU.is_ge, None, [rr_("lg"), rr_("gmax")], [rr_("goh")])
            ts("dve", R[:, 41:42], R[:, 40:41], -1.0, None, ALU.mult, None, [rr_("gmax")], [rr_("ngmax")])
            act(R[:, 48:52], R[:, 0:4], AF.Exp, [rr_("lg"), rr_("ngmax")], [rr_("gexp"), rr_("gsum")], bias=R[:, 41:42], scale=1.0, accum=R[:, 42:43])
            S.op("dve", lambda e, R=R: e.reciprocal(R[:, 43:44], R[:, 42:43]), [rr_("gsum")], [rr_("pg")])
            ts("dve", R[:, 52:56], R[:, 44:48], -1.0, 1e30, ALU.add, ALU.mult, [rr_("goh")], [rr_("gpen")])
            tt("dve", R[:, 64:96].rearrange("p (g e) -> p g e", g=4), R[:, 4:36].rearrange("p (g e) -> p g e", g=4),
               R[:, 52:56].unsqueeze(2).to_broadcast([128, 4, 8]), ALU.add, [rr_("lg"), rr_("gpen")], [rr_("em")])
            S.op("dve", lambda e, R=R: e.tensor_reduce(R[:, 56:57], R[:, 64:96], AX.X, ALU.max), [rr_("em")], [rr_("m1")])
            ts("dve", R[:, 96:128], R[:, 64:96], R[:, 56:57], None, ALU.is_ge, None, [rr_("em"), rr_("m1")], [rr_("oh1")])
            stt("dve", R[:, 128:160], R[:, 96:128], -1e30, R[:, 64:96], ALU.mult, ALU.add, [rr_("oh1"), rr_("em")], [rr_("em2")])
            S.op("dve", lambda e, R=R: e.tensor_reduce(R[:, 57:58], R[:, 128:160], AX.X, ALU.max), [rr_("em2")], [rr_("m2")])
            ts("dve", R[:, 160:192], R[:, 128:160], R[:, 57:58], None, ALU.is_ge, None, [rr_("em2"), rr_("m2")], [rr_("oh2")])
            tt("dve", R[:, 58:59], R[:, 57:58], R[:, 56:57], ALU.subtract, [rr_("m1"), rr_("m2")], [rr_("dm")])
            act(R[:, 59:60], R[:, 58:59], AF.Exp, [rr_("dm")], [rr_("edm")])
            ts("dve", R[:, 60:61], R[:, 59:60], 1.0, None, ALU.add, None, [rr_("edm")], [rr_("den")])
            S.op("dve", lambda e, R=R: e.reciprocal(R[:, 61:62], R[:, 60:61]), [rr_("den")], [rr_("w1")])
            tt("dve", R[:, 62:63], R[:, 61:62], R[:, 43:44], ALU.mult, [rr_("w1"), rr_("pg")], [rr_("g1")])
            tt("dve", R[:, 63:64], R[:, 43:44], R[:, 62:63], ALU.subtract, [rr_("pg"), rr_("g1")], [rr_("g2")])
            ts("dve", R[:, 192:224], R[:, 96:128], R[:, 62:63], None, ALU.mult, None, [rr_("oh1"), rr_("g1")], [rr_("t1")])
            stt("dve", GATE[:, i * 32:(i + 1) * 32], R[:, 160:192], R[:, 63:64], R[:, 192:224], ALU.mult, ALU.add,
                [rr_("oh2"), rr_("g2"), rr_("t1")], [("GATE", i)])

        S.barrier()

        A.off = P3_KEEP
        H1T = A.bf(8 * nq).rearrange("p (k n) -> p k n", k=8)
        ACC = A.f32(nslot * D)
        dma("sp", H1T, h1T_d.rearrange("k p n -> p k n"), [("h1T_d", i) for i in range(nslot)], ["H1Tall"])
        WGT = [A.bf(8 * 256).rearrange("p (k n) -> p k n", k=8) for _ in range(2)]
        WUP = [A.bf(8 * 256).rearrange("p (k n) -> p k n", k=8) for _ in range(2)]
        WDN = [A.bf(2 * 1024).rearrange("p (k n) -> p k n", k=2) for _ in range(2)]
        SG = [A.f32(512) for _ in range(2)]
        AT = [A.bf(2 * 512).rearrange("p (f n) -> p f n", f=2) for _ in range(2)]
        FX = [A.f32(D) for _ in range(2)]
        FO = [A.f32(D) for _ in range(2)]

        memset("pool", ACC[:, :], 0.0, ["ACCall"])
        nchq = max(1, nq // 512)
        ac = {"n": 0}
        for ex in range(nexp if stop_after >= 4 else 0):
            b = ex % 2
            dma("pool", WGT[b], wgate_d[ex].rearrange("(k p) n -> p k n", p=128), (), [("WGT", b)])
            dma("pool", WUP[b], wup_d[ex].rearrange("(k p) n -> p k n", p=128), (), [("WUP", b)])
            dma("pool", WDN[b], wdn_d[ex].rearrange("(k p) n -> p k n", p=128), (), [("WDN", b)])
            for ch in range(nchq):
                n = min(512, nq)
                q0 = ch * 512
                a = ac["n"] % 2
                ac["n"] += 1
                hres = ["H1Tall"]
                for ft in range(2):
                    pg_, rpg = pf()
                    items = [(pg_[:, :n], WGT[b][:, kc, ft * 128:(ft + 1) * 128], H1T[:, kc, q0:q0 + n], kc == 0, kc == 7) for kc in range(8)]
                    S.op("pe", mm_chain(items), [("WGT", b)] + hres, [rpg])
                    pu_, rpu = pf()
                    items = [(pu_[:, :n], WUP[b][:, kc, ft * 128:(ft + 1) * 128], H1T[:, kc, q0:q0 + n], kc == 0, kc == 7) for kc in range(8)]
                    S.op("pe", mm_chain(items), [("WUP", b)] + hres, [rpu])
                    s = (ac["n"] + ft) % 2
                    act(SG[s][:, :n], pg_[:, :n], AF.Silu, [rpg], [("SG", s)])
                    tt("dve", AT[a][:, ft, :n], SG[s][:, :n], pu_[:, :n], ALU.mult, [("SG", s), rpu], [("AT", a, ft)])
                for t in range(n // 128):
                    i = ch * 4 + t
                    for half in range(2):
                        c0 = half * 512
                        py, rpy = pf()
                        items = [(py[:, :], AT[a][:, ft, t * 128:(t + 1) * 128], WDN[b][:, ft, c0:c0 + 512], ft == 0, ft == 1) for ft in range(2)]
                        S.op("pe", mm_chain(items), [("WDN", b), ("AT", a, 0), ("AT", a, 1)], [rpy])
                        stt("dve", ACC[:, i * D + c0:i * D + c0 + 512], py[:, :], GATE[:, i * 32 + ex:i * 32 + ex + 1],
                            ACC[:, i * D + c0:i * D + c0 + 512], ALU.mult, ALU.add,
                            [rpy, ("GATE", i), "ACCall", ("ACC", i, half)], [("ACC", i, half)])
        for i in range(nslot):
            b = i % 2
            dma("sp", FX[b][:, :], h1_d[i * 128:(i + 1) * 128, :], [("h1_d", i)], [("FX", b)])
            stt("dve", FX[b][:, :], FX[b][:, :], ALPHA, ACC[:, i * D:(i + 1) * D], ALU.mult, ALU.add,
                [("FX", b), ("ACC", i, 0), ("ACC", i, 1), "ACCall"], [("FX", b)])
            layer_norm_tile(FX[b], 4, 5, FO[b][:, :], "ln2", ("FX", b), [("FO", b)])
            dma("sp", out_d[i * 128:(i + 1) * 128, :], FO[b][:, :], [("FO", b)], [("out", i)])
        S.finish()
        S.prepare()

        with nc.Block() as block:
            @block.tensor
            def _(e):
                S.replay("pe", e, sems)

            @block.scalar
            def _(e):
                S.replay("act", e, sems)

            @block.vector
            def _(e):
                S.replay("dve", e, sems)

            @block.gpsimd
            def _(e):
                S.replay("pool", e, sems)

            @block.sync
            def _(e):
                S.replay("sp", e, sems)
    return nc


def _consts(r):
    bf = ml_dtypes.bfloat16
    ident = np.eye(128, dtype=np.float32)
    k = np.arange(128)
    utri = (k[:, None] <= k[None, :]).astype(np.float32)
    ones = np.ones((128, 128), np.float32)
    q = np.arange(128)[:, None]
    kk = np.arange(128)[None, :]
    tri_ok = kk <= q
    cm = np.zeros((128, 4, 128), bool)
    for t in range(4):
        if t < r:
            cm[:, t, :] = True
        elif t == r:
            cm[:, t, :] = tri_ok
    cmadd = np.where(cm, 0.0, -1e30).astype(np.float32).reshape(128, 512)
    cmneg = np.where(cm, 0.0, NEGM).astype(np.float32).reshape(128, 512)
    selr = np.zeros((128, 4), np.float32)
    selr[:, r] = 1.0
    pow2 = np.tile((2.0 ** -(np.arange(NBIS) + 1.0)).astype(np.float32)[None], (128, 1))
    padmask = (np.arange(128) >= PAD).astype(np.float32)[:, None]
    cf = np.concatenate([ident, utri, ones, cmadd, selr, pow2, padmask], axis=1).astype(np.float32)
    P = np.zeros((128, 128), np.float32)
    for hb in (0, 64):
        for d in range(8):
            P[hb + d + 8, hb + d] = -1.0
            P[hb + d, hb + d + 8] = 1.0
    cb = np.concatenate([ident, np.tile(ident, (1, 4)), P, cmneg], axis=1).astype(bf)
    return cf, cb


def _rope_tables(pos):
    half = 8
    inv = (500000.0 ** (-np.arange(half, dtype=np.float32) / half)).astype(np.float32)
    ang = pos.astype(np.float32)[None, :] * inv[:, None]
    c8, s8 = np.cos(ang).astype(np.float32), np.sin(ang).astype(np.float32)
    n = pos.shape[0]
    C = np.ones((128, n), np.float32)
    Sn = np.zeros((128, n), np.float32)
    for hb in (0, 64):
        C[hb:hb + 8] = c8
        C[hb + 8:hb + 16] = c8
        Sn[hb:hb + 8] = s8
        Sn[hb + 8:hb + 16] = s8
    return C, Sn


_NC_CACHE = {}


def kernel(x, meta_tokens, emb_ln_g, emb_ln_b, w_in, b_forget, kv_norm_g, w_kv_up, w_branch_dsa,
           w_branch_fox, w_out, ln1_g, ln1_b, w_router_group, b_router_group, w_router_expert,
           b_router_expert, w_gate, w_up, w_down, ln2_g, ln2_b):
    f = lambda a: np.ascontiguousarray(np.asarray(a, dtype=np.float32))
    x = f(x)
    w_in0 = f(w_in)[0]
    offs = np.cumsum([0, 512, 256, 512, 64, 8, 512, 512, 512, 8, 1024, 1024])
    seg = lambda i: w_in0[:, offs[i]:offs[i + 1]]
    q_a, c_kv, q_i, k_i, w_i, q_f, k_f, v_f, f_lg, g_a, g_f = [seg(i) for i in range(11)]
    wk = f(np.concatenate([c_kv, k_i, k_i, k_f, v_f, f_lg], axis=1))
    wq = f(np.concatenate([q_a, q_i, q_f, w_i], axis=1))
    wg = f(np.concatenate([g_a, g_f], axis=1))
    rep = lambda v: f(np.tile(np.asarray(v, np.float32).reshape(1, -1), (128, 1)))
    lnp = f(np.concatenate([rep(emb_ln_g), rep(emb_ln_b), rep(ln1_g[0]), rep(ln1_b[0]), rep(ln2_g[0]), rep(ln2_b[0])], axis=1))
    wr = f(np.concatenate([f(w_router_group)[0], f(w_router_expert)[0]], axis=1))
    brt = rep(np.concatenate([f(b_router_group)[0], f(b_router_expert)[0]]))
    common = dict(
        wk=wk, wq=wq, wg=wg, wkv=f(w_kv_up)[0], wba=f(w_branch_dsa)[0], wbf=f(w_branch_fox)[0], wo=f(w_out)[0],
        wr=wr, wgate=f(w_gate)[0][:NEXP_RUN], wup=f(w_up)[0][:NEXP_RUN], wdn=f(w_down)[0][:NEXP_RUN], lnp=lnp, kvg=rep(kv_norm_g[0]),
        bfg=rep(b_forget[0]), brt=brt,
    )
    kpos = np.arange(TP) - PAD
    ck, sk = _rope_tables(kpos)
    in_maps = []
    own = []
    for c in range(8):
        b, r = c // 4, c % 4
        xkc = np.zeros((TP, D), np.float32)
        xkc[PAD:PAD + NMETA] = f(meta_tokens)
        xkc[PAD + NMETA:] = x[b]
        blocks = [4 * j + r for j in range(NSLOT)]
        rows = np.concatenate([np.arange(bl * 128, (bl + 1) * 128) for bl in blocks])
        own.append((b, rows))
        cq, sq = _rope_tables(rows + NMETA)
        cf, cb = _consts(r)
        m = dict(common)
        m.update(xk=xkc, xq=f(x[b][rows]), ck=ck, sk=sk, cq=cq, sq=sq, cf32=cf, cbf=cb)
        in_maps.append(m)
    if "nc" not in _NC_CACHE:
        _NC_CACHE["nc"] = build()
    nc = _NC_CACHE["nc"]
    res = run_bass_kernel_spmd(nc, in_maps, core_ids=list(range(8)))
    out = np.zeros((2, SEQ, D), np.float32)
    for c in range(8):
        b, rows = own[c]
        out[b, rows] = np.asarray(res.results[c]["out"], dtype=np.float32)
    return out
```

```python
import os
import numpy as np
import ml_dtypes
from contextlib import ExitStack
import concourse.bass as bass
import concourse.mybir as mybir
from concourse.bass_utils import run_bass_kernel_spmd

F32 = mybir.dt.float32
BF16 = mybir.dt.bfloat16
AF = mybir.ActivationFunctionType
ALU = mybir.AluOpType
AX = mybir.AxisListType

D = 1024
SEQ = 8192
NMETA = 16
PAD = 112
TP = SEQ + NMETA + PAD
NKB = TP // 128
NSLOT = 16
NQ = NSLOT * 128
NBIS = 20
ALPHA = 2.0 ** 0.25
NEGM = -30000.0
NEXP = 32
VW = 68
NEXP_RUN = 32

WK_COLS = 256 + 128 + 512 + 512 + 8
WQ_COLS = 512 * 3 + 8


class Sched:
    ENG = ("pe", "act", "dve", "pool", "sp")
    NDMA = 8

    def __init__(self):
        self.ops = {e: [] for e in self.ENG}
        self.count = {e: 0 for e in self.ENG}
        self.seen = {e: {} for e in self.ENG}
        self.res = {}
        self.dma_rr = {e: 0 for e in self.ENG}
        self.dma_val = {}

    def op(self, eng, fn, reads=(), writes=(), dma=False):
        self.nops = getattr(self, "nops", 0) + 1
        if self.nops > getattr(self, "maxops", 10 ** 9):
            return None
        need = {}

        def add(tok):
            if tok is None:
                return
            k, v = tok
            if need.get(k, 0) < v:
                need[k] = v

        reads = list(reads)
        writes = list(writes)
        for r in list(reads):
            if isinstance(r, tuple) and r and r[0] in ("PF", "PB") and r not in writes:
                writes.append(r)
        for r in reads:
            st = self.res.get(r)
            if st:
                add(st["w"])
        for w in writes:
            st = self.res.get(w)
            if st:
                add(st["w"])
                for k, v in st["r"].items():
                    add((k, v))
        waits = []
        for k, v in need.items():
            if eng == "pe" and k == "pe":
                continue
            if self.seen[eng].get(k, 0) >= v:
                continue
            waits.append((k, v))
            self.seen[eng][k] = v
        if dma:
            idx = self.dma_rr[eng]
            self.dma_rr[eng] = (idx + 1) % self.NDMA
            key = ("dma", eng, idx)
            prev = self.dma_val.get(key, 0)
            if prev > 0 and self.seen[eng].get(key, 0) < prev:
                waits.append((key, prev))
                self.seen[eng][key] = prev
            val = prev + 16
            self.dma_val[key] = val
            tok = (key, val)
        else:
            self.count[eng] += 1
            tok = (eng, self.count[eng])
        self.ops[eng].append((waits, fn, tok))
        for r in reads:
            st = self.res.get(r)
            if not st:
                st = {"w": None, "r": {}}
                self.res[r] = st
            if st["r"].get(tok[0], 0) < tok[1]:
                st["r"][tok[0]] = tok[1]
        for w in writes:
            self.res[w] = {"w": tok, "r": {}}
        return tok

    def barrier(self):
        toks = [(e, self.count[e]) for e in self.ENG if self.count[e] > 0]
        toks += [(k, v) for k, v in self.dma_val.items()]
        for e in self.ENG:
            waits = []
            for k, v in toks:
                if k == e and e == "pe":
                    continue
                if self.seen[e].get(k, 0) >= v:
                    continue
                waits.append((k, v))
                self.seen[e][k] = v
            if waits:
                self.ops[e].append((waits, None, None))

    def finish(self):
        waits = [(k, v) for k, v in self.dma_val.items() if self.seen["sp"].get(k, 0) < v]
        self.ops["sp"].append((waits, None, None))

    def prepare(self):
        import bisect as _b
        sig = {e: set() for e in self.ENG}
        for e in self.ENG:
            for waits, fn, tok in self.ops[e]:
                for k, v in waits:
                    if k in sig and k != e:
                        sig[k].add(v)
        self.sig = {e: sorted(v) for e, v in sig.items()}
        self._b = _b

    def rank(self, k, v):
        lst = self.sig[k]
        i = self._b.bisect_right(lst, v)
        assert i > 0 and lst[i - 1] == v, (k, v)
        return i

    def replay(self, name, e, sems):
        sigset = set(self.sig[name])
        for waits, fn, tok in self.ops[name]:
            for k, v in waits:
                if k == name:
                    e.drain()
                elif k in self.sig:
                    e.wait_ge(sems[k], self.rank(k, v))
                else:
                    e.wait_ge(sems[k], v)
            if fn is None:
                continue
            ins = fn(e)
            if tok[0][0] == "dma":
                ins.then_inc(sems[tok[0]], 16)
            elif tok[1] in sigset:
                ins.then_inc(sems[tok[0]], 1)


def build(nslot=NSLOT, nexp=NEXP, debug=False, nkb=NKB, stop_after=9):
    nc = bass.Bass("TRN2", target_bir_lowering=False)
    S = Sched()
    import os
    S.maxops = int(os.environ.get("MAXOPS", 10 ** 9))
    nq = nslot * 128

    def din(name, shape, dt=F32):
        return nc.dram_tensor(name, list(shape), dt, kind="ExternalInput").ap()

    def dscr(name, shape, dt=BF16):
        return nc.dram_tensor(name, list(shape), dt, kind="Internal").ap()

    xk = din("xk", [TP, D])
    xq = din("xq", [nq, D])
    wk_d = din("wk", [D, WK_COLS])
    wq_d = din("wq", [D, WQ_COLS])
    wg_d = din("wg", [D, 2048])
    wkv_d = din("wkv", [256, 1024])
    wba_d = din("wba", [512, 1024])
    wbf_d = din("wbf", [512, 1024])
    wo_d = din("wo", [D, D])
    wr_d = din("wr", [D, 36])
    wgate_d = din("wgate", [nexp, D, 256])
    wup_d = din("wup", [nexp, D, 256])
    wdn_d = din("wdn", [nexp, 256, D])
    lnp_d = din("lnp", [128, 6 * D])
    kvg_d = din("kvg", [128, 256])
    bfg_d = din("bfg", [128, 8])
    brt_d = din("brt", [128, 36])
    ck_d = din("ck", [128, TP])
    sk_d = din("sk", [128, TP])
    cq_d = din("cq", [128, nq])
    sq_d = din("sq", [128, nq])
    cf32_d = din("cf32", [128, 128 * 3 + 512 + 4 + NBIS + 1])
    cbf_d = din("cbf", [128, 128 + 512 + 128 + 512], BF16)
    out_d = nc.dram_tensor("out", [nq, D], F32, kind="ExternalOutput").ap()

    kaT_d = dscr("kaT_d", [4, 128, TP])
    kfT_d = dscr("kfT_d", [4, 128, TP])
    va_d = dscr("va_d", [NKB, 128, 8 * VW])
    vf_d = dscr("vf_d", [NKB, 128, 8 * VW])
    qaT_d = dscr("qaT_d", [4, 128, nq])
    qiT_d = dscr("qiT_d", [4, 128, nq])
    qfT_d = dscr("qfT_d", [4, 128, nq])
    hqT_d = dscr("hqT_d", [8, 128, nq])
    h1_d = dscr("h1_d", [nq, D], F32)
    h1T_d = dscr("h1T_d", [8, 128, nq])

    es = ExitStack()
    with es:
        def sb(name, cols, dt=F32, parts=128):
            return es.enter_context(nc.sbuf_tensor(name, [parts, cols], dt))

        CF = sb("CF", 128 * 3 + 512 + 4 + NBIS + 1)
        CB = sb("CB", 128 + 512 + 128 + 512, BF16)
        LNP = sb("LNP", 6 * D)
        KVG = sb("KVG", 256)
        BFG = sb("BFG", 8)
        BRT = sb("BRT", 36)
        identF = CF[:, 0:128]
        utri = CF[:, 128:256]
        onesF = CF[:, 256:384]
        cmadd = CF[:, 384:896]
        selr = CF[:, 896:900]
        pow2 = CF[:, 900:900 + NBIS]
        padmask = CF[:, 900 + NBIS:901 + NBIS]
        identB = CB[:, 0:128]
        ident4 = CB[:, 128:640]
        ropeP = CB[:, 640:768]
        cmneg = CB[:, 768:1280]

        ARENA_COLS = 38 * 1024
        ARENA = sb("ARENA", ARENA_COLS)
        ARENA_B = ARENA[:, :].bitcast(BF16)

        class Bump:
            def __init__(self):
                self.off = 0

            def f32(self, cols):
                o = self.off // 4
                self.off += cols * 4
                assert self.off <= ARENA_COLS * 4, self.off
                return ARENA[:, o:o + cols]

            def bf(self, cols):
                cols2 = (cols + 1) // 2 * 2
                o = self.off // 2
                self.off += cols2 * 2
                assert self.off <= ARENA_COLS * 4, self.off
                return ARENA_B[:, o:o + cols]

        PF = [es.enter_context(nc.psum_tensor(f"PF{i}", [128, 512], F32)) for i in range(6)]
        PB = [es.enter_context(nc.psum_tensor(f"PB{i}", [128, 1024], BF16)) for i in range(2)]
        rr = {"n": 0, "b": 0}

        def pf(lo=0, hi=6):
            i = lo + rr["n"] % (hi - lo)
            rr["n"] += 1
            return PF[i], ("PF", i)

        def pb():
            i = rr["b"] % 2
            rr["b"] += 1
            return PB[i], ("PB", i)

        sems = {}
        for e in Sched.ENG:
            sems[e] = es.enter_context(nc.semaphore(f"s_{e}"))
        for e in ("sp", "pool", "act"):
            for i in range(Sched.NDMA):
                sems[("dma", e, i)] = es.enter_context(nc.semaphore(f"d_{e}{i}"))

        def dma(q, out, in_, reads, writes):
            S.op(q, lambda e, o=out, i=in_: e.dma_start(out=o, in_=i), reads, writes, dma=True)

        def mm(out, lhsT, rhs, start, stop, skip=True):
            return lambda e: e.matmul(out, lhsT, rhs, start=start, stop=stop, skip_group_check=skip)

        def mm_chain(items):
            def fn(e):
                ins = None
                for (o, l, r, st, sp_) in items:
                    ins = e.matmul(o, l, r, start=st, stop=sp_, skip_group_check=True)
                return ins
            return fn

        def act(out, in_, func, reads, writes, bias=0.0, scale=1.0, accum=None):
            if accum is None:
                S.op("act", lambda e: e.activation(out=out, in_=in_, func=func, bias=bias, scale=scale), reads, writes)
            else:
                S.op("act", lambda e: e.activation(out=out, in_=in_, func=func, bias=bias, scale=scale, accum_out=accum), reads, writes)

        def tt(eng, out, in0, in1, op, reads, writes):
            S.op(eng, lambda e: e.tensor_tensor(out=out, in0=in0, in1=in1, op=op), reads, writes)

        def ts(eng, out, in0, s1, s2, op0, op1, reads, writes, accum=None):
            if accum is None:
                if op1 is None:
                    S.op(eng, lambda e: e.tensor_scalar(out, in0, s1, None, op0), reads, writes)
                else:
                    S.op(eng, lambda e: e.tensor_scalar(out, in0, s1, s2, op0, op1), reads, writes)
            else:
                S.op(eng, lambda e: e.tensor_scalar(out, in0, s1, s2, op0, op1, accum), reads, writes)

        def stt(eng, out, in0, scalar, in1, op0, op1, reads, writes):
            S.op(eng, lambda e: e.scalar_tensor_tensor(out=out, in0=in0, scalar=scalar, in1=in1, op0=op0, op1=op1), reads, writes)

        def copy(eng, out, in_, reads, writes):
            if eng == "act":
                S.op("act", lambda e: e.copy(out, in_), reads, writes)
            else:
                S.op(eng, lambda e: e.tensor_copy(out, in_), reads, writes)

        def memset(eng, ap, val, writes):
            S.op(eng, lambda e: e.memset(ap, val), (), writes)

        def transpose_to(out_ps, in_sb, ident):
            return lambda e: e.transpose(out_ps, in_sb, ident)

        dma("sp", CF[:, :], cf32_d, (), ["CF"])
        dma("sp", CB[:, :], cbf_d, (), ["CB"])
        dma("sp", LNP[:, :], lnp_d, (), ["LNP"])
        dma("sp", KVG[:, :], kvg_d, (), ["KVG"])
        dma("sp", BFG[:, :], bfg_d, (), ["BFG"])
        dma("sp", BRT[:, :], brt_d, (), ["BRT"])
        CONST = ["CF", "CB", "LNP", "KVG", "BFG", "BRT"]

        uid = {"n": 0}

        def layer_norm_tile(x32, gcol, bcol, out_ap, tag, rx, wout, eps=1e-5, prescale=None):
            uid["n"] += 1
            u = uid["n"] % 2
            st = LNS[u]
            rs = ("LNS", u)
            rx = rx if isinstance(rx, list) else [rx]
            S.op("dve", lambda e: e.bn_stats(st[:, 0:6], x32[:, 0:512]), rx, [rs])
            S.op("dve", lambda e: e.bn_stats(st[:, 6:12], x32[:, 512:1024]), rx, [(rs, 1)])
            S.op("dve", lambda e: e.bn_aggr(st[:, 12:14], st[:, 0:12]), [rs, (rs, 1)], [(rs, 2)])
            ts("dve", st[:, 11:12], st[:, 13:14], eps, None, ALU.add, None, [(rs, 2), (rs, 1)], [(rs, 5)])
            act(st[:, 10:11], st[:, 11:12], AF.Ln, [(rs, 5)], [(rs, 6)])
            act(st[:, 14:15], st[:, 10:11], AF.Exp, [(rs, 6)], [(rs, 3)], scale=-0.5)
            stt("dve", st[:, 15:16], st[:, 12:13], -1.0, st[:, 14:15], ALU.mult, ALU.mult, [(rs, 2), (rs, 3)], [(rs, 4)])
            xn = LNX[u]
            rxn = ("LNX", u)
            act(xn[:, :], x32, AF.Identity, rx + [(rs, 3), (rs, 4)], [rxn], bias=st[:, 15:16], scale=st[:, 14:15])
            tt("dve", xn[:, :], xn[:, :], LNP[:, gcol * D:(gcol + 1) * D], ALU.mult, [rxn, "LNP"], [rxn])
            tt("pool", out_ap, xn[:, :], LNP[:, bcol * D:(bcol + 1) * D], ALU.add, [rxn, "LNP"], wout)

        LNS = [sb(f"LNS{i}", 16) for i in range(2)]
        LNX = [sb(f"LNX{i}", D) for i in range(2)]

        A = Bump()
        KIT = A.bf(TP)
        LOGF = A.f32(NKB * 8)
        WIQ = A.f32(nslot * 8)
        CC = A.f32(NKB * 8)
        PI = A.f32(NKB * 8)
        CREF = A.f32(nslot * 8)
        P1_END = A.off
        WKs = A.bf(8 * WK_COLS)
        WK = WKs.rearrange("p (k n) -> p k n", k=8)
        WQs = A.bf(8 * WQ_COLS)
        WQ = WQs.rearrange("p (k n) -> p k n", k=8)
        WKVs = A.bf(2 * 1024)
        WKV = WKVs.rearrange("p (k n) -> p k n", k=2)
        XT = [A.f32(D) for _ in range(2)]
        HB = [A.bf(D) for _ in range(2)]
        HT = [A.bf(8 * 512).rearrange("p (k n) -> p k n", k=8) for _ in range(2)]
        FMB = [A.bf(512) for _ in range(2)]
        RT1 = [A.f32(512) for _ in range(2)]
        RT2 = [A.f32(512) for _ in range(2)]
        CS = [A.f32(1024) for _ in range(2)]
        OUTB = [A.bf(512) for _ in range(3)]
        VST = [A.bf(8 * VW) for _ in range(2)]
        CKV32 = [A.f32(256) for _ in range(4)]
        CKVB = [A.bf(256) for _ in range(4)]
        CKVT = [A.bf(2 * 512).rearrange("p (k n) -> p k n", k=2) for _ in range(2)]
        SM = [A.f32(32) for _ in range(4)]

        dma("pool", WK, wk_d.rearrange("(k p) n -> p k n", p=128), (), ["WK"])
        dma("pool", WQ, wq_d.rearrange("(k p) n -> p k n", p=128), (), ["WQ"])
        dma("pool", WKV, wkv_d.rearrange("(k p) n -> p k n", p=128), (), ["WKV"])

        cnt = {"fm": 0, "ob": 0, "vs": 0, "ck": 0}
        if nkb < NKB:
            memset("pool", LOGF[:, :], 0.0, [("LOGF", kb) for kb in range(NKB)])

        def rope_store(ps, rps, n, cs_ap, rcs, dst_dram, rdst, dup_dst=None, rdup=None):
            i = cnt["fm"] % 2
            cnt["fm"] += 1
            copy("act", FMB[i][:, :n], ps[:, :n], [rps], [("FMB", i)])
            tt("dve", RT1[i][:, :n], ps[:, :n], cs_ap[:, 0:n], ALU.mult, [rps, rcs], [("RT1", i)])
            pp, rpp = pf()
            S.op("pe", mm(pp[:, :n], ropeP, FMB[i][:, :n], True, True), ["CB", ("FMB", i)], [rpp])
            tt("dve", RT2[i][:, :n], pp[:, :n], cs_ap[:, 512:512 + n], ALU.mult, [rpp, rcs], [("RT2", i)])
            o = cnt["ob"] % 3
            cnt["ob"] += 1
            if dup_dst is not None:
                tt("pool", dup_dst, RT1[i][:, :n], RT2[i][:, :n], ALU.add, [("RT1", i), ("RT2", i)], [rdup])
            else:
                tt("pool", OUTB[o][:, :n], RT1[i][:, :n], RT2[i][:, :n], ALU.add, [("RT1", i), ("RT2", i)], [("OUTB", o)])
                dma("sp", dst_dram, OUTB[o][:, :n], [("OUTB", o)], [rdst])

        def plain_store(ps, rps, n, dst_dram, rdst):
            o = cnt["ob"] % 3
            cnt["ob"] += 1
            copy("act", OUTB[o][:, :n], ps[:, :n], [rps], [("OUTB", o)])
            dma("sp", dst_dram, OUTB[o][:, :n], [("OUTB", o)], [rdst])

        def load_ln_transpose(src_rows, ti, hti, col0):
            b = ti % 2
            dma("sp", XT[b][:, :], src_rows, (), [("XT", b)])
            layer_norm_tile(XT[b], 0, 1, HB[b][:, :], "emb", ("XT", b), [("HB", b)])
            for half in range(2):
                tp_, rtp = pb()
                def fn(e, tp_=tp_, b=b, half=half):
                    ins = None
                    for kc in range(4):
                        k = half * 4 + kc
                        ins = e.transpose(tp_[:, kc * 128:(kc + 1) * 128], HB[b][:, k * 128:(k + 1) * 128], identB)
                    return ins
                S.op("pe", fn, [("HB", b), "CB"], [rtp])
                dst = HT[hti][:, half * 4:half * 4 + 4, col0:col0 + 128]
                src = tp_[:, 0:512].rearrange("p (k n) -> p k n", k=4)
                copy("act" if half == 0 else "dve", dst, src, [rtp], [("HT", hti, half, col0)])

        def ht_res(hti, ntile):
            return [("HT", hti, half, c * 128) for half in range(2) for c in range(ntile)]

        def proj_fm(W, col0, hti, n, rw):
            ps, rps = pf()
            items = [(ps[:, :n], W[:, kc, col0:col0 + 128], HT[hti][:, kc, :n], kc == 0, kc == 7) for kc in range(8)]
            S.op("pe", mm_chain(items), [rw] + ht_res(hti, (n + 127) // 128), [rps])
            return ps, rps

        nchunks = (nkb + 3) // 4 if stop_after >= 1 else 0
        def ln_chunk(ch):
            t0_ = ch * 4
            for t in range(min(4, nkb - t0_)):
                load_ln_transpose(xk[(t0_ + t) * 128:(t0_ + t + 1) * 128, :], t0_ + t, ch % 2, t * 128)

        if nchunks:
            ln_chunk(0)
        for ch in range(nchunks):
            t0 = ch * 4
            ntile = min(4, nkb - t0)
            n = ntile * 128
            k0 = t0 * 128
            hti = ch % 2
            if ch + 1 < nchunks:
                ln_chunk(ch + 1)
            ci = ch % 2
            dma("sp", CS[ci][:, 0:n], ck_d[:, k0:k0 + n], (), [("CS", ci, 0)])
            dma("sp", CS[ci][:, 512:512 + n], sk_d[:, k0:k0 + n], (), [("CS", ci, 1)])
            csr = [("CS", ci, 0), ("CS", ci, 1)]

            def rope_head(ps, rps):
                i = cnt["fm"] % 2
                cnt["fm"] += 1
                copy("act", FMB[i][:, :n], ps[:, :n], [rps], [("FMB", i)])
                tt("dve", RT1[i][:, :n], ps[:, :n], CS[ci][:, 0:n], ALU.mult, [rps] + csr, [("RT1", i)])
                return i

            def rope_tail(i, out_ap, rout):
                pp, rpp = pf()
                S.op("pe", mm(pp[:, :n], ropeP, FMB[i][:, :n], True, True), ["CB", ("FMB", i)], [rpp])
                tt("dve", RT2[i][:, :n], pp[:, :n], CS[ci][:, 512:512 + n], ALU.mult, [rpp] + csr, [("RT2", i)])
                tt("pool", out_ap, RT1[i][:, :n], RT2[i][:, :n], ALU.add, [("RT1", i), ("RT2", i)], [rout])

            def hres_t(t):
                return [("HT", hti, half, t * 128) for half in range(2)]

            for t in range(ntile):
                ps, rps = pf()
                items = [(ps[:, 0:256], HT[hti][:, kc, t * 128:(t + 1) * 128], WK[:, kc, 0:256], kc == 0, kc == 7) for kc in range(8)]
                S.op("pe", mm_chain(items), ["WK"] + hres_t(t), [rps])
                c = t
                act(CKV32[c][:, :], ps[:, 0:256], AF.Square, [rps], [("CKV32", c)], accum=SM[c][:, 0:1])
                ts("dve", SM[c][:, 1:2], SM[c][:, 0:1], 1.0 / 256.0, 1e-6, ALU.mult, ALU.add, [("CKV32", c)], [("SM", c, 1)])
                act(SM[c][:, 3:4], SM[c][:, 1:2], AF.Ln, [("SM", c, 1)], [("SM", c, 3)])
                act(SM[c][:, 2:3], SM[c][:, 3:4], AF.Exp, [("SM", c, 3)], [("SM", c, 2)], scale=-0.5)
                stt("dve", CKVB[c][:, :], ps[:, 0:256], SM[c][:, 2:3], KVG[:, :], ALU.mult, ALU.mult, [rps, ("SM", c, 2), "KVG"], [("CKVB", c)])
            ps, rps = proj_fm(WK, 256, hti, n, "WK")
            ki_i = rope_head(ps, rps)
            for ot in range(4):
                ps, rps = proj_fm(WK, 384 + ot * 128, hti, n, "WK")
                plain_store(ps, rps, n, kfT_d[ot, :, k0:k0 + n], ("kfT_d", ot, ch))
            for t in range(ntile):
                kb = t0 + t
                ps, rps = pf()
                items = [(ps[:, 0:512], HT[hti][:, kc, t * 128:(t + 1) * 128], WK[:, kc, 896:1408], kc == 0, kc == 7) for kc in range(8)]
                S.op("pe", mm_chain(items), ["WK"] + hres_t(t), [rps])
                v = cnt["vs"] % 2
                cnt["vs"] += 1
                VV = VST[v].rearrange("p (h c) -> p h c", h=8)
                memset("pool", VST[v][:, :], 1.0, [("VST", v)])
                if kb == 0:
                    ts("dve", VV[:, :, 0:64], ps[:, 0:512].rearrange("p (h c) -> p h c", h=8), padmask, None, ALU.mult, None, [rps, ("VST", v), "CF"], [("VST", v)])
                    ts("dve", VV[:, :, 64:65], VV[:, :, 64:65], padmask, None, ALU.mult, None, [("VST", v), "CF"], [("VST", v)])
                else:
                    copy("act", VV[:, :, 0:64], ps[:, 0:512].rearrange("p (h c) -> p h c", h=8), [rps, ("VST", v)], [("VST", v)])
                dma("sp", vf_d[kb], VST[v][:, :], [("VST", v)], [("vf_d", kb)])
                ps, rps = pf()
                items = [(ps[:, 0:8], HT[hti][:, kc, t * 128:(t + 1) * 128], WK[:, kc, 1408:1416], kc == 0, kc == 7) for kc in range(8)]
                S.op("pe", mm_chain(items), ["WK"] + hres_t(t), [rps])
                c2 = t
                tt("dve", SM[c2][:, 8:16], ps[:, 0:8], BFG[:, :], ALU.add, [rps, "BFG"], [("SM", c2, 8)])
                act(SM[c2][:, 16:24], SM[c2][:, 8:16], AF.Exp, [("SM", c2, 8)], [("SM", c2, 16)], scale=-1.0)
                act(SM[c2][:, 24:32], SM[c2][:, 16:24], AF.Ln, [("SM", c2, 16)], [("SM", c2, 24)], bias=1.0)
                if kb == 0:
                    stt("dve", LOGF[:, kb * 8:(kb + 1) * 8], SM[c2][:, 24:32], -1.0, padmask.to_broadcast([128, 8]), ALU.mult, ALU.mult, [("SM", c2, 24), "CF"], [("LOGF", kb)])
                else:
                    ts("dve", LOGF[:, kb * 8:(kb + 1) * 8], SM[c2][:, 24:32], -1.0, None, ALU.mult, None, [("SM", c2, 24)], [("LOGF", kb)])
            rope_tail(ki_i, KIT[:, k0:k0 + n], ("KIT", ch))
            for t in range(ntile):
                c = t
                tp_, rtp = pb()
                def fn(e, tp_=tp_, c=c):
                    ins = None
                    for kc in range(2):
                        ins = e.transpose(tp_[:, kc * 128:(kc + 1) * 128], CKVB[c][:, kc * 128:(kc + 1) * 128], identB)
                    return ins
                S.op("pe", fn, [("CKVB", c), "CB"], [rtp])
                copy("act", CKVT[hti][:, :, t * 128:(t + 1) * 128], tp_[:, 0:256].rearrange("p (k n) -> p k n", k=2), [rtp], [("CKVT", hti, t)])
            for t in range(ntile):
                kb = t0 + t
                ps, rps = pf()
                items = [(ps[:, 0:512], CKVT[hti][:, kc, t * 128:(t + 1) * 128], WKV[:, kc, 512:1024], kc == 0, kc == 1) for kc in range(2)]
                S.op("pe", mm_chain(items), ["WKV", ("CKVT", hti, t)], [rps])
                v = cnt["vs"] % 2
                cnt["vs"] += 1
                VV = VST[v].rearrange("p (h c) -> p h c", h=8)
                memset("pool", VST[v][:, :], 1.0, [("VST", v)])
                if kb == 0:
                    ts("dve", VV[:, :, 0:64], ps[:, 0:512].rearrange("p (h c) -> p h c", h=8), padmask, None, ALU.mult, None, [rps, ("VST", v), "CF"], [("VST", v)])
                    ts("dve", VV[:, :, 64:65], VV[:, :, 64:65], padmask, None, ALU.mult, None, [("VST", v), "CF"], [("VST", v)])
                else:
                    copy("act", VV[:, :, 0:64], ps[:, 0:512].rearrange("p (h c) -> p h c", h=8), [rps, ("VST", v)], [("VST", v)])
                dma("sp", va_d[kb], VST[v][:, :], [("VST", v)], [("va_d", kb)])
            for pair in range(2):
                heads_ = []
                for ot in (2 * pair, 2 * pair + 1):
                    ps, rps = pf()
                    items = [(ps[:, :n], WKV[:, kc, ot * 128:(ot + 1) * 128], CKVT[hti][:, kc, :n], kc == 0, kc == 1) for kc in range(2)]
                    S.op("pe", mm_chain(items), ["WKV"] + [("CKVT", hti, t) for t in range(ntile)], [rps])
                    heads_.append((ot, rope_head(ps, rps)))
                for ot, i in heads_:
                    o = cnt["ob"] % 3
                    cnt["ob"] += 1
                    rope_tail(i, OUTB[o][:, :n], ("OUTB", o))
                    dma("sp", kaT_d[ot, :, k0:k0 + n], OUTB[o][:, :n], [("OUTB", o)], [("kaT_d", ot, ch)])

        logf_res = [("LOGF", kb) for kb in range(NKB)]
        nb8 = NKB * 8
        for half in range(2):
            c0 = half * 264
            c1 = min(nb8, c0 + 264)
            ps, rps = pf()
            S.op("pe", mm(ps[:, 0:c1 - c0], utri, LOGF[:, c0:c1], True, True), ["CF"] + logf_res, [rps])
            copy("dve", CC[:, c0:c1], ps[:, 0:c1 - c0], [rps], [("CC", half)])
            ps2, rps2 = pf()
            S.op("pe", mm(ps2[:, 0:c1 - c0], onesF, LOGF[:, c0:c1], True, True), ["CF"] + logf_res, [rps2])
            copy("dve", PI[:, c0:c1], ps2[:, 0:c1 - c0], [rps2], [("PI", half)])
        for kb in range(1, NKB):
            tt("dve", PI[:, kb * 8:(kb + 1) * 8], PI[:, kb * 8:(kb + 1) * 8], PI[:, (kb - 1) * 8:kb * 8], ALU.add,
               [("PI", 0), ("PI", 1), ("PIx", kb - 1)], [("PIx", kb)])
        for kb in range(1, NKB):
            tt("pool", CC[:, kb * 8:(kb + 1) * 8], CC[:, kb * 8:(kb + 1) * 8], PI[:, (kb - 1) * 8:kb * 8], ALU.add,
               [("CC", 0), ("CC", 1), ("PIx", kb - 1), ("PIx", max(kb - 2, 0))], [("CCx", kb)])
        cc_res = [("CC", 0), ("CC", 1)] + [("CCx", kb) for kb in range(1, NKB)]
        pi_res = [("PI", 0), ("PI", 1)] + [("PIx", kb) for kb in range(1, NKB)]
        for j in range(nslot):
            ts("dve", CREF[:, j * 8:(j + 1) * 8], PI[:, (4 * j + 1) * 8:(4 * j + 2) * 8], selr[:, 0:1], None, ALU.mult, None, pi_res + ["CF"], [("CREF", j)])
            for t in range(1, 4):
                stt("dve", CREF[:, j * 8:(j + 1) * 8], PI[:, (4 * j + 1 + t) * 8:(4 * j + 2 + t) * 8], selr[:, t:t + 1], CREF[:, j * 8:(j + 1) * 8],
                    ALU.mult, ALU.add, pi_res + ["CF", ("CREF", j)], [("CREF", j)])

        for ch in range(nslot // 4 if nslot >= 4 else 1):
            ntile = min(4, nslot)
            n = ntile * 128
            q0 = ch * 512
            hti = ch % 2
            for t in range(ntile):
                load_ln_transpose(xq[q0 + t * 128:q0 + (t + 1) * 128, :], t, hti, t * 128)
            for kc in range(8):
                dma("sp", hqT_d[kc, :, q0:q0 + n], HT[hti][:, kc, :n], ht_res(hti, ntile), [("hqT_d", ch, kc)])
            ci = ch % 2
            dma("sp", CS[ci][:, 0:n], cq_d[:, q0:q0 + n], (), [("CS", ci, 0)])
            dma("sp", CS[ci][:, 512:512 + n], sq_d[:, q0:q0 + n], (), [("CS", ci, 1)])
            csr = [("CS", ci, 0), ("CS", ci, 1)]
            for grp, dst in ((0, qaT_d), (1, qiT_d)):
                for ot in range(4):
                    ps, rps = proj_fm(WQ, grp * 512 + ot * 128, hti, n, "WQ")
                    i = cnt["fm"] % 2
                    cnt["fm"] += 1
                    copy("act", FMB[i][:, :n], ps[:, :n], [rps], [("FMB", i)])
                    tt("dve", RT1[i][:, :n], ps[:, :n], CS[ci][:, 0:n], ALU.mult, [rps] + csr, [("RT1", i)])
                    pp, rpp = pf()
                    S.op("pe", mm(pp[:, :n], ropeP, FMB[i][:, :n], True, True), ["CB", ("FMB", i)], [rpp])
                    tt("dve", RT2[i][:, :n], pp[:, :n], CS[ci][:, 512:512 + n], ALU.mult, [rpp] + csr, [("RT2", i)])
                    o = cnt["ob"] % 3
                    cnt["ob"] += 1
                    tt("pool", OUTB[o][:, :n], RT1[i][:, :n], RT2[i][:, :n], ALU.add, [("RT1", i), ("RT2", i)], [("OUTB", o)])
                    dma("sp", dst[ot, :, q0:q0 + n], OUTB[o][:, :n], [("OUTB", o)], [(id(dst), ot, ch)])
            for ot in range(4):
                ps, rps = proj_fm(WQ, 1024 + ot * 128, hti, n, "WQ")
                plain_store(ps, rps, n, qfT_d[ot, :, q0:q0 + n], ("qfT_d", ot, ch))
            for t in range(ntile):
                sl = ch * 4 + t
                hres = [("HT", hti, half, t * 128) for half in range(2)]
                ps, rps = pf()
                items = [(ps[:, 0:8], HT[hti][:, kc, t * 128:(t + 1) * 128], WQ[:, kc, 1536:1544], kc == 0, kc == 7) for kc in range(8)]
                S.op("pe", mm_chain(items), ["WQ"] + hres, [rps])
                copy("dve", WIQ[:, sl * 8:(sl + 1) * 8], ps[:, 0:8], [rps], [("WIQ", sl)])

        S.barrier()

        A.off = P1_END
        SIDX = A.f32(TP)
        MNEGS = [A.bf(TP) for _ in range(2)]
        RL = [A.bf(512) for _ in range(8)]
        DW = A.bf(8 * 128)
        QI = A.bf(4 * 128).rearrange("p (t n) -> p t n", t=4)
        QA = A.bf(4 * 128).rearrange("p (t n) -> p t n", t=4)
        QF = A.bf(4 * 128).rearrange("p (t n) -> p t n", t=4)
        KS = [A.bf(4 * 512).rearrange("p (t n) -> p t n", t=4) for _ in range(2)]
        VS = [A.bf(4 * 8 * VW).rearrange("p (b n) -> p b n", b=4) for _ in range(2)]
        PT = [A.bf(1024) for _ in range(2)]
        BIASK = A.f32(NKB * 8)
        EFOX = A.f32(NKB * 8)
        BS = A.f32(64)
        HW = A.f32(NBIS)
        OSB = A.f32(8 * VW)
        RCP = A.f32(8)
        YB = A.bf(512)
        YT = A.bf(4 * 128)
        yaT_d = dscr("yaT_d", [4, 128, nq])
        yfT_d = dscr("yfT_d", [4, 128, nq])
        P2_END = A.off

        O_A = (PF[4], ("PF", 4))
        O_B = (PF[5], ("PF", 5))
        sc = {"k": 0}

        def attention(j, nk, kT_dram, v_dram, Qt, rQ, fox, yT_dram, tagname, hook):
            memset("dve", O_A[0][:, :], 0.0, [O_A[1]])
            memset("dve", O_B[0][:, :], 0.0, [O_B[1]])
            if hook is not None:
                hook()
            MNEG = MNEGS[j % 2]
            cbuf = {}

            def stage_a(kb):
                ch, kbl = divmod(kb, 4)
                if kbl == 0:
                    nb = min(4, nk - ch * 4)
                    w = nb * 128
                    k0 = ch * 512
                    cbuf[ch] = sc["k"] % 2
                    sc["k"] += 1
                    b = cbuf[ch]
                    dma("sp", KS[b][:, :, 0:w], kT_dram[:, :, k0:k0 + w].rearrange("t p k -> p t k"),
                        [(tagname + "kT", ot, ch) for ot in range(4)], [("KS", b)])
                    dma("sp", VS[b][:, 0:nb, :], v_dram[ch * 4:ch * 4 + nb].rearrange("b p c -> p b c"),
                        [(tagname + "v", kk) for kk in range(ch * 4, ch * 4 + nb)], [("VS", b)])
                    if fox:
                        vv = VS[b][:, 0:nb, :].rearrange("p b (h c) -> p b h c", h=8)
                        ee = EFOX[:, ch * 32:ch * 32 + nb * 8].rearrange("p (b h) -> p b h", h=8).unsqueeze(3).to_broadcast([128, nb, 8, VW])
                        tt("pool", vv, vv, ee, ALU.mult, [("VS", b), "EFOX"], [("VS", b)])
                b = cbuf[ch]
                tail = kb - (nk - 4)
                for par in range(2):
                    st_, rst = pf(0, 4)
                    items = []
                    if not fox:
                        items.append((st_[:, :], MNEG[:, kb * 128:(kb + 1) * 128], ident4, True, False))
                    elif tail >= 0:
                        items.append((st_[:, :], cmneg[:, tail * 128:(tail + 1) * 128], ident4, True, False))
                    masked = len(items) > 0
                    p0 = par * 64
                    for hh in range(4):
                        items.append((st_[:, hh * 128:(hh + 1) * 128],
                                      KS[b][p0:p0 + 64, hh, kbl * 128:(kbl + 1) * 128],
                                      Qt[p0:p0 + 64, hh, :], not masked, True))
                    reads = [("KS", b), rQ, "CB"]
                    if not fox:
                        reads.append(("MNEG", j % 2))
                    S.op("pe", mm_chain(items), reads, [rst])
                    act(PTcur(kb)[:, par * 512:(par + 1) * 512], st_[:, :], AF.Exp, [rst], [("PT", kb % 2, par)], scale=0.125)

            def stage_b(kb):
                ch, kbl = divmod(kb, 4)
                b = cbuf[ch]
                for hg, (O_, rO) in enumerate((O_A, O_B)):
                    items = []
                    for hh in range(4):
                        h = hg * 4 + hh
                        sl = (h % 2) * 4 + h // 2
                        items.append((O_[:, hh * VW:hh * VW + 65], PTcur(kb)[:, sl * 128:(sl + 1) * 128],
                                      VS[b][:, kbl, h * VW:h * VW + 65], False, False))
                    S.op("pe", mm_chain(items), [("PT", kb % 2, 0), ("PT", kb % 2, 1), ("VS", b)], [rO])

            stage_a(0)
            for kb in range(nk):
                if kb + 1 < nk:
                    stage_a(kb + 1)
                stage_b(kb)
            for hg, (O_, rO) in enumerate((O_A, O_B)):
                copy("dve", OSB[:, hg * 4 * VW:(hg + 1) * 4 * VW], O_[:, 0:4 * VW], [rO], [("OSB", hg)])
            OV = OSB.rearrange("p (h c) -> p h c", h=8)
            S.op("dve", lambda e: e.reciprocal(RCP[:, :], OV[:, :, 64]), [("OSB", 0), ("OSB", 1)], ["RCP"])
            for h in range(8):
                ts("dve", YB[:, h * 64:(h + 1) * 64], OV[:, h, 0:64], RCP[:, h:h + 1], None, ALU.mult, None,
                   [("OSB", 0), ("OSB", 1), "RCP"], [("YB", h)])
            tp_, rtp = pb()
            def fn(e, tp_=tp_):
                ins = None
                for kc in range(4):
                    ins = e.transpose(tp_[:, kc * 128:(kc + 1) * 128], YB[:, kc * 128:(kc + 1) * 128], identB)
                return ins
            S.op("pe", fn, [("YB", h) for h in range(8)] + ["CB"], [rtp])
            copy("act", YT[:, :], tp_[:, 0:512], [rtp], ["YT"])
            dma("sp", yT_dram[:, :, j * 128:(j + 1) * 128].rearrange("t p n -> p t n"), YT.rearrange("p (t n) -> p t n", t=4),
                ["YT"], [(tagname + "yT", j)])

        def kT_dram_name(x):
            return id(x)

        def PTcur(kb):
            return PT[kb % 2]

        def emit_idx(j):
            nk = 4 * j + 5
            n = nk * 128
            dma("sp", QI, qiT_d[:, :, j * 128:(j + 1) * 128].rearrange("t p n -> p t n"), (), ["QI"])
            for h in range(8):
                ts("dve", DW[:, h * 128:(h + 1) * 128], identB, WIQ[:, j * 8 + h:j * 8 + h + 1], None, ALU.mult, None,
                   ["CB", ("WIQ", j)], [("DW", h)])
            nch = (nk + 3) // 4
            for ch in range(nch):
                w = min(512, n - ch * 512)
                k0 = ch * 512
                for h in range(8):
                    p0 = (h % 2) * 64
                    z, rz = pf(0, 4)
                    S.op("pe", mm(z[:, :w], QI[p0:p0 + 64, h // 2, :], KIT[p0:p0 + 64, k0:k0 + w], True, True), ["QI", "KITall"], [rz])
                    if h % 2 == 0:
                        act(RL[h][:, :w], z[:, :w], AF.Relu, [rz], [("RL", h)])
                    else:
                        ts("dve", RL[h][:, :w], z[:, :w], 0.0, None, ALU.max, None, [rz], [("RL", h)])
                acc, racc = pf(4, 6)
                items = [(acc[:, :w], DW[:, h * 128:(h + 1) * 128], RL[h][:, :w], h == 0, h == 7) for h in range(8)]
                S.op("pe", mm_chain(items), [("DW", h) for h in range(8)] + [("RL", h) for h in range(8)], [racc])
                copy("dve", SIDX[:, k0:k0 + w], acc[:, :w], [racc], [("SIDX", ch)])

        def emit_bis(j):
            nk = 4 * j + 5
            n = nk * 128
            nch = (nk + 3) // 4
            MN = MNEGS[j % 2]
            rm = ("MNEG", j % 2)
            sres = [("SIDX", ch) for ch in range(nch)]
            S.op("dve", lambda e, n=n: e.tensor_reduce(BS[:, 0:1], SIDX[:, 0:n], AX.X, ALU.max, True), sres, [("BS", 0)])
            memset("dve", SIDX[:, 0:PAD], -1e30, [("SIDX", 0)])
            tt("dve", SIDX[:, n - 512:n], SIDX[:, n - 512:n], cmadd, ALU.add, sres + ["CF"], sres)
            ts("dve", BS[:, 2:3], BS[:, 0:1], -1.0, None, ALU.mult, None, [("BS", 0)], [("BS", 2)])
            ts("dve", BS[:, 1:2], BS[:, 0:1], 2.0, None, ALU.mult, None, [("BS", 0)], [("BS", 1)])
            ts("dve", HW[:, :], pow2, BS[:, 1:2], None, ALU.mult, None, ["CF", ("BS", 1)], ["HW"])
            for it in range(NBIS):
                tt("dve", BS[:, 3:4], BS[:, 2:3], HW[:, it:it + 1], ALU.add, [("BS", 2), "HW"], [("BS", 3)])
                ts("dve", MN[:, 0:n], SIDX[:, 0:n], BS[:, 3:4], 0.0, ALU.is_ge, ALU.add, sres + [("BS", 3)], [rm, ("BS", 4)], accum=BS[:, 4:5])
                stt("dve", BS[:, 5:6], BS[:, 4:5], 255.5, HW[:, it:it + 1], ALU.is_ge, ALU.mult, [("BS", 4), "HW"], [("BS", 5)])
                tt("dve", BS[:, 2:3], BS[:, 2:3], BS[:, 5:6], ALU.add, [("BS", 2), ("BS", 5)], [("BS", 2)])
            ts("dve", MN[:, 0:n], SIDX[:, 0:n], BS[:, 2:3], NEGM, ALU.is_lt, ALU.mult, sres + [("BS", 2)], [rm])

        nsl = nslot if stop_after >= 2 else 0
        if nsl:
            emit_idx(0)
            emit_bis(0)
        for j in range(nsl):
            nk = 4 * j + 5
            dma("sp", QA, qaT_d[:, :, j * 128:(j + 1) * 128].rearrange("t p n -> p t n"), (), ["QA"])
            dma("sp", QF, qfT_d[:, :, j * 128:(j + 1) * 128].rearrange("t p n -> p t n"), (), ["QF"])
            hook = None
            if j + 1 < nsl:
                emit_idx(j + 1)
                hook = (lambda jj=j + 1: emit_bis(jj))
            attention(j, nk, kaT_d, va_d, QA, "QA", False, yaT_d, "a", hook)
            BK = BIASK[:, 0:nk * 8].rearrange("p (k h) -> p k h", h=8)
            tt("dve", BK, CREF[:, j * 8:(j + 1) * 8].unsqueeze(1).to_broadcast([128, nk, 8]),
               CC[:, 0:nk * 8].rearrange("p (k h) -> p k h", h=8), ALU.subtract, [("CREF", j)] + cc_res, ["BIASK"])
            act(EFOX[:, 0:nk * 8], BIASK[:, 0:nk * 8], AF.Exp, ["BIASK"], ["EFOX"])
            attention(j, nk, kfT_d, vf_d, QF, "QF", True, yfT_d, "f", None)

        S.barrier()

        A.off = 0
        GATE = A.f32(nslot * 32)
        P3_KEEP = A.off
        WBA = A.bf(4 * 1024).rearrange("p (k n) -> p k n", k=4)
        WBF = A.bf(4 * 1024).rearrange("p (k n) -> p k n", k=4)
        WG = A.bf(8 * 2048).rearrange("p (k n) -> p k n", k=8)
        WO = A.bf(8 * 1024).rearrange("p (k n) -> p k n", k=8)
        WR = A.f32(8 * 36).rearrange("p (k n) -> p k n", k=8)
        H1TS = [A.bf(D).rearrange("p (k n) -> p k n", k=8) for _ in range(2)]
        YAT = [A.bf(512).rearrange("p (t n) -> p t n", t=4) for _ in range(2)]
        YFT = [A.bf(512).rearrange("p (t n) -> p t n", t=4) for _ in range(2)]
        HQ = [A.bf(1024).rearrange("p (k n) -> p k n", k=8) for _ in range(2)]
        XQ = [A.f32(D) for _ in range(2)]
        H32 = [A.f32(D) for _ in range(2)]
        GA = [A.f32(D) for _ in range(2)]
        GF = [A.f32(D) for _ in range(2)]
        MGB = [A.bf(D) for _ in range(2)]
        MGT = [A.bf(D).rearrange("p (k n) -> p k n", k=8) for _ in range(2)]
        H1P = [A.f32(D) for _ in range(2)]
        H1 = [A.f32(D) for _ in range(2)]
        H1B = [A.bf(D) for _ in range(2)]
        H1T32 = [A.f32(D).rearrange("p (k n) -> p k n", k=8) for _ in range(2)]
        RS = [A.f32(256) for _ in range(2)]

        dma("pool", WBA, wba_d.rearrange("(k p) n -> p k n", p=128), (), ["WBA"])
        dma("pool", WBF, wbf_d.rearrange("(k p) n -> p k n", p=128), (), ["WBF"])
        dma("pool", WG, wg_d.rearrange("(k p) n -> p k n", p=128), (), ["WG"])
        dma("pool", WO, wo_d.rearrange("(k p) n -> p k n", p=128), (), ["WO"])
        dma("sp", WR, wr_d.rearrange("(k p) n -> p k n", p=128), (), ["WR"])

        for i in range(nslot if stop_after >= 3 else 0):
            b = i % 2
            dma("sp", YAT[b], yaT_d[:, :, i * 128:(i + 1) * 128].rearrange("t p n -> p t n"), [("ayT", i)], [("YAT", b)])
            dma("sp", YFT[b], yfT_d[:, :, i * 128:(i + 1) * 128].rearrange("t p n -> p t n"), [("fyT", i)], [("YFT", b)])
            dma("sp", HQ[b], hqT_d[:, :, i * 128:(i + 1) * 128].rearrange("k p n -> p k n"),
                [("hqT_d", i // 4, kc) for kc in range(8)], [("HQ", b)])
            dma("sp", XQ[b][:, :], xq[i * 128:(i + 1) * 128, :], (), [("XQ", b)])
            layer_norm_tile(XQ[b], 0, 1, H32[b][:, :], "emb", ("XQ", b), [("H32", b)])
            for half in range(2):
                c0 = half * 512
                for (Wcol, GT, nm) in ((0, GA, "GA"), (1024, GF, "GF")):
                    ps, rps = pf()
                    items = [(ps[:, :], HQ[b][:, kc, :], WG[:, kc, Wcol + c0:Wcol + c0 + 512], kc == 0, kc == 7) for kc in range(8)]
                    S.op("pe", mm_chain(items), ["WG", ("HQ", b)], [rps])
                    act(GT[b][:, c0:c0 + 512], ps[:, :], AF.Sigmoid, [rps], [(nm, b, half)])
                ps, rps = pf()
                items = [(ps[:, :], YAT[b][:, kc, :], WBA[:, kc, c0:c0 + 512], kc == 0, kc == 3) for kc in range(4)]
                S.op("pe", mm_chain(items), ["WBA", ("YAT", b)], [rps])
                tt("dve", GA[b][:, c0:c0 + 512], ps[:, :], GA[b][:, c0:c0 + 512], ALU.mult, [rps, ("GA", b, half)], [("GA", b, half)])
                ps, rps = pf()
                items = [(ps[:, :], YFT[b][:, kc, :], WBF[:, kc, c0:c0 + 512], kc == 0, kc == 3) for kc in range(4)]
                S.op("pe", mm_chain(items), ["WBF", ("YFT", b)], [rps])
                tt("dve", GF[b][:, c0:c0 + 512], ps[:, :], GF[b][:, c0:c0 + 512], ALU.mult, [rps, ("GF", b, half)], [("GF", b, half)])
                tt("pool", MGB[b][:, c0:c0 + 512], GA[b][:, c0:c0 + 512], GF[b][:, c0:c0 + 512], ALU.add,
                   [("GA", b, half), ("GF", b, half)], [("MGB", b, half)])
            for half in range(2):
                tp_, rtp = pb()
                def fn(e, tp_=tp_, b=b, half=half):
                    ins = None
                    for kc in range(4):
                        k = half * 4 + kc
                        ins = e.transpose(tp_[:, kc * 128:(kc + 1) * 128], MGB[b][:, k * 128:(k + 1) * 128], identB)
                    return ins
                S.op("pe", fn, [("MGB", b, 0), ("MGB", b, 1), "CB"], [rtp])
                copy("act", MGT[b][:, half * 4:half * 4 + 4, :], tp_[:, 0:512].rearrange("p (k n) -> p k n", k=4), [rtp], [("MGT", b, half)])
            for half in range(2):
                c0 = half * 512
                ps, rps = pf()
                items = [(ps[:, :], MGT[b][:, kc, :], WO[:, kc, c0:c0 + 512], kc == 0, kc == 7) for kc in range(8)]
                S.op("pe", mm_chain(items), ["WO", ("MGT", b, 0), ("MGT", b, 1)], [rps])
                stt("dve", H1P[b][:, c0:c0 + 512], H32[b][:, c0:c0 + 512], ALPHA, ps[:, :], ALU.mult, ALU.add, [("H32", b), rps], [("H1P", b, half)])
            layer_norm_tile(H1P[b], 2, 3, H1[b][:, :], "ln1", [("H1P", b, 0), ("H1P", b, 1)], [("H1", b)])
            dma("sp", h1_d[i * 128:(i + 1) * 128, :], H1[b][:, :], [("H1", b)], [("h1_d", i)])
            copy("act", H1B[b][:, :], H1[b][:, :], [("H1", b)], [("H1B", b)])
            for half in range(2):
                tp_, rtp = pb()
                def fn(e, tp_=tp_, b=b, half=half):
                    ins = None
                    for kc in range(4):
                        k = half * 4 + kc
                        ins = e.transpose(tp_[:, kc * 128:(kc + 1) * 128], H1B[b][:, k * 128:(k + 1) * 128], identB)
                    return ins
                S.op("pe", fn, [("H1B", b), "CB"], [rtp])
                copy("act", H1TS[b][:, half * 4:half * 4 + 4, :], tp_[:, 0:512].rearrange("p (k n) -> p k n", k=4), [rtp], [("H1TS", b, half)])
            dma("sp", h1T_d[:, :, i * 128:(i + 1) * 128].rearrange("k p n -> p k n"), H1TS[b], [("H1TS", b, 0), ("H1TS", b, 1)], [("h1T_d", i)])
            for half in range(2):
                ps, rps = pf()
                def fn(e, ps=ps, b=b, half=half):
                    ins = None
                    for kc in range(4):
                        k = half * 4 + kc
                        ins = e.transpose(ps[:, kc * 128:(kc + 1) * 128], H1[b][:, k * 128:(k + 1) * 128], identF)
                    return ins
                S.op("pe", fn, [("H1", b), "CF"], [rps])
                copy("dve", H1T32[b][:, half * 4:half * 4 + 4, :], ps[:, :].rearrange("p (k n) -> p k n", k=4), [rps], [("H1T32", b, half)])
            ps, rps = pf()
            items = [(ps[:, 0:36], H1T32[b][:, kc, :], WR[:, kc, :], kc == 0, kc == 7) for kc in range(8)]
            S.op("pe", mm_chain(items), ["WR", ("H1T32", b, 0), ("H1T32", b, 1)], [rps])
            R = RS[b]
            rR = ("RS", b)
            k_ = [0]

            def rr_(nm):
                return ("RS", b, nm)
            tt("dve", R[:, 0:36], ps[:, 0:36], BRT[:, :], ALU.add, [rps, "BRT"], [rr_("lg")])
            S.op("dve", lambda e, R=R: e.tensor_reduce(R[:, 40:41], R[:, 0:4], AX.X, ALU.max), [rr_("lg")], [rr_("gmax")])
            ts("dve", R[:, 44:48], R[:, 0:4], R[:, 40:41], None, ALU.is_ge, None, [rr_("lg"), rr_("gmax")], [rr_("goh")])
            ts("dve", R[:, 41:42], R[:, 40:41], -1.0, None, ALU.mult, None, [rr_("gmax")], [rr_("ngmax")])
            act(R[:, 48:52], R[:, 0:4], AF.Exp, [rr_("lg"), rr_("ngmax")], [rr_("gexp"), rr_("gsum")], bias=R[:, 41:42], scale=1.0, accum=R[:, 42:43])
            S.op("dve", lambda e, R=R: e.reciprocal(R[:, 43:44], R[:, 42:43]), [rr_("gsum")], [rr_("pg")])
            ts("dve", R[:, 52:56], R[:, 44:48], -1.0, 1e30, ALU.add, ALU.mult, [rr_("goh")], [rr_("gpen")])
            tt("dve", R[:, 64:96].rearrange("p (g e) -> p g e", g=4), R[:, 4:36].rearrange("p (g e) -> p g e", g=4),
               R[:, 52:56].unsqueeze(2).to_broadcast([128, 4, 8]), ALU.add, [rr_("lg"), rr_("gpen")], [rr_("em")])
            S.op("dve", lambda e, R=R: e.tensor_reduce(R[:, 56:57], R[:, 64:96], AX.X, ALU.max), [rr_("em")], [rr_("m1")])
            ts("dve", R[:, 96:128], R[:, 64:96], R[:, 56:57], None, ALU.is_ge, None, [rr_("em"), rr_("m1")], [rr_("oh1")])
            stt("dve", R[:, 128:160], R[:, 96:128], -1e30, R[:, 64:96], ALU.mult, ALU.add, [rr_("oh1"), rr_("em")], [rr_("em2")])
            S.op("dve", lambda e, R=R: e.tensor_reduce(R[:, 57:58], R[:, 128:160], AX.X, ALU.max), [rr_("em2")], [rr_("m2")])
            ts("dve", R[:, 160:192], R[:, 128:160], R[:, 57:58], None, ALU.is_ge, None, [rr_("em2"), rr_("m2")], [rr_("oh2")])
            tt("dve", R[:, 58:59], R[:, 57:58], R[:, 56:57], ALU.subtract, [rr_("m1"), rr_("m2")], [rr_("dm")])
            act(R[:, 59:60], R[:, 58:59], AF.Exp, [rr_("dm")], [rr_("edm")])
            ts("dve", R[:, 60:61], R[:, 59:60], 1.0, None, ALU.add, None, [rr_("edm")], [rr_("den")])
            S.op("dve", lambda e, R=R: e.reciprocal(R[:, 61:62], R[:, 60:61]), [rr_("den")], [rr_("w1")])
            tt("dve", R[:, 62:63], R[:, 61:62], R[:, 43:44], ALU.mult, [rr_("w1"), rr_("pg")], [rr_("g1")])
            tt("dve", R[:, 63:64], R[:, 43:44], R[:, 62:63], ALU.subtract, [rr_("pg"), rr_("g1")], [rr_("g2")])
            ts("dve", R[:, 192:224], R[:, 96:128], R[:, 62:63], None, ALU.mult, None, [rr_("oh1"), rr_("g1")], [rr_("t1")])
            stt("dve", GATE[:, i * 32:(i + 1) * 32], R[:, 160:192], R[:, 63:64], R[:, 192:224], ALU.mult, ALU.add,
                [rr_("oh2"), rr_("g2"), rr_("t1")], [("GATE", i)])

        S.barrier()

        A.off = P3_KEEP
        H1T = A.bf(8 * nq).rearrange("p (k n) -> p k n", k=8)
        ACC = A.f32(nslot * D)
        dma("sp", H1T, h1T_d.rearrange("k p n -> p k n"), [("h1T_d", i) for i in range(nslot)], ["H1Tall"])
        WGT = [A.bf(8 * 256).rearrange("p (k n) -> p k n", k=8) for _ in range(2)]
        WUP = [A.bf(8 * 256).rearrange("p (k n) -> p k n", k=8) for _ in range(2)]
        WDN = [A.bf(2 * 1024).rearrange("p (k n) -> p k n", k=2) for _ in range(2)]
        SG = [A.f32(512) for _ in range(2)]
        AT = [A.bf(2 * 512).rearrange("p (f n) -> p f n", f=2) for _ in range(2)]
        FX = [A.f32(D) for _ in range(2)]
        FO = [A.f32(D) for _ in range(2)]

        memset("pool", ACC[:, :], 0.0, ["ACCall"])
        nchq = max(1, nq // 512)
        ac = {"n": 0}
        for ex in range(nexp if stop_after >= 4 else 0):
            b = ex % 2
            dma("pool", WGT[b], wgate_d[ex].rearrange("(k p) n -> p k n", p=128), (), [("WGT", b)])
            dma("pool", WUP[b], wup_d[ex].rearrange("(k p) n -> p k n", p=128), (), [("WUP", b)])
            dma("pool", WDN[b], wdn_d[ex].rearrange("(k p) n -> p k n", p=128), (), [("WDN", b)])
            for ch in range(nchq):
                n = min(512, nq)
                q0 = ch * 512
                a = ac["n"] % 2
                ac["n"] += 1
                hres = ["H1Tall"]
                for ft in range(2):
                    pg_, rpg = pf()
                    items = [(pg_[:, :n], WGT[b][:, kc, ft * 128:(ft + 1) * 128], H1T[:, kc, q0:q0 + n], kc == 0, kc == 7) for kc in range(8)]
                    S.op("pe", mm_chain(items), [("WGT", b)] + hres, [rpg])
                    pu_, rpu = pf()
                    items = [(pu_[:, :n], WUP[b][:, kc, ft * 128:(ft + 1) * 128], H1T[:, kc, q0:q0 + n], kc == 0, kc == 7) for kc in range(8)]
                    S.op("pe", mm_chain(items), [("WUP", b)] + hres, [rpu])
                    s = (ac["n"] + ft) % 2
                    act(SG[s][:, :n], pg_[:, :n], AF.Silu, [rpg], [("SG", s)])
                    tt("dve", AT[a][:, ft, :n], SG[s][:, :n], pu_[:, :n], ALU.mult, [("SG", s), rpu], [("AT", a, ft)])
                for t in range(n // 128):
                    i = ch * 4 + t
                    for half in range(2):
                        c0 = half * 512
                        py, rpy = pf()
                        items = [(py[:, :], AT[a][:, ft, t * 128:(t + 1) * 128], WDN[b][:, ft, c0:c0 + 512], ft == 0, ft == 1) for ft in range(2)]
                        S.op("pe", mm_chain(items), [("WDN", b), ("AT", a, 0), ("AT", a, 1)], [rpy])
                        stt("dve", ACC[:, i * D + c0:i * D + c0 + 512], py[:, :], GATE[:, i * 32 + ex:i * 32 + ex + 1],
                            ACC[:, i * D + c0:i * D + c0 + 512], ALU.mult, ALU.add,
                            [rpy, ("GATE", i), "ACCall", ("ACC", i, half)], [("ACC", i, half)])
        for i in range(nslot):
            b = i % 2
            dma("sp", FX[b][:, :], h1_d[i * 128:(i + 1) * 128, :], [("h1_d", i)], [("FX", b)])
            stt("dve", FX[b][:, :], FX[b][:, :], ALPHA, ACC[:, i * D:(i + 1) * D], ALU.mult, ALU.add,
                [("FX", b), ("ACC", i, 0), ("ACC", i, 1), "ACCall"], [("FX", b)])
            layer_norm_tile(FX[b], 4, 5, FO[b][:, :], "ln2", ("FX", b), [("FO", b)])
            dma("sp", out_d[i * 128:(i + 1) * 128, :], FO[b][:, :], [("FO", b)], [("out", i)])
        S.finish()
        S.prepare()

        with nc.Block() as block:
            @block.tensor
            def _(e):
                S.replay("pe", e, sems)

            @block.scalar
            def _(e):
                S.replay("act", e, sems)

            @block.vector
            def _(e):
                S.replay("dve", e, sems)

            @block.gpsimd
            def _(e):
                S.replay("pool", e, sems)

            @block.sync
            def _(e):
                S.replay("sp", e, sems)
    return nc


def _consts(r):
    bf = ml_dtypes.bfloat16
    ident = np.eye(128, dtype=np.float32)
    k = np.arange(128)
    utri = (k[:, None] <= k[None, :]).astype(np.float32)
    ones = np.ones((128, 128), np.float32)
    q = np.arange(128)[:, None]
    kk = np.arange(128)[None, :]
    tri_ok = kk <= q
    cm = np.zeros((128, 4, 128), bool)
    for t in range(4):
        if t < r:
            cm[:, t, :] = True
        elif t == r:
            cm[:, t, :] = tri_ok
    cmadd = np.where(cm, 0.0, -1e30).astype(np.float32).reshape(128, 512)
    cmneg = np.where(cm, 0.0, NEGM).astype(np.float32).reshape(128, 512)
    selr = np.zeros((128, 4), np.float32)
    selr[:, r] = 1.0
    pow2 = np.tile((2.0 ** -(np.arange(NBIS) + 1.0)).astype(np.float32)[None], (128, 1))
    padmask = (np.arange(128) >= PAD).astype(np.float32)[:, None]
    cf = np.concatenate([ident, utri, ones, cmadd, selr, pow2, padmask], axis=1).astype(np.float32)
    P = np.zeros((128, 128), np.float32)
    for hb in (0, 64):
        for d in range(8):
            P[hb + d + 8, hb + d] = -1.0
            P[hb + d, hb + d + 8] = 1.0
    cb = np.concatenate([ident, np.tile(ident, (1, 4)), P, cmneg], axis=1).astype(bf)
    return cf, cb


def _rope_tables(pos):
    half = 8
    inv = (500000.0 ** (-np.arange(half, dtype=np.float32) / half)).astype(np.float32)
    ang = pos.astype(np.float32)[None, :] * inv[:, None]
    c8, s8 = np.cos(ang).astype(np.float32), np.sin(ang).astype(np.float32)
    n = pos.shape[0]
    C = np.ones((128, n), np.float32)
    Sn = np.zeros((128, n), np.float32)
    for hb in (0, 64):
        C[hb:hb + 8] = c8
        C[hb + 8:hb + 16] = c8
        Sn[hb:hb + 8] = s8
        Sn[hb + 8:hb + 16] = s8
    return C, Sn


_NC_CACHE = {}


def kernel(x, meta_tokens, emb_ln_g, emb_ln_b, w_in, b_forget, kv_norm_g, w_kv_up, w_branch_dsa,
           w_branch_fox, w_out, ln1_g, ln1_b, w_router_group, b_router_group, w_router_expert,
           b_router_expert, w_gate, w_up, w_down, ln2_g, ln2_b):
    f = lambda a: np.ascontiguousarray(np.asarray(a, dtype=np.float32))
    x = f(x)
    w_in0 = f(w_in)[0]
    offs = np.cumsum([0, 512, 256, 512, 64, 8, 512, 512, 512, 8, 1024, 1024])
    seg = lambda i: w_in0[:, offs[i]:offs[i + 1]]
    q_a, c_kv, q_i, k_i, w_i, q_f, k_f, v_f, f_lg, g_a, g_f = [seg(i) for i in range(11)]
    wk = f(np.concatenate([c_kv, k_i, k_i, k_f, v_f, f_lg], axis=1))
    wq = f(np.concatenate([q_a, q_i, q_f, w_i], axis=1))
    wg = f(np.concatenate([g_a, g_f], axis=1))
    rep = lambda v: f(np.tile(np.asarray(v, np.float32).reshape(1, -1), (128, 1)))
    lnp = f(np.concatenate([rep(emb_ln_g), rep(emb_ln_b), rep(ln1_g[0]), rep(ln1_b[0]), rep(ln2_g[0]), rep(ln2_b[0])], axis=1))
    wr = f(np.concatenate([f(w_router_group)[0], f(w_router_expert)[0]], axis=1))
    brt = rep(np.concatenate([f(b_router_group)[0], f(b_router_expert)[0]]))
    common = dict(
        wk=wk, wq=wq, wg=wg, wkv=f(w_kv_up)[0], wba=f(w_branch_dsa)[0], wbf=f(w_branch_fox)[0], wo=f(w_out)[0],
        wr=wr, wgate=f(w_gate)[0][:NEXP_RUN], wup=f(w_up)[0][:NEXP_RUN], wdn=f(w_down)[0][:NEXP_RUN], lnp=lnp, kvg=rep(kv_norm_g[0]),
        bfg=rep(b_forget[0]), brt=brt,
    )
    kpos = np.arange(TP) - PAD
    ck, sk = _rope_tables(kpos)
    in_maps = []
    own = []
    for c in range(8):
        b, r = c // 4, c % 4
        xkc = np.zeros((TP, D), np.float32)
        xkc[PAD:PAD + NMETA] = f(meta_tokens)
        xkc[PAD + NMETA:] = x[b]
        blocks = [4 * j + r for j in range(NSLOT)]
        rows = np.concatenate([np.arange(bl * 128, (bl + 1) * 128) for bl in blocks])
        own.append((b, rows))
        cq, sq = _rope_tables(rows + NMETA)
        cf, cb = _consts(r)
        m = dict(common)
        m.update(xk=xkc, xq=f(x[b][rows]), ck=ck, sk=sk, cq=cq, sq=sq, cf32=cf, cbf=cb)
        in_maps.append(m)
    if "nc" not in _NC_CACHE:
        _NC_CACHE["nc"] = build()
    nc = _NC_CACHE["nc"]
    res = run_bass_kernel_spmd(nc, in_maps, core_ids=list(range(8)))
    out = np.zeros((2, SEQ, D), np.float32)
    for c in range(8):
        b, rows = own[c]
        out[b, rows] = np.asarray(res.results[c]["out"], dtype=np.float32)
    return out
```
